# Optimizing a Trainium2 kernel written in Bass

```python
import jax, jax.numpy as jnp
from jax import lax
import numpy as np

D_MODEL = 2048
BATCH = 4
SEQ = 4096
DEPTH = 1

PLE_DIM = 256
MIX_WIDTH = D_MODEL
RMS_EPS = 1e-6
RW_WIDTH = MIX_WIDTH // 2
RW_HEAD = 64
RW_HEADS = RW_WIDTH // RW_HEAD
RW_DECAY_LORA = 64
RW_AAA_LORA = 64
RW_GATE_LORA = 160
RW_GN_EPS = 64e-5
RW_COLS = 3 * RW_WIDTH + RW_DECAY_LORA + RW_AAA_LORA + RW_GATE_LORA
NSA_WIDTH = MIX_WIDTH - RW_WIDTH
NSA_HEAD = 64
NSA_HEADS = NSA_WIDTH // NSA_HEAD
NSA_KV_GROUPS = 4
NSA_REP = NSA_HEADS // NSA_KV_GROUPS
NSA_KV = NSA_KV_GROUPS * NSA_HEAD
CMP_LEN = 32
CMP_STRIDE = 16
CMP_HIDDEN = 256
SLC_BLOCK = 64
N_SELECT = 16
WINDOW = 512
Q_BLOCK = 64
NSA_COLS = NSA_WIDTH + 6 * NSA_KV + 3 * NSA_HEADS
IN_WIDTH = RW_COLS + NSA_COLS
N_EXPERTS = 32
TOP_K = 4
D_EXPERT = D_MODEL
SWIGLU_LIMIT = 7.0
SWIGLU_ALPHA = 1.702
EXPERT_ROWS = 512

kernel_name = "hymba_rwkv7_nsa_moe_ple_block"


def rmsnorm(x, g, eps=RMS_EPS):
    xf = x.astype(jnp.float32)
    y = xf * lax.rsqrt(jnp.mean(xf * xf, axis=-1, keepdims=True) + eps)
    return (y * g.astype(jnp.float32)).astype(x.dtype)


def split_cols(u, widths):
    return jnp.split(u, [int(o) for o in np.cumsum(widths)[:-1]], axis=-1)


def masked_softmax(s, mask):
    s = jnp.where(mask, s.astype(jnp.float32), -jnp.inf)
    m = jnp.max(s, axis=-1, keepdims=True)
    e = jnp.exp(s - jnp.where(jnp.isfinite(m), m, 0.0))
    return e / jnp.maximum(jnp.sum(e, axis=-1, keepdims=True), 1e-30)


def rwkv7_mixer(u, mu, w0, w2, a0, a2, g2, k_k, k_a, r_k, lnx_w, lnx_b):
    B, T, _ = u.shape
    f32 = jnp.float32
    shifted = jnp.pad(u, ((0, 0), (1, 0), (0, 0)))[:, :-1]
    u = u + (shifted - u) * mu
    r, k, v, xw, xa, xg = split_cols(u, [RW_WIDTH, RW_WIDTH, RW_WIDTH, RW_DECAY_LORA, RW_AAA_LORA, RW_GATE_LORA])
    w = -jax.nn.softplus(-(w0 + jnp.tanh(xw) @ w2)) - 0.5
    a = jax.nn.sigmoid(a0 + xa @ a2)
    g = jax.nn.sigmoid(xg) @ g2
    hd = lambda t: t.reshape(B, T, RW_HEADS, RW_HEAD)
    kk = hd(k * k_k).astype(f32)
    kk = kk / jnp.maximum(jnp.sqrt(jnp.sum(kk * kk, axis=-1, keepdims=True)), 1e-12)
    k = k * (1.0 + (a - 1.0) * k_a)
    r_h, k_h, v_h, a_h = hd(r).astype(f32), hd(k).astype(f32), hd(v).astype(f32), hd(a).astype(f32)
    decay = jnp.exp(-jnp.exp(hd(w).astype(f32)))
    a_vec, b_vec = -kk, kk * a_h

    def step(S, inp):
        r_t, w_t, k_t, v_t, a_t, b_t = inp
        sa = jnp.einsum('bhij,bhj->bhi', S, a_t)
        S = S * w_t[:, :, None, :] + sa[..., :, None] * b_t[..., None, :] + v_t[..., :, None] * k_t[..., None, :]
        return S, jnp.einsum('bhij,bhj->bhi', S, r_t)

    seqs = tuple(jnp.moveaxis(t, 1, 0) for t in (r_h, decay, k_h, v_h, a_vec, b_vec))
    S0 = jnp.zeros((B, RW_HEADS, RW_HEAD, RW_HEAD), f32)
    _, y = lax.scan(step, S0, seqs)
    y = jnp.moveaxis(y, 0, 1)
    mean = jnp.mean(y, axis=-1, keepdims=True)
    var = jnp.mean(jnp.square(y - mean), axis=-1, keepdims=True)
    y = ((y - mean) * lax.rsqrt(var + RW_GN_EPS)).reshape(B, T, RW_WIDTH) * lnx_w + lnx_b
    bonus = jnp.sum(r_h * k_h * r_k, axis=-1, keepdims=True) * v_h
    y = (y + bonus.reshape(B, T, RW_WIDTH)) * g
    return y.astype(u.dtype)


def nsa_mixer(u, q_norm, k_norm, pos_k, pos_v, ck_w1, ck_b1, ck_w2, ck_b2, cv_w1, cv_b1, cv_w2, cv_b2):
    B, T, _ = u.shape
    G, R, dk = NSA_KV_GROUPS, NSA_REP, NSA_HEAD
    q, kc, vc, ks, vs, kw, vw, gates = split_cols(u, [NSA_WIDTH] + [NSA_KV] * 6 + [3 * NSA_HEADS])
    q = rmsnorm(q.reshape(B, T, G, R, dk), q_norm)
    kc, vc, ks, vs, kw, vw = (t.reshape(B, T, G, dk) for t in (kc, vc, ks, vs, kw, vw))
    ks = rmsnorm(ks, k_norm[1])
    kw = rmsnorm(kw, k_norm[2])
    gates = jax.nn.sigmoid(gates.reshape(B, T, G, R, 3))
    scale = dk ** -0.5

    n_cmp = (T - CMP_LEN) // CMP_STRIDE + 1
    cmp_start = jnp.arange(n_cmp) * CMP_STRIDE
    idx = cmp_start[:, None] + jnp.arange(CMP_LEN)[None, :]

    def compress(t, pos, w1, b1, w2, b2):
        blk = t[:, idx] + pos[None, None, :, None, :]
        blk = blk.transpose(0, 1, 3, 2, 4).reshape(B, n_cmp, G, CMP_LEN * dk)
        return jax.nn.gelu(blk @ w1 + b1) @ w2 + b2

    Kc = rmsnorm(compress(kc, pos_k, ck_w1, ck_b1, ck_w2, ck_b2), k_norm[0])
    Vc = compress(vc, pos_v, cv_w1, cv_b1, cv_w2, cv_b2)
    cmp_end = cmp_start + CMP_LEN - 1

    n_slc = T // SLC_BLOCK
    n_sel = min(N_SELECT, n_slc)
    slc_start = jnp.arange(n_slc) * SLC_BLOCK
    overlap = ((cmp_start[:, None] < slc_start[None, :] + SLC_BLOCK)
               & (cmp_start[:, None] + CMP_LEN > slc_start[None, :])).astype(jnp.float32)
    Ks = ks.reshape(B, n_slc, SLC_BLOCK, G, dk).transpose(0, 3, 1, 2, 4)
    Vs = vs.reshape(B, n_slc, SLC_BLOCK, G, dk).transpose(0, 3, 1, 2, 4)
    b_ix = jnp.arange(B)[:, None, None]
    g_ix = jnp.arange(G)[None, :, None]
    Kw_pad = jnp.pad(kw, ((0, 0), (WINDOW, 0), (0, 0), (0, 0)))
    Vw_pad = jnp.pad(vw, ((0, 0), (WINDOW, 0), (0, 0), (0, 0)))

    def block(s):
        t_pos = s + jnp.arange(Q_BLOCK)
        qb = lax.dynamic_slice_in_dim(q, s, Q_BLOCK, axis=1)
        gb = lax.dynamic_slice_in_dim(gates, s, Q_BLOCK, axis=1)
        sc = jnp.einsum('bqgrd,bngd->bgrqn', qb, Kc) * scale
        p_c = masked_softmax(sc, cmp_end[None, :] <= t_pos[:, None])
        o_c = jnp.einsum('bgrqn,bngd->bqgrd', p_c.astype(Vc.dtype), Vc)
        imp = jnp.einsum('bgrqn,nj->bgqj', p_c, overlap)
        jj = jnp.arange(n_slc)[None, :]
        qblk = (t_pos // SLC_BLOCK)[:, None]
        forced = (jj == 0) | (jj == qblk) | (jj == qblk - 1)
        imp = jnp.where(forced, jnp.inf, imp)
        imp = jnp.where(slc_start[None, :] <= t_pos[:, None], imp, -jnp.inf)
        top_v, top_i = lax.top_k(imp, n_sel)
        blk_ok = top_v > -jnp.inf
        flat_i = top_i.reshape(B, G, Q_BLOCK * n_sel)
        Kg = Ks[b_ix, g_ix, flat_i].reshape(B, G, Q_BLOCK, n_sel * SLC_BLOCK, dk)
        Vg = Vs[b_ix, g_ix, flat_i].reshape(B, G, Q_BLOCK, n_sel * SLC_BLOCK, dk)
        kpos = top_i[..., None] * SLC_BLOCK + jnp.arange(SLC_BLOCK)
        ok = (blk_ok[..., None] & (kpos <= t_pos[None, None, :, None, None])).reshape(B, G, Q_BLOCK, -1)
        sc = jnp.einsum('bqgrd,bgqkd->bgrqk', qb, Kg) * scale
        p_s = masked_softmax(sc, ok[:, :, None])
        o_s = jnp.einsum('bgrqk,bgqkd->bqgrd', p_s.astype(Vg.dtype), Vg)
        Kwb = lax.dynamic_slice_in_dim(Kw_pad, s, WINDOW + Q_BLOCK, axis=1)
        Vwb = lax.dynamic_slice_in_dim(Vw_pad, s, WINDOW + Q_BLOCK, axis=1)
        wpos = s - WINDOW + jnp.arange(WINDOW + Q_BLOCK)
        wmask = ((wpos[None, :] <= t_pos[:, None]) & (wpos[None, :] > t_pos[:, None] - WINDOW)
                 & (wpos[None, :] >= 0))
        sc = jnp.einsum('bqgrd,bkgd->bgrqk', qb, Kwb) * scale
        p_w = masked_softmax(sc, wmask)
        o_w = jnp.einsum('bgrqk,bkgd->bqgrd', p_w.astype(Vwb.dtype), Vwb)
        return gb[..., 0:1] * o_c + gb[..., 1:2] * o_s + gb[..., 2:3] * o_w

    out = lax.map(block, jnp.arange(T // Q_BLOCK) * Q_BLOCK)
    return out.transpose(1, 0, 2, 3, 4, 5).reshape(B, T, NSA_WIDTH).astype(u.dtype)


def moe(h, router_w, router_b, w1, b1, w2, b2):
    B, T, D = h.shape
    xf = h.reshape(-1, D)
    N = xf.shape[0]
    logits = (xf @ router_w + router_b).astype(jnp.float32)
    top_v, top_e = lax.top_k(logits, TOP_K)
    gate = jax.nn.softmax(top_v, axis=-1)
    e_flat = top_e.reshape(-1)
    tok = jnp.repeat(jnp.arange(N), TOP_K)
    order = jnp.argsort(e_flat)
    e_sorted, tok_sorted, gw_sorted = e_flat[order], tok[order], gate.reshape(-1)[order]
    counts = jnp.bincount(e_flat, length=N_EXPERTS)
    start = jnp.cumsum(counts) - counts
    padded = (counts + EXPERT_ROWS - 1) // EXPERT_ROWS * EXPERT_ROWS
    pend = jnp.cumsum(padded)
    pstart = pend - padded
    dest = pstart[e_sorted] + (jnp.arange(N * TOP_K) - start[e_sorted])
    n_rows = (N * TOP_K + EXPERT_ROWS - 1) // EXPERT_ROWS * EXPERT_ROWS + N_EXPERTS * EXPERT_ROWS
    n_blk = n_rows // EXPERT_ROWS
    x_rows = jnp.zeros((n_rows, D), h.dtype).at[dest].set(xf[tok_sorted])
    blk_e = jnp.clip(jnp.searchsorted(pend, jnp.arange(n_blk) * EXPERT_ROWS, side='right'), 0, N_EXPERTS - 1)

    def expert_block(args):
        xb, e = args
        hh = xb @ w1[e] + b1[e]
        gt = jnp.minimum(hh[:, :D_EXPERT], SWIGLU_LIMIT)
        lin = jnp.clip(hh[:, D_EXPERT:], -SWIGLU_LIMIT, SWIGLU_LIMIT)
        act = gt * jax.nn.sigmoid(SWIGLU_ALPHA * gt) * (lin + 1.0)
        return act @ w2[e] + b2[e]

    y_rows = lax.map(expert_block, (x_rows.reshape(n_blk, EXPERT_ROWS, D), blk_e)).reshape(n_rows, D)
    y = jnp.zeros((N, D), jnp.float32).at[tok_sorted].add(y_rows[dest].astype(jnp.float32) * gw_sorted[:, None])
    return y.astype(h.dtype).reshape(B, T, D)


def setup_inputs(seed: int = 0) -> dict:
    key = jax.random.key(seed)
    ks = iter(jax.random.split(key, 40))
    nrm = lambda shape, s: jax.random.normal(next(ks), shape, jnp.float32) * s
    gain = lambda shape: 1.0 + nrm(shape, 0.02)
    uni = lambda shape, lo, hi: jax.random.uniform(next(ks), shape, jnp.float32, lo, hi)
    L, D = DEPTH, D_MODEL
    return {
        "x": nrm((BATCH, SEQ, D), 1.0),
        "p": nrm((DEPTH, BATCH, SEQ, PLE_DIM), 1.0),
        "mix_norm_g": gain((L, D)),
        "w_in": nrm((L, D, IN_WIDTH), D ** -0.5),
        "rw_mu": uni((L, RW_COLS), 0.0, 1.0),
        "rw_w0": uni((L, RW_WIDTH), -4.0, -0.5),
        "rw_w2": nrm((L, RW_DECAY_LORA, RW_WIDTH), 0.1),
        "rw_a0": nrm((L, RW_WIDTH), 0.1),
        "rw_a2": nrm((L, RW_AAA_LORA, RW_WIDTH), 0.5 * RW_AAA_LORA ** -0.5),
        "rw_g2": nrm((L, RW_GATE_LORA, RW_WIDTH), RW_GATE_LORA ** -0.5),
        "rw_k_k": 0.85 + nrm((L, RW_WIDTH), 0.02),
        "rw_k_a": gain((L, RW_WIDTH)),
        "rw_r_k": nrm((L, RW_HEADS, RW_HEAD), 0.1),
        "rw_lnx_w": gain((L, RW_WIDTH)),
        "rw_lnx_b": nrm((L, RW_WIDTH), 0.02),
        "nsa_q_norm": gain((L, NSA_HEAD)),
        "nsa_k_norm": gain((L, 3, NSA_HEAD)),
        "cmp_pos_k": nrm((L, CMP_LEN, NSA_HEAD), 0.02),
        "cmp_pos_v": nrm((L, CMP_LEN, NSA_HEAD), 0.02),
        "cmp_k_w1": nrm((L, CMP_LEN * NSA_HEAD, CMP_HIDDEN), (CMP_LEN * NSA_HEAD) ** -0.5),
        "cmp_k_b1": nrm((L, CMP_HIDDEN), 0.02),
        "cmp_k_w2": nrm((L, CMP_HIDDEN, NSA_HEAD), CMP_HIDDEN ** -0.5),
        "cmp_k_b2": nrm((L, NSA_HEAD), 0.02),
        "cmp_v_w1": nrm((L, CMP_LEN * NSA_HEAD, CMP_HIDDEN), (CMP_LEN * NSA_HEAD) ** -0.5),
        "cmp_v_b1": nrm((L, CMP_HIDDEN), 0.02),
        "cmp_v_w2": nrm((L, CMP_HIDDEN, NSA_HEAD), CMP_HIDDEN ** -0.5),
        "cmp_v_b2": nrm((L, NSA_HEAD), 0.02),
        "w_out": nrm((L, MIX_WIDTH, D), MIX_WIDTH ** -0.5),
        "moe_norm_g": gain((L, D)),
        "router_w": nrm((L, D, N_EXPERTS), D ** -0.5),
        "router_b": nrm((L, N_EXPERTS), 0.01),
        "moe_w1": nrm((L, N_EXPERTS, D, 2 * D_EXPERT), D ** -0.5),
        "moe_b1": nrm((L, N_EXPERTS, 2 * D_EXPERT), 0.02),
        "moe_w2": nrm((L, N_EXPERTS, D_EXPERT, D), D_EXPERT ** -0.5),
        "moe_b2": nrm((L, N_EXPERTS, D), 0.02),
        "ple_norm_g": gain((L, D)),
        "ple_w": nrm((L, PLE_DIM, D), PLE_DIM ** -0.5),
        "ple_gate_w": nrm((L, D, D), D ** -0.5),
    }


def reference(x, p, mix_norm_g, w_in, rw_mu, rw_w0, rw_w2, rw_a0, rw_a2, rw_g2, rw_k_k, rw_k_a, rw_r_k,
              rw_lnx_w, rw_lnx_b, nsa_q_norm, nsa_k_norm, cmp_pos_k, cmp_pos_v, cmp_k_w1, cmp_k_b1, cmp_k_w2,
              cmp_k_b2, cmp_v_w1, cmp_v_b1, cmp_v_w2, cmp_v_b2, w_out, moe_norm_g, router_w, router_b,
              moe_w1, moe_b1, moe_w2, moe_b2, ple_norm_g, ple_w, ple_gate_w):
    h = x
    for i in range(DEPTH):
        u = rmsnorm(h, mix_norm_g[i]) @ w_in[i]
        y_rw = rwkv7_mixer(u[..., :RW_COLS], rw_mu[i], rw_w0[i], rw_w2[i], rw_a0[i], rw_a2[i], rw_g2[i],
                           rw_k_k[i], rw_k_a[i], rw_r_k[i], rw_lnx_w[i], rw_lnx_b[i])
        y_nsa = nsa_mixer(u[..., RW_COLS:], nsa_q_norm[i], nsa_k_norm[i], cmp_pos_k[i], cmp_pos_v[i],
                          cmp_k_w1[i], cmp_k_b1[i], cmp_k_w2[i], cmp_k_b2[i],
                          cmp_v_w1[i], cmp_v_b1[i], cmp_v_w2[i], cmp_v_b2[i])
        h = h + jnp.concatenate([y_rw, y_nsa], axis=-1) @ w_out[i]
        h = h + moe(rmsnorm(h, moe_norm_g[i]), router_w[i], router_b[i], moe_w1[i], moe_b1[i], moe_w2[i], moe_b2[i])
        gate = jax.nn.sigmoid(rmsnorm(h, ple_norm_g[i]) @ ple_gate_w[i])
        h = h + gate * (p[i] @ ple_w[i])
    return h
```

```python
from contextlib import ExitStack
import numpy as np
import ml_dtypes
import concourse.bass as bass
import concourse.mybir as mybir
from concourse.bass_utils import run_bass_kernel_spmd

F32 = mybir.dt.float32
BF16 = mybir.dt.bfloat16
ALU = mybir.AluOpType
AF = mybir.ActivationFunctionType
AX = mybir.AxisListType

COMPUTE = ('pe', 'act', 'dve', 'pool')
QUEUES = ('sp', 'act', 'pool')
NDSEM = 20
WRITE_KEYS = ('out', 'ap', 'accum_out')


class T:
    def __init__(self, h, name):
        self.h = h; self.name = name
        self.lw = []; self.rd = []; self.prd = []

    def __getitem__(self, k):
        return A(self.h[k], self)


class A:
    def __init__(self, ap, t):
        self.ap = ap; self.t = t

    def __getitem__(self, k): return A(self.ap[k], self.t)
    def unsqueeze(self, i): return A(self.ap.unsqueeze(i), self.t)
    def to_broadcast(self, s): return A(self.ap.to_broadcast(list(s)), self.t)
    def rearrange(self, pat, **kw): return A(self.ap.rearrange(pat, **kw), self.t)
    def partition_broadcast(self, n): return A(self.ap.partition_broadcast(n), self.t)
    def bc(self, s): return A(self.ap.to_broadcast(list(s)), self.t)


class FW:
    def __init__(self, nc, es):
        self.nc = nc; self.es = es
        self.eng = {'pe': nc.tensor, 'act': nc.scalar, 'dve': nc.vector, 'pool': nc.gpsimd, 'sp': nc.sync}
        self.prog = {e: [] for e in self.eng}
        self.sem = {}; self.cnt = {}
        for e in COMPUTE:
            self.sem[e] = es.enter_context(nc.semaphore("s_" + e)); self.cnt[e] = 0
        self.dsem = {}; self.dcnt = {}; self.dnext = {}
        for q in QUEUES:
            self.dsem[q] = [es.enter_context(nc.semaphore("d_%s_%d" % (q, i))) for i in range(NDSEM)]
            self.dcnt[q] = [0] * NDSEM; self.dnext[q] = 0
        self.waited = {e: {} for e in self.eng}
        self.tiles = []; self.n_inst = 0

    def sb(self, name, shape, dtype=F32, es=None):
        h = (es or self.es).enter_context(self.nc.sbuf_tensor(name, list(shape), dtype))
        t = T(h, name); self.tiles.append(t); return t

    def ps(self, name, shape, dtype=F32, es=None):
        h = (es or self.es).enter_context(self.nc.psum_tensor(name, list(shape), dtype))
        t = T(h, name); self.tiles.append(t); return t

    def dram(self, name, shape, dtype=F32, kind="Internal"):
        h = self.nc.dram_tensor(name, list(shape), dtype, kind=kind).ap()
        t = T(h, name); self.tiles.append(t); return t

    def _collect(self, e, reads, writes, dma=False):
        own = None if dma else self.sem.get(e)
        waits = {}
        def add(tok, raw):
            s, v = tok
            if s is own and not raw: return
            k = id(s)
            if self.waited[e].get(k, 0) >= v: return
            if k not in waits or waits[k][1] < v: waits[k] = (s, v)
        for t in reads:
            for tok in t.lw: add(tok, True)
        for t in writes:
            for tok in t.rd: add(tok, False)
            for tok in t.prd: add(tok, False)
            for tok in t.lw: add(tok, False)
        return list(waits.values())

    @staticmethod
    def _compact(toks):
        best = {}
        for s, v in toks:
            k = id(s)
            if k not in best or best[k][1] < v: best[k] = (s, v)
        return list(best.values())

    def _update(self, tok, reads, writes, part):
        for t in reads:
            t.rd.append(tok)
            if len(t.rd) > 16: t.rd = self._compact(t.rd)
        for t in writes:
            if part and not t.rd:
                t.lw.append(tok)
                if len(t.lw) > 16: t.lw = self._compact(t.lw)
            else:
                t.prd = t.rd; t.rd = []; t.lw = [tok]

    def op(self, e, fn, reads=(), writes=(), part=False):
        waits = self._collect(e, reads, writes)
        for s, v in waits: self.waited[e][id(s)] = v
        sem = self.sem[e]; self.cnt[e] += 1
        tok = (sem, self.cnt[e])
        self.prog[e].append((waits, fn, sem, 1))
        self._update(tok, reads, writes, part)
        self.n_inst += 1
        return tok

    def I(self, e, meth, part=False, **kw):
        reads = []; writes = []; args = {}
        for k, v in kw.items():
            if isinstance(v, A):
                args[k] = v.ap
                if k in WRITE_KEYS:
                    writes.append(v.t)
                    if k == 'accum_out': reads.append(v.t)
                else:
                    reads.append(v.t)
            else:
                args[k] = v
        fn = lambda eng, m=meth, a=args: getattr(eng, m)(**a)
        return self.op(e, fn, reads, writes, part)

    def dma(self, q, out, in_, part=False, **kw):
        reads = [in_.t]; writes = [out.t]
        waits = self._collect(q, reads, writes, dma=True)
        i = self.dnext[q]; self.dnext[q] = (i + 1) % NDSEM
        s = self.dsem[q][i]
        if self.dcnt[q][i] > self.waited[q].get(id(s), 0):
            waits.append((s, self.dcnt[q][i]))
        for ss, v in waits:
            self.waited[q][id(ss)] = max(self.waited[q].get(id(ss), 0), v)
        self.dcnt[q][i] += 16
        tok = (s, self.dcnt[q][i])
        fn = lambda eng, o=out.ap, a=in_.ap, kw=kw: eng.dma_start(out=o, in_=a, **kw)
        self.prog[q].append((waits, fn, s, 16))
        self._update(tok, reads, writes, part)
        self.n_inst += 1
        return tok

    def barrier(self):
        targets = []
        for e in COMPUTE:
            if self.cnt[e] > 0: targets.append((self.sem[e], self.cnt[e]))
        for q in QUEUES:
            for i in range(NDSEM):
                if self.dcnt[q][i] > 0: targets.append((self.dsem[q][i], self.dcnt[q][i]))
        for e in self.eng:
            w = []
            for s, v in targets:
                if s is self.sem.get(e): continue
                if self.waited[e].get(id(s), 0) < v:
                    w.append((s, v)); self.waited[e][id(s)] = v
            if w: self.prog[e].append((w, None, None, 0))
        for t in self.tiles:
            t.lw = []; t.rd = []; t.prd = []

    def emit(self):
        with self.nc.Block() as block:
            def run(name):
                def body(eng):
                    for waits, fn, sem, inc in self.prog[name]:
                        for s, v in waits: eng.wait_ge(s, v)
                        if fn is not None: fn(eng).then_inc(sem, inc)
                return body
            block.tensor(run('pe')); block.scalar(run('act')); block.vector(run('dve'))
            block.gpsimd(run('pool')); block.sync(run('sp'))

    def mm(self, out, lhsT, rhs, start=True, stop=True):
        return self.I('pe', 'matmul', part=True, out=out, lhsT=lhsT, rhs=rhs, start=start, stop=stop)

    def tr(self, out, in_, identity):
        return self.I('pe', 'transpose', part=True, out=out, in_=in_, identity=identity)

    def tt(self, e, out, in0, in1, op, part=False):
        return self.I(e, 'tensor_tensor', part=part, out=out, in0=in0, in1=in1, op=op)

    def ts(self, e, out, in0, s1, op0, s2=None, op1=None, part=False):
        if op1 is None:
            return self.I(e, 'tensor_scalar', part=part, out=out, in0=in0, scalar1=s1, scalar2=None, op0=op0)
        return self.I(e, 'tensor_scalar', part=part, out=out, in0=in0, scalar1=s1, scalar2=s2, op0=op0, op1=op1)

    def cp(self, e, out, in_, part=False):
        if e == 'act':
            return self.I('act', 'activation', part=part, out=out, in_=in_, func=AF.Copy)
        return self.I(e, 'tensor_copy', part=part, out=out, in_=in_)

    def act(self, out, in_, func, part=False, **kw):
        return self.I('act', 'activation', part=part, out=out, in_=in_, func=func, **kw)


D = 2048; T_SEQ = 4096; IN_W = 5968
RMS_EPS = 1e-6


def rms_rstd(fw, xt, ss, junk, eps=RMS_EPS, d=D):
    fw.I('pool', 'memset', ap=ss[:, 0:1], constant=0.0)
    fw.act(junk, xt, AF.Square, accum_out=ss[:, 0:1])
    fw.act(ss[:, 1:2], ss[:, 0:1], AF.Sqrt, bias=eps, scale=1.0 / d)
    fw.I('dve', 'reciprocal', out=ss[:, 2:3], in_=ss[:, 1:2])


def stage_A(fw, C, x_d, w_in_d, g_d, u_d):
    with ExitStack() as es:
        xnT = fw.sb('A_xnT', [128, 16, 2048], BF16, es)
        wt = [fw.sb('A_wt%d' % i, [128, 16, 512], BF16, es) for i in range(2)]
        xt = [fw.sb('A_xt%d' % i, [128, 2048], F32, es) for i in range(2)]
        xb = [fw.sb('A_xb%d' % i, [128, 2048], BF16, es) for i in range(2)]
        junk = fw.sb('A_junk', [128, 2048], BF16, es)
        ss = [fw.sb('A_ss%d' % i, [128, 4], F32, es) for i in range(2)]
        ost = [fw.sb('A_ost%d' % i, [128, 512], F32, es) for i in range(4)]
        g_sb = fw.sb('A_g', [128, 16], F32, es)
        pT = [fw.ps('A_pT%d' % i, [128, 8, 128], BF16, es) for i in range(2)]
        pM = [fw.ps('A_pM%d' % i, [128, 512], F32, es) for i in range(4)]
        fw.dma('sp', g_sb[:], g_d[:])
        nblk = (IN_W + 511) // 512
        wi = 0; oi = 0
        for half in range(2):
            for tt in range(16):
                t0 = half * 2048 + tt * 128
                X = xt[tt % 2]; XB = xb[tt % 2]; S = ss[tt % 2]
                fw.dma('sp', X[:], x_d[t0:t0 + 128, :])
                rms_rstd(fw, X[:], S, junk[:])
                fw.ts('dve', XB[:], X[:], S[:, 2:3], ALU.mult)
                for hh in range(2):
                    P = pT[hh]
                    for j in range(8):
                        k = hh * 8 + j
                        fw.tr(P[:, j, :], XB[:, k * 128:(k + 1) * 128], C['ident_b'])
                    for j in range(8):
                        k = hh * 8 + j
                        fw.ts('dve' if j % 2 else 'pool_', xnT[:, k, tt * 128:(tt + 1) * 128], P[:, j, :], g_sb[:, k:k + 1], ALU.mult, part=True) if False else \
                            fw.act(xnT[:, k, tt * 128:(tt + 1) * 128], P[:, j, :], AF.Copy, scale=g_sb[:, k:k + 1], part=True) if j % 2 else \
                            fw.ts('dve', xnT[:, k, tt * 128:(tt + 1) * 128], P[:, j, :], g_sb[:, k:k + 1], ALU.mult, part=True)
            for cb in range(nblk):
                c0 = cb * 512; cw = min(512, IN_W - c0)
                W = wt[wi % 2]; wi += 1
                fw.dma('pool', W[:, :, 0:cw], w_in_d[:, c0:c0 + cw].rearrange("(k p) c -> p k c", p=128))
                for tt in range(16):
                    t0 = half * 2048 + tt * 128
                    P = pM[oi % 4]; O = ost[oi % 4]
                    for k in range(16):
                        fw.mm(P[:, 0:cw], xnT[:, k, tt * 128:(tt + 1) * 128], W[:, k, 0:cw], start=(k == 0), stop=(k == 15))
                    fw.cp('act' if oi % 2 else 'dve', O[:, 0:cw], P[:, 0:cw])
                    fw.dma('sp', u_d[t0:t0 + 128, c0:c0 + cw], O[:, 0:cw])
                    oi += 1
    fw.barrier()


RWC = 3360


def stage_B(fw, C, u_d, P_, yrw_d, hg, conv=None):
    NCH = 64; HW = 512
    c_r = hg * 512; c_k = 1024 + hg * 512; c_v = 2048 + hg * 512
    with ExitStack() as es:
        sb = lambda n, s, dt=F32: fw.sb('B%d_%s' % (hg, n), s, dt, es)
        U = sb('U', [64, 1824]); Us = sb('Us', [64, 1824]); mu = sb('mu', [64, 1824])
        bc = {n: sb(n, [64, HW]) for n in ('w0', 'a0', 'kkp', 'kap', 'rkp', 'lnw', 'lnb')}
        w2 = sb('w2', [64, HW]); a2 = sb('a2', [64, HW]); g2a = sb('g2a', [128, HW]); g2b = sb('g2b', [32, HW])
        names = ['logw', 'a', 'g', 'kk', 'km', 'b', 'W', 'Winv', 'Wprev', 'WCb', 'bt', 'kt', 't1', 't2', 'Y', 'yn']
        X = {n: sb(n, [64, HW]) for n in names}
        Vt = [sb('V%d' % i, [64, HW]) for i in range(2)]
        Bh = [sb('Bh%d' % i, [64, HW]) for i in range(2)]
        Kh = [sb('Kh%d' % i, [64, HW]) for i in range(2)]
        at = sb('at', [64, HW]); rt = sb('rt', [64, HW])
        aT = [sb('aT%d' % i, [64, HW]) for i in range(2)]; rT = [sb('rT%d' % i, [64, HW]) for i in range(2)]
        bT = sb('bT', [64, HW]); kT = sb('kT', [64, HW])
        Aab = sb('Aab', [64, HW]); AabT = sb('AabT', [64, HW])
        Aak = [sb('Aak%d' % i, [64, HW]) for i in range(2)]
        Abr = [sb('Abr%d' % i, [64, HW]) for i in range(2)]
        Akr = [sb('Akr%d' % i, [64, HW]) for i in range(2)]
        Tm = [sb('Tm%d' % i, [64, HW]) for i in range(2)]
        Pm = [sb('Pm%d' % i, [64, HW]) for i in range(2)]; PTm = [sb('PTm%d' % i, [64, HW]) for i in range(2)]
        XT = sb('XT', [64, HW]); SAT = sb('SAT', [64, HW]); ST = sb('ST', [64, HW])
        txw = sb('txw', [64, 64]); xaT = sb('xaT', [64, 64]); sg1 = sb('sg1', [128, 64]); sg2 = sb('sg2', [32, 64])
        sm = sb('sm', [64, 64]); wcc = [sb('wcc%d' % i, [64, 8]) for i in range(2)]
        pp = [fw.ps('B%d_p%d' % (hg, i), [128, 512], F32, es) for i in range(4)]
        pX = fw.ps('B%d_pX' % hg, [128, 512], F32, es); pS = fw.ps('B%d_pS' % hg, [128, 512], F32, es)
        pY = fw.ps('B%d_pY' % hg, [128, 512], F32, es); pN = fw.ps('B%d_pN' % hg, [128, 512], F32, es)
        pi = [0]
        def bank():
            pi[0] += 1; return pp[pi[0] % 4]
        I64 = C['ident_f'][0:64, 0:64]; ones = C['ones'][0:64, 0:64]
        triu_t = sb('triu', [64, 64]); fw.dma('sp', triu_t[:], P_['c_triu'][:, :]); tri = triu_t[:]
        mk_t = {}
        for n_ in ('mus', 'mls', 'mui', 'eye8'):
            mk_t[n_] = sb('m_' + n_, [64, HW]); fw.dma('sp', mk_t[n_][:], P_['c_' + n_][:, :])
        MUs = mk_t['mus'][:]; MLs = mk_t['mls'][:]; MUi = mk_t['mui'][:]; EYE = mk_t['eye8'][:]
        fw.dma('sp', mu[:], P_['mu'][:, :].partition_broadcast(64))
        for n in bc: fw.dma('sp', bc[n][:], P_[n][:, :].partition_broadcast(64))
        fw.dma('sp', w2[:], P_['w2'][:, :]); fw.dma('sp', a2[:], P_['a2'][:, :])
        fw.dma('sp', g2a[:], P_['g2'][0:128, :]); fw.dma('sp', g2b[:], P_['g2'][128:160, :])
        fw.I('pool', 'memset', ap=ST[:], constant=0.0)
        cols = ((c_r, 0), (c_k, 512), (c_v, 1024))
        for c in range(NCH):
            t0 = c * 64; d = c % 2
            if conv is not None:
                for _ in range(3): next(conv, None)
            for (cs, o) in cols:
                fw.dma('sp', U[:, o:o + 512], u_d[t0:t0 + 64, cs:cs + 512], part=True)
            fw.dma('sp', U[:, 1536:1824], u_d[t0:t0 + 64, 3072:3360], part=True)
            if c == 0:
                fw.I('pool', 'memset', ap=Us[:], constant=0.0)
                for (cs, o) in cols:
                    fw.dma('sp', Us[1:64, o:o + 512], u_d[0:63, cs:cs + 512], part=True)
                fw.dma('sp', Us[1:64, 1536:1824], u_d[0:63, 3072:3360], part=True)
            else:
                for (cs, o) in cols:
                    fw.dma('sp', Us[:, o:o + 512], u_d[t0 - 1:t0 + 63, cs:cs + 512], part=True)
                fw.dma('sp', Us[:, 1536:1824], u_d[t0 - 1:t0 + 63, 3072:3360], part=True)
            fw.tt('pool', Us[:], Us[:], U[:], ALU.subtract)
            fw.tt('pool', Us[:], Us[:], mu[:], ALU.mult)
            fw.tt('dve', U[:], U[:], Us[:], ALU.add)
            r = U[:, 0:512]; k = U[:, 512:1024]
            V = Vt[d]
            fw.cp('pool', V[:], U[:, 1024:1536])
            P1 = bank()
            fw.mm(P1[0:64, 0:64], U[:, 1536:1600], I64)
            fw.mm(P1[0:64, 64:128], U[:, 1600:1664], I64)
            fw.mm(P1[0:128, 128:192], U[:, 1664:1792], I64)
            fw.mm(P1[0:32, 192:256], U[:, 1792:1824], I64)
            fw.act(txw[:], P1[0:64, 0:64], AF.Tanh)
            fw.cp('dve', xaT[:], P1[0:64, 64:128])
            fw.act(sg1[:], P1[0:128, 128:192], AF.Sigmoid)
            fw.act(sg2[:], P1[0:32, 192:256], AF.Sigmoid)
            Pz = bank(); fw.mm(Pz[0:64, :], txw[:], w2[:])
            fw.tt('dve', X['t1'][:], Pz[0:64, :], bc['w0'][:], ALU.add)
            fw.act(X['t2'][:], X['t1'][:], AF.Sigmoid)
            fw.ts('pool', X['logw'][:], X['t2'][:], -0.6065306597126334, ALU.mult)
            Pa = bank(); fw.mm(Pa[0:64, :], xaT[:], a2[:])
            fw.tt('dve', X['t1'][:], Pa[0:64, :], bc['a0'][:], ALU.add)
            fw.act(X['a'][:], X['t1'][:], AF.Sigmoid)
            Pg = bank(); fw.mm(Pg[0:64, :], sg1[:], g2a[:], start=True, stop=False); fw.mm(Pg[0:64, :], sg2[:], g2b[:], start=False, stop=True)
            fw.cp('act', X['g'][:], Pg[0:64, :])
            fw.tt('pool', X['kk'][:], k, bc['kkp'][:], ALU.mult)
            fw.tt('pool', X['t1'][:], X['kk'][:], X['kk'][:], ALU.mult)
            fw.I('dve', 'tensor_reduce', out=sm[:, 0:8], in_=X['t1'][:].rearrange("p (h j) -> p h j", h=8), axis=AX.X, op=ALU.add)
            fw.act(sm[:, 8:16], sm[:, 0:8], AF.Sqrt)
            fw.ts('dve', sm[:, 8:16], sm[:, 8:16], 1e-12, ALU.max)
            fw.I('dve', 'reciprocal', out=sm[:, 16:24], in_=sm[:, 8:16])
            for h in range(8):
                fw.ts('dve', X['kk'][:, h * 64:(h + 1) * 64], X['kk'][:, h * 64:(h + 1) * 64], sm[:, 16 + h:17 + h], ALU.mult, part=True)
            fw.ts('pool', X['t1'][:], X['a'][:], -1.0, ALU.add)
            fw.tt('pool', X['t1'][:], X['t1'][:], bc['kap'][:], ALU.mult)
            fw.ts('pool', X['t1'][:], X['t1'][:], 1.0, ALU.add)
            fw.tt('dve', X['km'][:], k, X['t1'][:], ALU.mult)
            fw.tt('pool', X['b'][:], X['kk'][:], X['a'][:], ALU.mult)
            Pc = bank(); fw.mm(Pc[0:64, :], tri, X['logw'][:])
            fw.act(X['W'][:], Pc[0:64, :], AF.Exp)
            fw.act(X['Winv'][:], Pc[0:64, :], AF.Exp, scale=-1.0)
            fw.tt('dve', X['t1'][:], Pc[0:64, :], X['logw'][:], ALU.subtract)
            fw.act(X['Wprev'][:], X['t1'][:], AF.Exp)
            Pt = bank(); fw.mm(Pt[0:64, :], ones, X['logw'][:])
            fw.act(X['WCb'][:], Pt[0:64, :], AF.Exp)
            Pw = bank()
            for h in range(8):
                fw.mm(Pw[0:64, h:h + 1], X['logw'][:, h * 64:(h + 1) * 64], ones[:, 0:1])
            fw.act(wcc[d][:], Pw[0:64, 0:8], AF.Exp)
            fw.I('dve', 'scalar_tensor_tensor', out=at[:], in0=X['kk'][:], scalar=-1.0, in1=X['Wprev'][:], op0=ALU.mult, op1=ALU.mult)
            fw.tt('pool', X['bt'][:], X['b'][:], X['Winv'][:], ALU.mult)
            fw.tt('pool', X['kt'][:], X['km'][:], X['Winv'][:], ALU.mult)
            fw.tt('pool', rt[:], r, X['W'][:], ALU.mult)
            fw.tt('pool', Bh[d][:], X['bt'][:], X['WCb'][:], ALU.mult)
            fw.tt('pool', Kh[d][:], X['kt'][:], X['WCb'][:], ALU.mult)
            for (src, dst, e_) in ((at, aT[d], 'act'), (X['bt'], bT, 'dve'), (X['kt'], kT, 'act'), (rt, rT[d], 'dve')):
                Pq = bank()
                for h in range(8):
                    fw.mm(Pq[0:64, h * 64:(h + 1) * 64], src[:, h * 64:(h + 1) * 64], I64)
                fw.cp(e_, dst[:], Pq[0:64, :])
            for (l_, r_, dst, msk) in ((bT, aT[d], Aab, MUs), (aT[d], bT, AabT, MLs), (kT, aT[d], Aak[d], MUs), (bT, rT[d], Abr[d], MUi), (kT, rT[d], Akr[d], MUi)):
                Pq = bank()
                for h in range(8):
                    fw.mm(Pq[0:64, h * 64:(h + 1) * 64], l_[:, h * 64:(h + 1) * 64], r_[:, h * 64:(h + 1) * 64])
                fw.tt('dve', dst[:], Pq[0:64, :], msk, ALU.mult)
            T_ = Tm[d]
            fw.tt('pool', T_[:], Aab[:], EYE, ALU.add)
            Pc_, PTc_ = Aab, AabT
            for lvl in range(5):
                Pn, PTn = Pm[lvl % 2], PTm[lvl % 2]
                Pq = bank()
                for h in range(8):
                    hs = slice(h * 64, (h + 1) * 64)
                    fw.mm(Pq[0:64, hs], Pc_[:, hs], PTc_[:, hs])
                fw.cp('act', PTn[:], Pq[0:64, :])
                if lvl < 4:
                    Pq2 = bank()
                    for h in range(8):
                        hs = slice(h * 64, (h + 1) * 64)
                        fw.mm(Pq2[0:64, hs], PTc_[:, hs], Pc_[:, hs])
                    fw.cp('dve', Pn[:], Pq2[0:64, :])
                Pq3 = bank()
                for h in range(8):
                    hs = slice(h * 64, (h + 1) * 64)
                    fw.mm(Pq3[0:64, hs], PTn[:, hs], T_[:, hs])
                fw.tt('dve', T_[:], T_[:], Pq3[0:64, :], ALU.add)
                Pc_, PTc_ = Pn, PTn
            for h in range(8):
                hs = slice(h * 64, (h + 1) * 64)
                fw.mm(pX[0:64, hs], aT[d][:, hs], ST[:, hs], start=True, stop=False)
                fw.mm(pX[0:64, hs], Aak[d][:, hs], V[:, hs], start=False, stop=True)
            fw.cp('act', XT[:], pX[0:64, :])
            for h in range(8):
                hs = slice(h * 64, (h + 1) * 64)
                fw.mm(pS[0:64, hs], T_[:, hs], XT[:, hs])
            fw.cp('dve', SAT[:], pS[0:64, :])
            for h in range(8):
                hs = slice(h * 64, (h + 1) * 64)
                fw.mm(pY[0:64, hs], rT[d][:, hs], ST[:, hs], start=True, stop=False)
                fw.mm(pY[0:64, hs], Abr[d][:, hs], SAT[:, hs], start=False, stop=False)
                fw.mm(pY[0:64, hs], Akr[d][:, hs], V[:, hs], start=False, stop=True)
            fw.cp('act', X['Y'][:], pY[0:64, :])
            for h in range(8):
                hs = slice(h * 64, (h + 1) * 64)
                fw.mm(pN[0:64, hs], Bh[d][:, hs], SAT[:, hs], start=True, stop=False)
                fw.mm(pN[0:64, hs], Kh[d][:, hs], V[:, hs], start=False, stop=True)
            for h in range(8):
                hs = slice(h * 64, (h + 1) * 64)
                fw.I('dve', 'scalar_tensor_tensor', part=True, out=ST[:, hs], in0=ST[:, hs], scalar=wcc[d][:, h:h + 1], in1=pN[0:64, hs], op0=ALU.mult, op1=ALU.add)
            Y = X['Y']; yn = X['yn']; t1 = X['t1']; t2 = X['t2']
            Y3 = Y[:].rearrange("p (h j) -> p h j", h=8)
            fw.I('dve', 'tensor_reduce', out=sm[:, 24:32], in_=Y3, axis=AX.X, op=ALU.add)
            fw.tt('pool', t1[:], Y[:], Y[:], ALU.mult)
            fw.I('dve', 'tensor_reduce', out=sm[:, 32:40], in_=t1[:].rearrange("p (h j) -> p h j", h=8), axis=AX.X, op=ALU.add)
            fw.ts('dve', sm[:, 24:32], sm[:, 24:32], 1.0 / 64, ALU.mult)
            fw.tt('dve', sm[:, 40:48], sm[:, 24:32], sm[:, 24:32], ALU.mult)
            fw.I('dve', 'scalar_tensor_tensor', out=sm[:, 32:40], in0=sm[:, 32:40], scalar=1.0 / 64, in1=sm[:, 40:48], op0=ALU.mult, op1=ALU.subtract)
            fw.act(sm[:, 40:48], sm[:, 32:40], AF.Sqrt, bias=64e-5)
            fw.I('dve', 'reciprocal', out=sm[:, 48:56], in_=sm[:, 40:48])
            for h in range(8):
                hs = slice(h * 64, (h + 1) * 64)
                fw.ts('dve', yn[:, hs], Y[:, hs], sm[:, 24 + h:25 + h], ALU.subtract, sm[:, 48 + h:49 + h], ALU.mult, part=True)
            fw.tt('pool', yn[:], yn[:], bc['lnw'][:], ALU.mult)
            fw.tt('pool', yn[:], yn[:], bc['lnb'][:], ALU.add)
            fw.tt('pool', t1[:], r, X['km'][:], ALU.mult)
            fw.tt('pool', t1[:], t1[:], bc['rkp'][:], ALU.mult)
            fw.I('dve', 'tensor_reduce', out=sm[:, 56:64], in_=t1[:].rearrange("p (h j) -> p h j", h=8), axis=AX.X, op=ALU.add)
            for h in range(8):
                hs = slice(h * 64, (h + 1) * 64)
                fw.I('dve', 'scalar_tensor_tensor', part=True, out=t2[:, hs], in0=V[:, hs], scalar=sm[:, 56 + h:57 + h], in1=yn[:, hs], op0=ALU.mult, op1=ALU.add)
            fw.tt('pool', t2[:], t2[:], X['g'][:], ALU.mult)
            fw.dma('sp', yrw_d[t0:t0 + 64, hg * 512:(hg + 1) * 512], t2[:])
    fw.barrier()


DBG_C = 9
DBG_C0 = 9
NQ = 3360


def stage_C0(fw, C, u_d, Pn, KcT, Vaug, es_outer):
    with ExitStack() as es:
        sb = lambda n, s, dt=F32: fw.sb('C0_' + n, s, dt, es)
        kcT = sb('kcT', [64, 4, 4096], BF16); vcT = sb('vcT', [64, 4, 4096], BF16)
        Nt = [sb('N%d' % i, [128, 512]) for i in range(2)]
        Nb = [sb('Nb%d' % i, [128, 512], BF16) for i in range(2)]
        w1 = {kv: sb('w1' + kv, [64, 32, 256], BF16) for kv in 'kv'}
        w2 = {kv: sb('w2' + kv, [128, 2, 64], BF16) for kv in 'kv'}
        posT = {kv: sb('pos' + kv, [64, 32], BF16) for kv in 'kv'}
        b1 = {kv: sb('b1' + kv, [128, 2]) for kv in 'kv'}
        b2k = sb('b2k', [64, 1]); b2v = sb('b2v', [128, 64]); kn0 = sb('kn0', [64, 1])
        bias = {kv: sb('bias' + kv, [128, 2]) for kv in 'kv'}
        hid = [sb('hid%d' % i, [128, 256], BF16) for i in range(2)]
        x = sb('x', [128, 256]); x2 = sb('x2', [128, 256]); x3 = sb('x3', [128, 256]); sg = sb('sg', [128, 256])
        kc_f = sb('kc_f', [64, 256]); sq = sb('sq', [64, 256]); rs = sb('rs', [64, 256])
        vtmp = sb('vtmp', [128, 64])
        pT = [fw.ps('C0_pT%d' % i, [128, 4, 128], F32, es) for i in range(2)]
        pH = [fw.ps('C0_pH%d' % i, [128, 512], F32, es) for i in range(2)]
        pK = fw.ps('C0_pK', [128, 512], F32, es); pB = fw.ps('C0_pB', [128, 512], F32, es)
        for kv in 'kv':
            fw.dma('pool', w1[kv][:], Pn['w1' + kv][:, :].rearrange("(l d) h -> d l h", d=64))
            fw.dma('pool', w2[kv][:], Pn['w2' + kv][:, :].rearrange("(c p) d -> p c d", p=128))
            fw.dma('pool', posT[kv][:], Pn['posT' + kv][:, :])
            fw.dma('sp', b1[kv][:], Pn['b1' + kv][:, :])
        fw.dma('sp', b2k[:], Pn['b2k'][:, :]); fw.dma('sp', b2v[:], Pn['b2v'][:, :].partition_broadcast(128))
        fw.dma('sp', kn0[:], Pn['kn0'][:, :])
        fw.dma('sp', Vaug[:, 0, :, 65:129], Pn['ovl'][0:128, :, :], part=True)
        fw.dma('sp', Vaug[:, 1, :, 65:129], Pn['ovl'][128:256, :, :], part=True)
        fw.I('dve', 'memset', ap=Vaug[:, :, :, 64:65], constant=1.0, part=True)
        for tt in range(32 if DBG_C0 >= 1 else 0):
            t0 = tt * 128
            N = Nt[tt % 2]; NB = Nb[tt % 2]
            fw.dma('sp', N[:], u_d[t0:t0 + 128, NQ + 1024:NQ + 1536])
            fw.cp('dve', NB[:], N[:])
            for hf, dstT in ((0, kcT), (1, vcT)):
                P = pT[hf]
                for j in range(4):
                    fw.mm(P[0:64, j, :], NB[:, (hf * 4 + j) * 64:(hf * 4 + j + 1) * 64], C['ident_b'])
                fw.cp('act' if hf else 'dve', dstT[:, :, t0:t0 + 128], P[0:64, 0:4, :], part=True)
        for kv in ('kv' if DBG_C0 >= 2 else ''):
            for hh in range(2):
                for l in range(32):
                    fw.mm(pB[:, hh:hh + 1], w1[kv][:, l, hh * 128:(hh + 1) * 128], posT[kv][:, l:l + 1], start=(l == 0), stop=(l == 31))
            fw.tt('dve', bias[kv][:], pB[:, 0:2], b1[kv][:], ALU.add)
        hi = 0
        for kv in ('kv' if DBG_C0 >= 3 else ''):
            src = kcT if kv == 'k' else vcT
            for g in range(4):
                for hh in range(2):
                    P = pH[hi % 2]; hi += 1
                    for l in range(32):
                        fw.mm(P[:, 0:255], w1[kv][:, l, hh * 128:(hh + 1) * 128], (src[:, g, :].rearrange("p (n s) -> p n s", s=16)[:, 0:255, l] if l < 16 else src[:, g, :].rearrange("p (n s) -> p n s", s=16)[:, 1:256, l - 16]), start=(l == 0), stop=(l == 31))
                    fw.ts('dve', x[:, 0:255], P[:, 0:255], bias[kv][:, hh:hh + 1], ALU.add)
                    fw.tt('pool', x2[:, 0:255], x[:, 0:255], x[:, 0:255], ALU.mult)
                    fw.tt('pool', x3[:, 0:255], x2[:, 0:255], x[:, 0:255], ALU.mult)
                    fw.I('dve', 'scalar_tensor_tensor', out=x2[:, 0:255], in0=x3[:, 0:255], scalar=0.044715, in1=x[:, 0:255], op0=ALU.mult, op1=ALU.add)
                    fw.act(sg[:, 0:255], x2[:, 0:255], AF.Sigmoid, scale=1.5957691216057308)
                    fw.tt('dve', hid[hh][:, 0:255], x[:, 0:255], sg[:, 0:255], ALU.mult)
                if DBG_C0 < 4: continue
                if kv == 'k':
                    for hh in range(2):
                        fw.mm(pK[0:64, 0:255], w2['k'][:, hh, :], hid[hh][:, 0:255], start=(hh == 0), stop=(hh == 1))
                    fw.ts('dve', kc_f[:, 0:255], pK[0:64, 0:255], b2k[:, 0:1], ALU.add)
                    fw.tt('pool', sq[:, 0:255], kc_f[:, 0:255], kc_f[:, 0:255], ALU.mult)
                    fw.mm(pK[0:64, 256:511], C['ones'][0:64, 0:64], sq[:, 0:255])
                    fw.act(rs[:, 0:255], pK[0:64, 256:511], AF.Sqrt, bias=RMS_EPS, scale=1.0 / 64)
                    fw.I('dve', 'reciprocal', out=rs[:, 0:255], in_=rs[:, 0:255])
                    fw.tt('pool', kc_f[:, 0:255], kc_f[:, 0:255], rs[:, 0:255], ALU.mult)
                    fw.ts('dve', KcT[:, g, 0:255], kc_f[:, 0:255], kn0[:, 0:1], ALU.mult, part=True)
                else:
                    for c in range(2):
                        n = 128 if c == 0 else 127
                        for hh in range(2):
                            fw.mm(pK[0:n, 0:64], hid[hh][:, c * 128:c * 128 + n], w2['v'][:, hh, :], start=(hh == 0), stop=(hh == 1))
                        fw.tt('dve', Vaug[0:n, c, g, 0:64], pK[0:n, 0:64], b2v[0:n, :], ALU.add, part=True)
    fw.barrier()


def stage_C(fw, C, u_d, Pn, ynsa_d):
    with ExitStack() as es:
        sb = lambda n, s, dt=F32: fw.sb('C_' + n, s, dt, es)
        KcT = sb('KcT', [64, 4, 256], BF16); Vaug = sb('Vaug', [128, 2, 4, 129], BF16)
        fw.I('dve', 'memset', ap=KcT[:], constant=0.0)
        fw.I('dve', 'memset', ap=Vaug[:], constant=0.0)
        stage_C0(fw, C, u_d, Pn, KcT, Vaug, es)
        if DBG_C < 1:
            return
        ksT = sb('ksT', [64, 4, 4096], BF16); kwT = sb('kwT', [64, 4, 4096], BF16)
        Vs = sb('Vs', [128, 32, 4, 65], BF16); Vw = sb('Vw', [128, 32, 4, 65], BF16)
        Nt = [sb('N0', [128, 2608])] * 2
        Eb = sb('Eb', [128, 32, 4, 128], BF16)
        Ew = sb('Ew', [128, 5, 4, 128], BF16)
        sq = sb('sq', [128, 1024]); qn = sb('qn', [128, 1024], BF16); kn = sb('kn', [128, 512], BF16)
        qnb = sb('qnb', [128, 1024]); knb = sb('knb', [128, 512])
        st = sb('st', [128, 96]); gat = sb('gat', [128, 48])
        qT = [sb('qT%d' % i, [64, 4, 128], BF16) for i in range(2)]
        E = [sb('E%d' % i, [128, 4, 128], BF16) for i in range(3)]
        cm = [sb('cm%d' % i, [128, 2, 128], BF16) for i in range(2)]
        keep = [sb('keep%d' % i, [128, 64]) for i in range(2)]; addm = [sb('addm%d' % i, [128, 64]) for i in range(2)]
        selx = sb('selx', [64, 32, 128], BF16)
        cauT = sb('cauT', [128, 128], BF16); acauT = sb('acauT', [128, 128], BF16)
        imp = sb('imp', [128, 64]); mr = sb('mr', [128, 64]); selm = sb('selm', [128, 64], BF16); selT = sb('selT', [64, 128], BF16)
        t8 = sb('t8', [128, 24]); rsd = sb('rsd', [128, 16]); coef = sb('coef', [128, 16])
        yt = [sb('yt0', [128, 1024])] * 2
        qnw = sb('qnw', [128, 1024]); knw = sb('knw', [128, 512])
        pS = [fw.ps('C_pS%d' % i, [128, 512], F32, es) for i in range(2)]
        pM = fw.ps('C_pM', [128, 512], F32, es)
        pT = fw.ps('C_pT', [128, 4, 128], F32, es)
        pOc = [fw.ps('C_pOc%d' % i, [128, 2, 129], F32, es) for i in range(2)]
        pOs = fw.ps('C_pOs', [128, 4, 65], F32, es); pOw = fw.ps('C_pOw', [128, 4, 65], F32, es)
        fw.dma('sp', qnw[:], Pn['qnw'][:, :].partition_broadcast(128))
        fw.dma('sp', knw[:], Pn['knw'][:, :].partition_broadcast(128))
        fw.dma('sp', selx[:], Pn['selx'][:, :, :]); fw.dma('sp', cauT[:], Pn['cauT'][:, :]); fw.dma('sp', acauT[:], Pn['acauT'][:, :])
        fw.I('dve', 'memset', ap=Vs[:, :, :, 64:65], constant=1.0, part=True)
        fw.I('dve', 'memset', ap=Vw[:, :, :, 64:65], constant=1.0, part=True)
        ei = 0; si = 0
        for i in range(32):
            t0 = i * 128
            N = Nt[i % 2]
            fw.dma('sp', N[:], u_d[t0:t0 + 128, NQ:NQ + 2608])
            fw.dma('sp', cm[i % 2][:], Pn['cmask'][i, :, :, :])
            fw.dma('sp', keep[i % 2][:], Pn['keep'][i, :, :]); fw.dma('sp', addm[i % 2][:], Pn['addm'][i, :, :])
            fw.tt('pool', sq[:], N[:, 0:1024], N[:, 0:1024], ALU.mult)
            fw.I('dve', 'tensor_reduce', out=st[:, 0:16], in_=sq[:].rearrange("p (h j) -> p h j", h=16), axis=AX.X, op=ALU.add)
            fw.act(st[:, 16:32], st[:, 0:16], AF.Sqrt, bias=RMS_EPS, scale=1.0 / 64)
            fw.I('dve', 'reciprocal', out=st[:, 32:48], in_=st[:, 16:32])
            fw.ts('dve', st[:, 32:48], st[:, 32:48], 0.125, ALU.mult)
            for h in range(16):
                fw.ts('dve', qnb[:, h * 64:(h + 1) * 64], N[:, h * 64:(h + 1) * 64], st[:, 32 + h:33 + h], ALU.mult, part=True)
            fw.tt('dve', qn[:], qnb[:], qnw[:], ALU.mult)
            for (bi, off, dstT, Vd) in ((0, 1536, ksT, Vs), (1, 2048, kwT, Vw)):
                fw.tt('pool', sq[:, 0:256], N[:, off:off + 256], N[:, off:off + 256], ALU.mult)
                fw.I('dve', 'tensor_reduce', out=st[:, 48:52], in_=sq[:, 0:256].rearrange("p (h j) -> p h j", h=4), axis=AX.X, op=ALU.add)
                fw.act(st[:, 52:56], st[:, 48:52], AF.Sqrt, bias=RMS_EPS, scale=1.0 / 64)
                fw.I('dve', 'reciprocal', out=st[:, 56:60], in_=st[:, 52:56])
                for g in range(4):
                    fw.ts('dve', knb[:, bi * 256 + g * 64:bi * 256 + (g + 1) * 64], N[:, off + g * 64:off + (g + 1) * 64], st[:, 56 + g:57 + g], ALU.mult, part=True)
                fw.tt('dve', kn[:, bi * 256:(bi + 1) * 256], knb[:, bi * 256:(bi + 1) * 256], knw[:, bi * 256:(bi + 1) * 256], ALU.mult, part=True)
                for g in range(4):
                    fw.mm(pT[0:64, g, :], kn[:, bi * 256 + g * 64:bi * 256 + (g + 1) * 64], C['ident_b'])
                fw.cp('act', dstT[:, :, t0:t0 + 128], pT[0:64, 0:4, :], part=True)
                fw.cp('dve', Vd[:, i, :, 0:64], N[:, off + 256:off + 512].rearrange("p (g d) -> p g d", g=4), part=True)
            fw.act(gat[:], N[:, 2560:2608], AF.Sigmoid)
            y = yt[i % 2]
            for g in range(4 if DBG_C >= 2 else 0):
                Q = qT[si % 2]; si += 1
                for r_ in range(4):
                    h = g * 4 + r_
                    fw.mm(pT[0:64, r_, :], qn[:, h * 64:(h + 1) * 64], C['ident_b'])
                fw.cp('act', Q[:], pT[0:64, 0:4, :])
                Q2 = Q[:].rearrange("p r q -> p (r q)")
                nchunk = 1 if (8 * i + 6) < 128 else 2
                Ets = []
                for c in range(nchunk):
                    n = 128 if c == 0 else 127
                    P = pS[ei % 2]; Et = E[ei % 3]; ei += 1
                    Ets.append(Et)
                    fw.mm(P[0:n, :], KcT[:, g, c * 128:c * 128 + n], Q2)
                    fw.act(Et[0:n].rearrange("p r q -> p (r q)"), P[0:n, :], AF.Exp)
                    for r_ in range(4):
                        fw.tt('dve', Et[0:n, r_, :], Et[0:n, r_, :], cm[i % 2][0:n, c, :], ALU.mult, part=True)
                for r_ in range(4):
                    for c in range(nchunk):
                        n = 128 if c == 0 else 127
                        fw.mm(pOc[r_ // 2][:, r_ % 2, :], Ets[c][0:n, r_, :], Vaug[0:n, c, g, :], start=(c == 0), stop=(c == nchunk - 1))
                for r_ in range(4):
                    fw.ts('dve', rsd[:, r_:r_ + 1], pOc[r_ // 2][:, r_ % 2, 64:65], 1e-30, ALU.max, part=True)
                fw.I('dve', 'reciprocal', out=rsd[:, 4:8], in_=rsd[:, 0:4])
                fw.ts('dve', imp[:], pOc[0][:, 0, 65:129], rsd[:, 4:5], ALU.mult)
                for r_ in range(1, 4):
                    fw.I('dve', 'scalar_tensor_tensor', out=imp[:], in0=pOc[r_ // 2][:, r_ % 2, 65:129], scalar=rsd[:, 4 + r_:5 + r_], in1=imp[:], op0=ALU.mult, op1=ALU.add)
                if DBG_C < 3: continue
                fw.tt('dve', imp[:], imp[:], keep[i % 2][:], ALU.mult)
                fw.tt('dve', imp[:], imp[:], addm[i % 2][:], ALU.add)
                fw.I('dve', 'max', out=t8[:, 0:8], in_=imp[:])
                fw.I('dve', 'match_replace', out=mr[:], in_to_replace=t8[:, 0:8], in_values=imp[:], imm_value=-2e30)
                fw.I('dve', 'max', out=t8[:, 8:16], in_=mr[:])
                fw.ts('dve', t8[:, 16:17], t8[:, 15:16], -1e29, ALU.max)
                fw.ts('dve', selm[:], imp[:], t8[:, 16:17], ALU.is_ge)
                fw.mm(pT[0:64, 0, :], selm[:], C['ident_b'])
                fw.cp('act', selT[:], pT[0:64, 0, :])
                if DBG_C < 4: continue
                for kt in range(i + 1):
                    P = pS[ei % 2]; ei += 1
                    fw.mm(P[:], ksT[:, g, kt * 128:(kt + 1) * 128], Q2)
                    fw.mm(pM[:, 0:128], selx[:, kt, :], selT[:])
                    fw.act(Eb[:, kt].rearrange("p r q -> p (r q)"), P[:], AF.Exp, part=True)
                    for r_ in range(4):
                        fw.tt('dve', Eb[:, kt, r_, :], Eb[:, kt, r_, :], pM[:, 0:128], ALU.mult, part=True)
                        if kt == i:
                            fw.tt('dve', Eb[:, kt, r_, :], Eb[:, kt, r_, :], cauT[:], ALU.mult, part=True)
                for r_ in range(4):
                    for kt in range(i + 1):
                        fw.mm(pOs[:, r_, :], Eb[:, kt, r_, :], Vs[:, kt, g, :], start=(kt == 0), stop=(kt == i))
                if DBG_C < 5: continue
                k0 = max(0, i - 4)
                for kt in range(k0, i + 1):
                    P = pS[ei % 2]; ei += 1
                    sl_ = kt - k0
                    fw.mm(P[:], kwT[:, g, kt * 128:(kt + 1) * 128], Q2)
                    fw.act(Ew[:, sl_].rearrange("p r q -> p (r q)"), P[:], AF.Exp, part=True)
                    msk = cauT if kt == i else (acauT if kt == i - 4 else None)
                    if msk is not None:
                        for r_ in range(4):
                            fw.tt('dve', Ew[:, sl_, r_, :], Ew[:, sl_, r_, :], msk[:], ALU.mult, part=True)
                for r_ in range(4):
                    for kt in range(k0, i + 1):
                        fw.mm(pOw[:, r_, :], Ew[:, kt - k0, r_, :], Vw[:, kt, g, :], start=(kt == k0), stop=(kt == i))
                if DBG_C < 6: continue
                for r_ in range(4):
                    fw.ts('dve', rsd[:, 8 + r_:9 + r_], pOs[:, r_, 64:65], 1e-30, ALU.max, part=True)
                    fw.ts('dve', rsd[:, 12 + r_:13 + r_], pOw[:, r_, 64:65], 1e-30, ALU.max, part=True)
                fw.I('dve', 'reciprocal', out=coef[:, 4:12], in_=rsd[:, 8:16])
                fw.cp('dve', coef[:, 0:4], rsd[:, 4:8])
                for r_ in range(4):
                    h = g * 4 + r_
                    for b_ in range(3):
                        fw.tt('dve', coef[:, b_ * 4 + r_:b_ * 4 + r_ + 1], coef[:, b_ * 4 + r_:b_ * 4 + r_ + 1], gat[:, h * 3 + b_:h * 3 + b_ + 1], ALU.mult, part=True)
                for r_ in range(4):
                    h = g * 4 + r_
                    ys = y[:, h * 64:(h + 1) * 64]
                    fw.ts('dve', ys, pOc[r_ // 2][:, r_ % 2, 0:64], coef[:, r_:r_ + 1], ALU.mult, part=True)
                    fw.I('dve', 'scalar_tensor_tensor', part=True, out=ys, in0=pOs[:, r_, 0:64], scalar=coef[:, 4 + r_:5 + r_], in1=ys, op0=ALU.mult, op1=ALU.add)
                    fw.I('dve', 'scalar_tensor_tensor', part=True, out=ys, in0=pOw[:, r_, 0:64], scalar=coef[:, 8 + r_:9 + r_], in1=ys, op0=ALU.mult, op1=ALU.add)
            fw.dma('sp', ynsa_d[t0:t0 + 128, :], y[:])
    fw.barrier()


DBG_D = 9
NE = 32


def conv_weights(fw, w1_d, w2_d, w1b_d, w2b_d):
    for e in range(NE):
        for i in range(4):
            fw.dma('pool', w1b_d[e][i * 512:(i + 1) * 512, :].rearrange("(p a) c -> p (a c)", p=128),
                   w1_d[e, i * 512:(i + 1) * 512, :].rearrange("(p a) c -> p (a c)", p=128), part=True)
            yield
        for i in range(2):
            fw.dma('pool', w2b_d[e][i * 1024:(i + 1) * 1024, :].rearrange("(p a) c -> p (a c)", p=128),
                   w2_d[e, i * 1024:(i + 1) * 1024, :].rearrange("(p a) c -> p (a c)", p=128), part=True)
            yield


def stage_D(fw, C, yrw_d, ynsa_d, sel_d, xown_d, wout_d, mg_d, rw_d, rb_d, h1_d, xn2T_d, gates_d, gatesT_d):
    with ExitStack() as es:
        wo = fw.sb('D_wo', [128, 16, 2048], BF16, es)
        Y0 = [fw.sb('D_Y0%d' % i, [128, 2048], F32, es) for i in range(2)]
        Y1 = [fw.sb('D_Y1%d' % i, [128, 2048], F32, es) for i in range(2)]
        XO = [fw.sb('D_XO%d' % i, [128, 2048], F32, es) for i in range(2)]
        Yf = fw.sb('D_Yf', [128, 2048], F32, es)
        Yb = fw.sb('D_Yb', [128, 2048], BF16, es)
        yT = fw.sb('D_yT', [128, 16, 128], BF16, es)
        H = fw.sb('D_H', [128, 2048], F32, es)
        XN = fw.sb('D_XN', [128, 2048], F32, es)
        junk = fw.sb('D_junk', [128, 2048], BF16, es)
        xTh = fw.sb('D_xTh', [128, 16, 128], BF16, es)
        xTl = fw.sb('D_xTl', [128, 16, 128], BF16, es)
        XH = fw.sb('D_XH', [128, 2048], BF16, es)
        XL = fw.sb('D_XL', [128, 2048], BF16, es)
        rwh = fw.sb('D_rwh', [128, 16, 32], BF16, es)
        rwl = fw.sb('D_rwl', [128, 16, 32], BF16, es)
        xTb = fw.sb('D_xTb', [128, 16, 128], BF16, es)
        ss = fw.sb('D_ss', [128, 4], F32, es)
        sel = fw.sb('D_sel', [128, 2], F32, es)
        mg = fw.sb('D_mg', [128, 16], F32, es)
        rwg = fw.sb('D_rwg', [128, 16, 32], F32, es)
        rb = fw.sb('D_rb', [128, 32], F32, es)
        lg = fw.sb('D_lg', [128, 32], F32, es)
        ex = fw.sb('D_ex', [128, 32], F32, es)
        mk = fw.sb('D_mk', [128, 32], F32, es)
        t8 = fw.sb('D_t8', [128, 16], F32, es)
        gt_sb = fw.sb('D_gT', [32, 128], F32, es)
        pT = [fw.ps('D_pT%d' % i, [128, 8, 128], BF16, es) for i in range(2)]
        pM = [fw.ps('D_pM%d' % i, [128, 512], F32, es) for i in range(3)]
        pR = fw.ps('D_pR', [128, 512], F32, es)
        for i in range(4):
            fw.dma('pool', wo[:, :, i * 512:(i + 1) * 512], wout_d[:, i * 512:(i + 1) * 512].rearrange("(k p) c -> p k c", p=128), part=True)
        fw.dma('sp', sel[:], sel_d[:]); fw.dma('sp', mg[:], mg_d[:])
        fw.dma('sp', rwg[:], rw_d[:, :].rearrange("(k p) e -> p k e", p=128))
        fw.dma('sp', rb[:], rb_d[:, :].partition_broadcast(128))
        for k in range(16):
            fw.ts('dve', rwg[:, k, :], rwg[:, k, :], mg[:, k:k + 1], ALU.mult, part=True)
        fw.cp('dve', rwh[:], rwg[:])
        fw.tt('dve', rwl[:], rwg[:], rwh[:], ALU.subtract)
        for tt in range(16):
            t0 = tt * 128
            y0 = Y0[tt % 2]; y1 = Y1[tt % 2]; xo = XO[tt % 2]
            fw.dma('sp', y0[:, 0:1024], yrw_d[t0:t0 + 128, :], part=True)
            fw.dma('sp', y0[:, 1024:2048], ynsa_d[t0:t0 + 128, :], part=True)
            fw.dma('sp', y1[:, 0:1024], yrw_d[2048 + t0:2048 + t0 + 128, :], part=True)
            fw.dma('sp', y1[:, 1024:2048], ynsa_d[2048 + t0:2048 + t0 + 128, :], part=True)
            fw.dma('sp', xo[:], xown_d[t0:t0 + 128, :])
            fw.ts('pool', Yf[:], y0[:], sel[:, 0:1], ALU.mult)
            fw.I('dve', 'scalar_tensor_tensor', out=Yb[:], in0=y1[:], scalar=sel[:, 1:2], in1=Yf[:], op0=ALU.mult, op1=ALU.add)
            for hh in range(2):
                P = pT[hh]
                for j in range(8):
                    k = hh * 8 + j
                    fw.tr(P[:, j, :], Yb[:, k * 128:(k + 1) * 128], C['ident_b'])
                fw.cp('act' if hh else 'dve', yT[:, hh * 8:(hh + 1) * 8, :], P[:], part=True)
            for cb in range(4):
                P = pM[cb % 3]
                for k in range(16):
                    fw.mm(P[:], yT[:, k, :], wo[:, k, cb * 512:(cb + 1) * 512], start=(k == 0), stop=(k == 15))
                fw.tt('dve', H[:, cb * 512:(cb + 1) * 512], P[:], xo[:, cb * 512:(cb + 1) * 512], ALU.add, part=True)
            fw.dma('sp', h1_d[t0:t0 + 128, :], H[:])
            if DBG_D < 2: continue
            rms_rstd(fw, H[:], ss, junk[:])
            fw.ts('pool', XN[:], H[:], ss[:, 2:3], ALU.mult)
            fw.cp('dve', XH[:], XN[:])
            fw.tt('dve', XL[:], XN[:], XH[:], ALU.subtract)
            for (src, dstT, scaled) in ((XH, xTh, True), (XL, xTl, False)):
                for hh in range(2):
                    P = pT[hh]
                    for j in range(8):
                        k = hh * 8 + j
                        fw.tr(P[:, j, :], src[:, k * 128:(k + 1) * 128], C['ident_b'])
                    fw.cp('act', dstT[:, hh * 8:(hh + 1) * 8, :], P[:], part=True)
                    if scaled:
                        for j in range(8):
                            k = hh * 8 + j
                            fw.ts('dve', xTb[:, k, :], P[:, j, :], mg[:, k:k + 1], ALU.mult, part=True)
            fw.dma('sp', xn2T_d[:, :, t0:t0 + 128], xTb[:])
            if DBG_D < 3: continue
            n = 0
            for (a_, w_) in ((xTh, rwh), (xTl, rwh), (xTh, rwl)):
                for k in range(16):
                    fw.mm(pR[:, 0:32], a_[:, k, :], w_[:, k, :], start=(n == 0), stop=(n == 47)); n += 1
            fw.tt('dve', lg[:], pR[:, 0:32], rb[:], ALU.add)
            fw.I('dve', 'max', out=t8[:, 0:8], in_=lg[:])
            fw.ts('dve', mk[:], lg[:], t8[:, 3:4], ALU.is_ge)
            fw.ts('dve', t8[:, 8:9], t8[:, 0:1], -1.0, ALU.mult)
            fw.act(ex[:], lg[:], AF.Exp, bias=t8[:, 8:9])
            fw.tt('dve', ex[:], ex[:], mk[:], ALU.mult)
            fw.I('dve', 'tensor_reduce', out=t8[:, 9:10], in_=ex[:], axis=AX.X, op=ALU.add)
            fw.I('dve', 'reciprocal', out=t8[:, 10:11], in_=t8[:, 9:10])
            fw.ts('dve', lg[:], ex[:], t8[:, 10:11], ALU.mult)
            fw.dma('sp', gates_d[t0:t0 + 128, :], lg[:])
    fw.barrier()


def stage_E(fw, C, w1b_d, w2b_d, b1_d, b2_d, h1_d, xn2T_d, gates_d, gatesT_d, h2_d):
    TP = 512; NT = TP // 128
    with ExitStack() as es:
        xT = fw.sb('E_xT', [128, 16, TP], BF16, es)
        acc = fw.sb('E_acc', [128, NT, 2048], F32, es)
        actT = fw.sb('E_actT', [128, 16, TP], BF16, es)
        W1 = [fw.sb('E_W1%d' % i, [128, 16, 512], BF16, es) for i in range(2)]
        W2 = [fw.sb('E_W2%d' % i, [128, 16, 512], BF16, es) for i in range(2)]
        b1 = fw.sb('E_b1', [128, 32, 32], F32, es)
        b2bc = [fw.sb('E_b2bc0', [128, 2048], F32, es)] * 2
        gat = fw.sb('E_gat', [128, NT, 32], F32, es)
        h1t = [fw.sb('E_h10', [128, 2048], F32, es)] * 2
        tg = [fw.sb('E_tg%d' % i, [128, 512], F32, es) for i in range(2)]
        tsg = [fw.sb('E_ts%d' % i, [128, 512], F32, es) for i in range(2)]
        tl = [fw.sb('E_tl%d' % i, [128, 512], F32, es) for i in range(2)]
        tl2 = [fw.sb('E_tm%d' % i, [128, 512], F32, es) for i in range(2)]
        pG = [fw.ps('E_pG%d' % i, [128, 512], F32, es) for i in range(2)]
        pL = [fw.ps('E_pL%d' % i, [128, 512], F32, es) for i in range(2)]
        pY = [fw.ps('E_pY%d' % i, [128, 512], F32, es) for i in range(4)]
        fw.dma('sp', b1[:], b1_d[:])
        w1i = 0; w2i = 0; si = 0; yi = 0
        for tp in range(2048 // TP):
            T0 = tp * TP
            fw.dma('sp', xT[:], xn2T_d[:, :, T0:T0 + TP])
            fw.dma('sp', gat[:], gates_d[T0:T0 + TP, :].rearrange("(n p) e -> p n e", p=128))
            fw.I('pool', 'memset', ap=acc[:], constant=0.0)
            for e in range(NE):
                for hb in range(8):
                    W = W1[w1i % 2]; w1i += 1
                    q = 'sp' if w1i % 2 else 'act'
                    fw.dma(q, W[:, :, 0:256], w1b_d[e][:, hb * 256:(hb + 1) * 256].rearrange("(k p) c -> p k c", p=128), part=True)
                    fw.dma(q, W[:, :, 256:512], w1b_d[e][:, 2048 + hb * 256:2048 + (hb + 1) * 256].rearrange("(k p) c -> p k c", p=128), part=True)
                    for sub in range(2):
                        G = pG[si % 2]; L = pL[si % 2]
                        g_ = tg[si % 2]; s_ = tsg[si % 2]; l_ = tl[si % 2]; m_ = tl2[si % 2]; si += 1
                        for k in range(16):
                            fw.mm(G[:], W[:, k, sub * 128:(sub + 1) * 128], xT[:, k, :], start=(k == 0), stop=(k == 15))
                        for k in range(16):
                            fw.mm(L[:], W[:, k, 256 + sub * 128:256 + (sub + 1) * 128], xT[:, k, :], start=(k == 0), stop=(k == 15))
                        cg = hb * 2 + sub
                        fw.ts('dve', g_[:], G[:], b1[:, e, cg:cg + 1], ALU.add, 7.0, ALU.min)
                        fw.act(s_[:], g_[:], AF.Sigmoid, scale=1.702)
                        fw.ts('dve', l_[:], L[:], b1[:, e, 16 + cg:16 + cg + 1], ALU.add, 7.0, ALU.min)
                        fw.ts('pool', m_[:], l_[:], -7.0, ALU.max, 1.0, ALU.add)
                        fw.tt('pool', s_[:], s_[:], g_[:], ALU.mult)
                        fw.tt('dve', actT[:, cg, :], s_[:], m_[:], ALU.mult, part=True)
                bb = b2bc[e % 2]
                fw.dma('sp', bb[:], b2_d[e:e + 1, :].partition_broadcast(128))
                for tl_ in range(NT):
                    fw.I('dve', 'scalar_tensor_tensor', part=True, out=acc[:, tl_, :], in0=bb[:], scalar=gat[:, tl_, e:e + 1], in1=acc[:, tl_, :], op0=ALU.mult, op1=ALU.add)
                for fb in range(4):
                    W = W2[w2i % 2]; w2i += 1
                    q = 'sp' if w2i % 2 else 'act'
                    fw.dma(q, W[:], w2b_d[e][:, fb * 512:(fb + 1) * 512].rearrange("(k p) c -> p k c", p=128))
                    for tl_ in range(NT):
                        P = pY[yi % 4]; yi += 1
                        for k in range(16):
                            fw.mm(P[:], actT[:, k, tl_ * 128:(tl_ + 1) * 128], W[:, k, :], start=(k == 0), stop=(k == 15))
                        fw.I('dve', 'scalar_tensor_tensor', part=True, out=acc[:, tl_, fb * 512:(fb + 1) * 512], in0=P[:],
                             scalar=gat[:, tl_, e:e + 1], in1=acc[:, tl_, fb * 512:(fb + 1) * 512], op0=ALU.mult, op1=ALU.add)
            for tl_ in range(NT):
                h = h1t[tl_ % 2]
                fw.dma('sp', h[:], h1_d[T0 + tl_ * 128:T0 + (tl_ + 1) * 128, :])
                fw.tt('pool', h[:], h[:], acc[:, tl_, :], ALU.add)
                fw.dma('sp', h2_d[T0 + tl_ * 128:T0 + (tl_ + 1) * 128, :], h[:])
    fw.barrier()


def stage_F(fw, C, h2_d, pown_d, pg_d, plw_d, pgw_d, out_d):
    with ExitStack() as es:
        pgw = fw.sb('F_pgw', [128, 16, 2048], BF16, es)
        plw = fw.sb('F_plw', [128, 2, 2048], BF16, es)
        Hh = [fw.sb('F_H%d' % i, [128, 2048], F32, es) for i in range(2)]
        Pp = [fw.sb('F_P%d' % i, [128, 256], F32, es) for i in range(2)]
        Pb = fw.sb('F_Pb', [128, 256], BF16, es)
        XB = fw.sb('F_XB', [128, 2048], BF16, es)
        junk = fw.sb('F_junk', [128, 2048], BF16, es)
        xT = fw.sb('F_xT', [128, 16, 128], BF16, es)
        ppT = fw.sb('F_ppT', [128, 2, 128], BF16, es)
        gsb = [fw.sb('F_g%d' % i, [128, 512], F32, es) for i in range(2)]
        O = [fw.sb('F_O%d' % i, [128, 2048], F32, es) for i in range(2)]
        ss = fw.sb('F_ss', [128, 4], F32, es)
        pg = fw.sb('F_pg', [128, 16], F32, es)
        pT = [fw.ps('F_pT%d' % i, [128, 8, 128], BF16, es) for i in range(2)]
        pA = [fw.ps('F_pA%d' % i, [128, 512], F32, es) for i in range(2)]
        pB = [fw.ps('F_pB%d' % i, [128, 512], F32, es) for i in range(2)]
        pQ = fw.ps('F_pQ', [128, 2, 128], BF16, es)
        for i in range(4):
            fw.dma('pool', pgw[:, :, i * 512:(i + 1) * 512], pgw_d[:, i * 512:(i + 1) * 512].rearrange("(k p) c -> p k c", p=128), part=True)
        fw.dma('pool', plw[:], plw_d[:, :].rearrange("(k p) c -> p k c", p=128))
        fw.dma('sp', pg[:], pg_d[:])
        ci = 0
        for tt in range(16):
            t0 = tt * 128
            h = Hh[tt % 2]; pp = Pp[tt % 2]; o = O[tt % 2]
            fw.dma('sp', h[:], h2_d[t0:t0 + 128, :])
            fw.dma('sp', pp[:], pown_d[t0:t0 + 128, :])
            rms_rstd(fw, h[:], ss, junk[:])
            fw.ts('dve', XB[:], h[:], ss[:, 2:3], ALU.mult)
            fw.cp('dve', Pb[:], pp[:])
            for hh in range(2):
                P = pT[hh]
                for j in range(8):
                    k = hh * 8 + j
                    fw.tr(P[:, j, :], XB[:, k * 128:(k + 1) * 128], C['ident_b'])
                for j in range(8):
                    k = hh * 8 + j
                    if j % 2: fw.act(xT[:, k, :], P[:, j, :], AF.Copy, scale=pg[:, k:k + 1], part=True)
                    else: fw.ts('dve', xT[:, k, :], P[:, j, :], pg[:, k:k + 1], ALU.mult, part=True)
            for j in range(2):
                fw.tr(pQ[:, j, :], Pb[:, j * 128:(j + 1) * 128], C['ident_b'])
            fw.cp('act', ppT[:], pQ[:])
            for cb in range(4):
                A_ = pA[ci % 2]; B_ = pB[ci % 2]; g_ = gsb[ci % 2]; ci += 1
                cs = slice(cb * 512, (cb + 1) * 512)
                for k in range(16):
                    fw.mm(A_[:], xT[:, k, :], pgw[:, k, cs], start=(k == 0), stop=(k == 15))
                for k in range(2):
                    fw.mm(B_[:], ppT[:, k, :], plw[:, k, cs], start=(k == 0), stop=(k == 1))
                fw.act(g_[:], A_[:], AF.Sigmoid)
                fw.tt('dve', g_[:], g_[:], B_[:], ALU.mult)
                fw.tt('pool', o[:, cs], g_[:], h[:, cs], ALU.add, part=True)
            fw.dma('sp', out_d[t0:t0 + 128, :], o[:])
    fw.barrier()


BFNP = ml_dtypes.bfloat16


def _consts():
    c = {}
    c['ident_b'] = np.eye(128).astype(BFNP)
    c['ident_f'] = np.eye(128, dtype=np.float32)
    c['ones'] = np.ones((128, 128), np.float32)
    return c


def _rw_consts():
    r = np.arange(64)[:, None]; s = np.arange(64)[None, :]
    rep = lambda m: np.tile(m.astype(np.float32), (1, 8))
    return {'c_triu': (r <= s).astype(np.float32), 'c_mus': rep(r < s), 'c_mls': rep(r > s), 'c_mui': rep(r <= s), 'c_eye8': rep(r == s)}


def _rw_params(I, hg):
    sl = slice(hg * 512, (hg + 1) * 512)
    mu = I['rw_mu'][0]
    p = {}
    p['mu'] = np.concatenate([mu[0:1024][sl], mu[1024:2048][sl], mu[2048:3072][sl], mu[3072:3360]])[None, :]
    p['w0'] = I['rw_w0'][0][sl][None]; p['a0'] = I['rw_a0'][0][sl][None]; p['kkp'] = I['rw_k_k'][0][sl][None]
    p['kap'] = I['rw_k_a'][0][sl][None]; p['rkp'] = I['rw_r_k'][0].reshape(-1)[sl][None]
    p['lnw'] = I['rw_lnx_w'][0][sl][None]; p['lnb'] = I['rw_lnx_b'][0][sl][None]
    p['w2'] = I['rw_w2'][0][:, sl]; p['a2'] = I['rw_a2'][0][:, sl]; p['g2'] = I['rw_g2'][0][:, sl]
    return {k: np.ascontiguousarray(v, dtype=np.float32) for k, v in p.items()}


def _nsa_consts():
    c = {}
    i = np.arange(32)[:, None, None, None]; nl = np.arange(128)[None, :, None, None]; cc = np.arange(2)[None, None, :, None]; q = np.arange(128)[None, None, None, :]
    c['cmask'] = ((16 * (128 * cc + nl) + 31) <= (128 * i + q)).astype(BFNP)
    i = np.arange(32)[:, None, None]; q = np.arange(128)[None, :, None]; j = np.arange(64)[None, None, :]
    qblk = (128 * i + q) // 64
    forced = (j == 0) | (j == qblk) | (j == qblk - 1); fut = j > qblk
    c['keep'] = (~(forced | fut)).astype(np.float32)
    c['addm'] = np.where(fut, -1e30, np.where(forced, 1e30, 0.0)).astype(np.float32)
    j = np.arange(64)[:, None, None]; kt = np.arange(32)[None, :, None]; k = np.arange(128)[None, None, :]
    c['selx'] = (j == 2 * kt + k // 64).astype(BFNP)
    kl = np.arange(128)[:, None]; ql = np.arange(128)[None, :]
    c['cauT'] = (kl <= ql).astype(BFNP); c['acauT'] = (kl > ql).astype(BFNP)
    n = np.arange(256)[:, None]; j = np.arange(64)[None, :]
    ov = ((16 * n < 64 * j + 64) & (16 * n + 32 > 64 * j) & (n < 255)).astype(BFNP)
    c['ovl'] = np.ascontiguousarray(np.tile(ov[:, None, :], (1, 4, 1)))
    return c


def _nsa_params(I):
    p = {}
    p['qnw'] = np.tile(I['nsa_q_norm'][0], 16)[None, :]
    kn = I['nsa_k_norm'][0]
    p['knw'] = np.concatenate([np.tile(kn[1], 4), np.tile(kn[2], 4)])[None, :]
    p['kn0'] = kn[0][:, None]
    p['b2k'] = I['cmp_k_b2'][0][:, None]; p['b2v'] = I['cmp_v_b2'][0][None, :]
    for kv, nm in (('k', 'cmp_k'), ('v', 'cmp_v')):
        p['w1' + kv] = I[nm + '_w1'][0]; p['w2' + kv] = I[nm + '_w2'][0]
        p['b1' + kv] = I[nm + '_b1'][0].reshape(2, 128).T
    p['posTk'] = I['cmp_pos_k'][0].T; p['posTv'] = I['cmp_pos_v'][0].T
    return {k: np.ascontiguousarray(v, dtype=np.float32) for k, v in p.items()}


def _shared_inputs(I):
    pa = lambda v: np.ascontiguousarray(v.reshape(16, 128).T, dtype=np.float32)
    m = {}
    for k, v in _consts().items(): m['c_' + k] = v
    rc = _rw_consts()
    for hg in range(2):
        for k, v in _rw_params(I, hg).items(): m['rw%d_%s' % (hg, k)] = v
        for k, v in rc.items(): m['rw%d_%s' % (hg, k)] = v
    for k, v in _nsa_consts().items(): m['n_' + k] = v
    for k, v in _nsa_params(I).items(): m['n_' + k] = v
    m['w_in'] = np.ascontiguousarray(I['w_in'][0]); m['mix_g'] = pa(I['mix_norm_g'][0])
    m['w_out'] = np.ascontiguousarray(I['w_out'][0]); m['moe_g'] = pa(I['moe_norm_g'][0])
    m['router_w'] = np.ascontiguousarray(I['router_w'][0]); m['router_b'] = np.ascontiguousarray(I['router_b'].reshape(1, 32))
    m['moe_w1'] = np.ascontiguousarray(I['moe_w1'][0]); m['moe_w2'] = np.ascontiguousarray(I['moe_w2'][0])
    m['moe_b1'] = np.ascontiguousarray(I['moe_b1'][0].reshape(32, 32, 128).transpose(2, 0, 1)); m['moe_b2'] = np.ascontiguousarray(I['moe_b2'][0])
    m['ple_g'] = pa(I['ple_norm_g'][0]); m['ple_w'] = np.ascontiguousarray(I['ple_w'][0]); m['ple_gate_w'] = np.ascontiguousarray(I['ple_gate_w'][0])
    return m


def build_program(shared):
    nc = bass.Bass("TRN2", target_bir_lowering=False)
    with ExitStack() as es:
        fw = FW(nc, es)
        def EI(n, shape=None, dt=None):
            v = shared.get(n)
            if shape is None: shape = list(v.shape)
            if dt is None: dt = BF16 if (v is not None and v.dtype == BFNP) else F32
            return fw.dram(n, shape, dt, kind="ExternalInput")
        x_d = EI("x_full", [4096, 2048], F32); xown_d = EI("x_own", [2048, 2048], F32)
        pown_d = EI("p_own", [2048, 256], F32); sel_d = EI("sel", [128, 2], F32)
        D_ = {k: EI(k) for k in shared}
        out_d = fw.dram("out", [2048, 2048], F32, kind="ExternalOutput")
        u_d = fw.dram("u_d", [4096, IN_W], F32)
        yrw_d = fw.dram("yrw_d", [4096, 1024], F32); ynsa_d = fw.dram("ynsa_d", [4096, 1024], F32)
        h1_d = fw.dram("h1_d", [2048, 2048], F32); h2_d = fw.dram("h2_d", [2048, 2048], F32)
        gates_d = fw.dram("gates_d", [2048, 32], F32)
        xn2T_d = fw.dram("xn2T_d", [128, 16, 2048], BF16)
        w1b_d = [fw.dram("w1b_%d" % e, [2048, 4096], BF16) for e in range(32)]
        w2b_d = [fw.dram("w2b_%d" % e, [2048, 2048], BF16) for e in range(32)]
        C = {}
        for k, dt in (('ident_b', BF16), ('ident_f', F32), ('ones', F32)):
            t = fw.sb('k_' + k, [128, 128], dt); fw.dma('sp', t[:], D_['c_' + k][:, :]); C[k] = t[:]
        stage_A(fw, C, x_d, D_['w_in'], D_['mix_g'], u_d)
        conv = conv_weights(fw, D_['moe_w1'], D_['moe_w2'], w1b_d, w2b_d)
        for hg in range(2):
            P_ = {k[4:]: v for k, v in D_.items() if k.startswith('rw%d_' % hg)}
            stage_B(fw, C, u_d, P_, yrw_d, hg, conv if hg == 0 else None)
        for _ in conv: pass
        Pn = {k[2:]: v for k, v in D_.items() if k.startswith('n_')}
        stage_C(fw, C, u_d, Pn, ynsa_d)
        stage_D(fw, C, yrw_d, ynsa_d, sel_d, xown_d, D_['w_out'], D_['moe_g'], D_['router_w'], D_['router_b'], h1_d, xn2T_d, gates_d, None)
        stage_E(fw, C, w1b_d, w2b_d, D_['moe_b1'], D_['moe_b2'], h1_d, xn2T_d, gates_d, None, h2_d)
        stage_F(fw, C, h2_d, pown_d, D_['ple_g'], D_['ple_w'], D_['ple_gate_w'], out_d)
        fw.emit()
    return nc


def kernel(**inputs):
    I = {k: np.asarray(v) for k, v in inputs.items()}
    shared = _shared_inputs(I)
    nc = build_program(shared)
    x = I['x']; p = I['p']
    in_maps = []
    for c in range(8):
        b, s = c // 2, c % 2
        m = dict(shared)
        m['x_full'] = np.ascontiguousarray(x[b], dtype=np.float32)
        m['x_own'] = np.ascontiguousarray(x[b, s * 2048:(s + 1) * 2048], dtype=np.float32)
        m['p_own'] = np.ascontiguousarray(p[0, b, s * 2048:(s + 1) * 2048], dtype=np.float32)
        sel = np.zeros((128, 2), np.float32); sel[:, s] = 1.0
        m['sel'] = sel
        in_maps.append(m)
    res = run_bass_kernel_spmd(nc, in_maps, core_ids=list(range(8)))
    out = np.empty((4, 4096, 2048), np.float32)
    for c in range(8):
        b, s = c // 2, c % 2
        out[b, s * 2048:(s + 1) * 2048] = np.asarray(res.results[c]['out'], dtype=np.float32)
    return out
```

```python
from contextlib import ExitStack
import numpy as np
import ml_dtypes
import concourse.bass as bass
import concourse.mybir as mybir
from concourse.bass_utils import run_bass_kernel_spmd

F32 = mybir.dt.float32
BF16 = mybir.dt.bfloat16
ALU = mybir.AluOpType
AF = mybir.ActivationFunctionType
AX = mybir.AxisListType

COMPUTE = ('pe', 'act', 'dve', 'pool')
QUEUES = ('sp', 'act', 'pool')
NDSEM = 20
WRITE_KEYS = ('out', 'ap', 'accum_out')


class T:
    def __init__(self, h, name):
        self.h = h; self.name = name
        self.lw = []; self.rd = []; self.prd = []

    def __getitem__(self, k):
        return A(self.h[k], self)


class A:
    def __init__(self, ap, t):
        self.ap = ap; self.t = t

    def __getitem__(self, k): return A(self.ap[k], self.t)
    def unsqueeze(self, i): return A(self.ap.unsqueeze(i), self.t)
    def to_broadcast(self, s): return A(self.ap.to_broadcast(list(s)), self.t)
    def rearrange(self, pat, **kw): return A(self.ap.rearrange(pat, **kw), self.t)
    def partition_broadcast(self, n): return A(self.ap.partition_broadcast(n), self.t)
    def bc(self, s): return A(self.ap.to_broadcast(list(s)), self.t)


class FW:
    def __init__(self, nc, es):
        self.nc = nc; self.es = es
        self.eng = {'pe': nc.tensor, 'act': nc.scalar, 'dve': nc.vector, 'pool': nc.gpsimd, 'sp': nc.sync}
        self.prog = {e: [] for e in self.eng}
        self.sem = {}; self.cnt = {}
        for e in COMPUTE:
            self.sem[e] = es.enter_context(nc.semaphore("s_" + e)); self.cnt[e] = 0
        self.dsem = {}; self.dcnt = {}; self.dnext = {}
        for q in QUEUES:
            self.dsem[q] = [es.enter_context(nc.semaphore("d_%s_%d" % (q, i))) for i in range(NDSEM)]
            self.dcnt[q] = [0] * NDSEM; self.dnext[q] = 0
        self.waited = {e: {} for e in self.eng}
        self.tiles = []; self.n_inst = 0

    def sb(self, name, shape, dtype=F32, es=None):
        h = (es or self.es).enter_context(self.nc.sbuf_tensor(name, list(shape), dtype))
        t = T(h, name); self.tiles.append(t); return t

    def ps(self, name, shape, dtype=F32, es=None):
        h = (es or self.es).enter_context(self.nc.psum_tensor(name, list(shape), dtype))
        t = T(h, name); self.tiles.append(t); return t

    def dram(self, name, shape, dtype=F32, kind="Internal"):
        h = self.nc.dram_tensor(name, list(shape), dtype, kind=kind).ap()
        t = T(h, name); self.tiles.append(t); return t

    def _collect(self, e, reads, writes, dma=False):
        own = None if dma else self.sem.get(e)
        waits = {}
        def add(tok, raw):
            s, v = tok
            if s is own and not raw: return
            k = id(s)
            if self.waited[e].get(k, 0) >= v: return
            if k not in waits or waits[k][1] < v: waits[k] = (s, v)
        for t in reads:
            for tok in t.lw: add(tok, True)
        for t in writes:
            for tok in t.rd: add(tok, False)
            for tok in t.prd: add(tok, False)
            for tok in t.lw: add(tok, False)
        return list(waits.values())

    @staticmethod
    def _compact(toks):
        best = {}
        for s, v in toks:
            k = id(s)
            if k not in best or best[k][1] < v: best[k] = (s, v)
        return list(best.values())

    def _update(self, tok, reads, writes, part):
        for t in reads:
            t.rd.append(tok)
            if len(t.rd) > 16: t.rd = self._compact(t.rd)
        for t in writes:
            if part and not t.rd:
                t.lw.append(tok)
                if len(t.lw) > 16: t.lw = self._compact(t.lw)
            else:
                t.prd = t.rd; t.rd = []; t.lw = [tok]

    def op(self, e, fn, reads=(), writes=(), part=False):
        waits = self._collect(e, reads, writes)
        for s, v in waits: self.waited[e][id(s)] = v
        sem = self.sem[e]; self.cnt[e] += 1
        tok = (sem, self.cnt[e])
        self.prog[e].append((waits, fn, sem, 1))
        self._update(tok, reads, writes, part)
        self.n_inst += 1
        return tok

    def I(self, e, meth, part=False, **kw):
        reads = []; writes = []; args = {}
        for k, v in kw.items():
            if isinstance(v, A):
                args[k] = v.ap
                if k in WRITE_KEYS:
                    writes.append(v.t)
                    if k == 'accum_out': reads.append(v.t)
                else:
                    reads.append(v.t)
            else:
                args[k] = v
        fn = lambda eng, m=meth, a=args: getattr(eng, m)(**a)
        return self.op(e, fn, reads, writes, part)

    def dma(self, q, out, in_, part=False, **kw):
        reads = [in_.t]; writes = [out.t]
        waits = self._collect(q, reads, writes, dma=True)
        i = self.dnext[q]; self.dnext[q] = (i + 1) % NDSEM
        s = self.dsem[q][i]
        if self.dcnt[q][i] > self.waited[q].get(id(s), 0):
            waits.append((s, self.dcnt[q][i]))
        for ss, v in waits:
            self.waited[q][id(ss)] = max(self.waited[q].get(id(ss), 0), v)
        self.dcnt[q][i] += 16
        tok = (s, self.dcnt[q][i])
        fn = lambda eng, o=out.ap, a=in_.ap, kw=kw: eng.dma_start(out=o, in_=a, **kw)
        self.prog[q].append((waits, fn, s, 16))
        self._update(tok, reads, writes, part)
        self.n_inst += 1
        return tok

    def barrier(self):
        targets = []
        for e in COMPUTE:
            if self.cnt[e] > 0: targets.append((self.sem[e], self.cnt[e]))
        for q in QUEUES:
            for i in range(NDSEM):
                if self.dcnt[q][i] > 0: targets.append((self.dsem[q][i], self.dcnt[q][i]))
        for e in self.eng:
            w = []
            for s, v in targets:
                if s is self.sem.get(e): continue
                if self.waited[e].get(id(s), 0) < v:
                    w.append((s, v)); self.waited[e][id(s)] = v
            if w: self.prog[e].append((w, None, None, 0))
        for t in self.tiles:
            t.lw = []; t.rd = []; t.prd = []

    def emit(self):
        with self.nc.Block() as block:
            def run(name):
                def body(eng):
                    for waits, fn, sem, inc in self.prog[name]:
                        for s, v in waits: eng.wait_ge(s, v)
                        if fn is not None: fn(eng).then_inc(sem, inc)
                return body
            block.tensor(run('pe')); block.scalar(run('act')); block.vector(run('dve'))
            block.gpsimd(run('pool')); block.sync(run('sp'))

    def mm(self, out, lhsT, rhs, start=True, stop=True):
        return self.I('pe', 'matmul', part=True, out=out, lhsT=lhsT, rhs=rhs, start=start, stop=stop)

    def tr(self, out, in_, identity):
        return self.I('pe', 'transpose', part=True, out=out, in_=in_, identity=identity)

    def tt(self, e, out, in0, in1, op, part=False):
        return self.I(e, 'tensor_tensor', part=part, out=out, in0=in0, in1=in1, op=op)

    def ts(self, e, out, in0, s1, op0, s2=None, op1=None, part=False):
        if op1 is None:
            return self.I(e, 'tensor_scalar', part=part, out=out, in0=in0, scalar1=s1, scalar2=None, op0=op0)
        return self.I(e, 'tensor_scalar', part=part, out=out, in0=in0, scalar1=s1, scalar2=s2, op0=op0, op1=op1)

    def cp(self, e, out, in_, part=False):
        if e == 'act':
            return self.I('act', 'activation', part=part, out=out, in_=in_, func=AF.Copy)
        return self.I(e, 'tensor_copy', part=part, out=out, in_=in_)

    def act(self, out, in_, func, part=False, **kw):
        return self.I('act', 'activation', part=part, out=out, in_=in_, func=func, **kw)


D = 2048; T_SEQ = 4096; IN_W = 5968
RMS_EPS = 1e-6


def rms_rstd(fw, xt, ss, junk, eps=RMS_EPS, d=D):
    fw.I('pool', 'memset', ap=ss[:, 0:1], constant=0.0)
    fw.act(junk, xt, AF.Square, accum_out=ss[:, 0:1])
    fw.act(ss[:, 1:2], ss[:, 0:1], AF.Sqrt, bias=eps, scale=1.0 / d)
    fw.I('dve', 'reciprocal', out=ss[:, 2:3], in_=ss[:, 1:2])


def stage_A(fw, C, x_d, w_in_d, g_d, u_d):
    with ExitStack() as es:
        xnT = fw.sb('A_xnT', [128, 16, 2048], BF16, es)
        wt = [fw.sb('A_wt%d' % i, [128, 16, 512], BF16, es) for i in range(2)]
        xt = [fw.sb('A_xt%d' % i, [128, 2048], F32, es) for i in range(2)]
        xb = [fw.sb('A_xb%d' % i, [128, 2048], BF16, es) for i in range(2)]
        junk = fw.sb('A_junk', [128, 2048], BF16, es)
        ss = [fw.sb('A_ss%d' % i, [128, 4], F32, es) for i in range(2)]
        ost = [fw.sb('A_ost%d' % i, [128, 512], F32, es) for i in range(4)]
        g_sb = fw.sb('A_g', [128, 16], F32, es)
        pT = [fw.ps('A_pT%d' % i, [128, 8, 128], BF16, es) for i in range(2)]
        pM = [fw.ps('A_pM%d' % i, [128, 512], F32, es) for i in range(4)]
        fw.dma('sp', g_sb[:], g_d[:])
        nblk = (IN_W + 511) // 512
        wi = 0; oi = 0
        for half in range(2):
            for tt in range(16):
                t0 = half * 2048 + tt * 128
                X = xt[tt % 2]; XB = xb[tt % 2]; S = ss[tt % 2]
                fw.dma('sp', X[:], x_d[t0:t0 + 128, :])
                rms_rstd(fw, X[:], S, junk[:])
                fw.ts('dve', XB[:], X[:], S[:, 2:3], ALU.mult)
                for hh in range(2):
                    P = pT[hh]
                    for j in range(8):
                        k = hh * 8 + j
                        fw.tr(P[:, j, :], XB[:, k * 128:(k + 1) * 128], C['ident_b'])
                    for j in range(8):
                        k = hh * 8 + j
                        fw.ts('dve' if j % 2 else 'pool_', xnT[:, k, tt * 128:(tt + 1) * 128], P[:, j, :], g_sb[:, k:k + 1], ALU.mult, part=True) if False else \
                            fw.act(xnT[:, k, tt * 128:(tt + 1) * 128], P[:, j, :], AF.Copy, scale=g_sb[:, k:k + 1], part=True) if j % 2 else \
                            fw.ts('dve', xnT[:, k, tt * 128:(tt + 1) * 128], P[:, j, :], g_sb[:, k:k + 1], ALU.mult, part=True)
            for cb in range(nblk):
                c0 = cb * 512; cw = min(512, IN_W - c0)
                W = wt[wi % 2]; wi += 1
                fw.dma('pool', W[:, :, 0:cw], w_in_d[:, c0:c0 + cw].rearrange("(k p) c -> p k c", p=128))
                for tt in range(16):
                    t0 = half * 2048 + tt * 128
                    P = pM[oi % 4]; O = ost[oi % 4]
                    for k in range(16):
                        fw.mm(P[:, 0:cw], xnT[:, k, tt * 128:(tt + 1) * 128], W[:, k, 0:cw], start=(k == 0), stop=(k == 15))
                    fw.cp('act' if oi % 2 else 'dve', O[:, 0:cw], P[:, 0:cw])
                    fw.dma('sp', u_d[t0:t0 + 128, c0:c0 + cw], O[:, 0:cw])
                    oi += 1
    fw.barrier()


RWC = 3360


def stage_B(fw, C, u_d, P_, yrw_d, hg, conv=None):
    NCH = 64; HW = 512
    c_r = hg * 512; c_k = 1024 + hg * 512; c_v = 2048 + hg * 512
    with ExitStack() as es:
        sb = lambda n, s, dt=F32: fw.sb('B%d_%s' % (hg, n), s, dt, es)
        U = sb('U', [64, 1824]); Us = sb('Us', [64, 1824]); mu = sb('mu', [64, 1824])
        bc = {n: sb(n, [64, HW]) for n in ('w0', 'a0', 'kkp', 'kap', 'rkp', 'lnw', 'lnb')}
        w2 = sb('w2', [64, HW]); a2 = sb('a2', [64, HW]); g2a = sb('g2a', [128, HW]); g2b = sb('g2b', [32, HW])
        names = ['logw', 'a', 'g', 'kk', 'km', 'b', 'W', 'Winv', 'Wprev', 'WCb', 'bt', 'kt', 't1', 't2', 'Y', 'yn']
        X = {n: sb(n, [64, HW]) for n in names}
        Vt = [sb('V%d' % i, [64, HW]) for i in range(2)]
        Bh = [sb('Bh%d' % i, [64, HW]) for i in range(2)]
        Kh = [sb('Kh%d' % i, [64, HW]) for i in range(2)]
        at = sb('at', [64, HW]); rt = sb('rt', [64, HW])
        aT = [sb('aT%d' % i, [64, HW]) for i in range(2)]; rT = [sb('rT%d' % i, [64, HW]) for i in range(2)]
        bT = sb('bT', [64, HW]); kT = sb('kT', [64, HW])
        Aab = sb('Aab', [64, HW]); AabT = sb('AabT', [64, HW])
        Aak = [sb('Aak%d' % i, [64, HW]) for i in range(2)]
        Abr = [sb('Abr%d' % i, [64, HW]) for i in range(2)]
        Akr = [sb('Akr%d' % i, [64, HW]) for i in range(2)]
        Tm = [sb('Tm%d' % i, [64, HW]) for i in range(2)]
        Pm = [sb('Pm%d' % i, [64, HW]) for i in range(2)]; PTm = [sb('PTm%d' % i, [64, HW]) for i in range(2)]
        XT = sb('XT', [64, HW]); SAT = sb('SAT', [64, HW]); ST = sb('ST', [64, HW])
        txw = sb('txw', [64, 64]); xaT = sb('xaT', [64, 64]); sg1 = sb('sg1', [128, 64]); sg2 = sb('sg2', [32, 64])
        sm = sb('sm', [64, 64]); wcc = [sb('wcc%d' % i, [64, 8]) for i in range(2)]
        pp = [fw.ps('B%d_p%d' % (hg, i), [128, 512], F32, es) for i in range(4)]
        pX = fw.ps('B%d_pX' % hg, [128, 512], F32, es); pS = fw.ps('B%d_pS' % hg, [128, 512], F32, es)
        pY = fw.ps('B%d_pY' % hg, [128, 512], F32, es); pN = fw.ps('B%d_pN' % hg, [128, 512], F32, es)
        pi = [0]
        def bank():
            pi[0] += 1; return pp[pi[0] % 4]
        I64 = C['ident_f'][0:64, 0:64]; ones = C['ones'][0:64, 0:64]
        triu_t = sb('triu', [64, 64]); fw.dma('sp', triu_t[:], P_['c_triu'][:, :]); tri = triu_t[:]
        mk_t = {}
        for n_ in ('mus', 'mls', 'mui', 'eye8'):
            mk_t[n_] = sb('m_' + n_, [64, HW]); fw.dma('sp', mk_t[n_][:], P_['c_' + n_][:, :])
        MUs = mk_t['mus'][:]; MLs = mk_t['mls'][:]; MUi = mk_t['mui'][:]; EYE = mk_t['eye8'][:]
        fw.dma('sp', mu[:], P_['mu'][:, :].partition_broadcast(64))
        for n in bc: fw.dma('sp', bc[n][:], P_[n][:, :].partition_broadcast(64))
        fw.dma('sp', w2[:], P_['w2'][:, :]); fw.dma('sp', a2[:], P_['a2'][:, :])
        fw.dma('sp', g2a[:], P_['g2'][0:128, :]); fw.dma('sp', g2b[:], P_['g2'][128:160, :])
        fw.I('pool', 'memset', ap=ST[:], constant=0.0)
        cols = ((c_r, 0), (c_k, 512), (c_v, 1024))
        for c in range(NCH):
            t0 = c * 64; d = c % 2
            if conv is not None:
                for _ in range(3): next(conv, None)
            for (cs, o) in cols:
                fw.dma('sp', U[:, o:o + 512], u_d[t0:t0 + 64, cs:cs + 512], part=True)
            fw.dma('sp', U[:, 1536:1824], u_d[t0:t0 + 64, 3072:3360], part=True)
            if c == 0:
                fw.I('pool', 'memset', ap=Us[:], constant=0.0)
                for (cs, o) in cols:
                    fw.dma('sp', Us[1:64, o:o + 512], u_d[0:63, cs:cs + 512], part=True)
                fw.dma('sp', Us[1:64, 1536:1824], u_d[0:63, 3072:3360], part=True)
            else:
                for (cs, o) in cols:
                    fw.dma('sp', Us[:, o:o + 512], u_d[t0 - 1:t0 + 63, cs:cs + 512], part=True)
                fw.dma('sp', Us[:, 1536:1824], u_d[t0 - 1:t0 + 63, 3072:3360], part=True)
            fw.tt('pool', Us[:], Us[:], U[:], ALU.subtract)
            fw.tt('pool', Us[:], Us[:], mu[:], ALU.mult)
            fw.tt('dve', U[:], U[:], Us[:], ALU.add)
            r = U[:, 0:512]; k = U[:, 512:1024]
            V = Vt[d]
            fw.cp('pool', V[:], U[:, 1024:1536])
            P1 = bank()
            fw.mm(P1[0:64, 0:64], U[:, 1536:1600], I64)
            fw.mm(P1[0:64, 64:128], U[:, 1600:1664], I64)
            fw.mm(P1[0:128, 128:192], U[:, 1664:1792], I64)
            fw.mm(P1[0:32, 192:256], U[:, 1792:1824], I64)
            fw.act(txw[:], P1[0:64, 0:64], AF.Tanh)
            fw.cp('dve', xaT[:], P1[0:64, 64:128])
            fw.act(sg1[:], P1[0:128, 128:192], AF.Sigmoid)
            fw.act(sg2[:], P1[0:32, 192:256], AF.Sigmoid)
            Pz = bank(); fw.mm(Pz[0:64, :], txw[:], w2[:])
            fw.tt('dve', X['t1'][:], Pz[0:64, :], bc['w0'][:], ALU.add)
            fw.act(X['t2'][:], X['t1'][:], AF.Sigmoid)
            fw.ts('pool', X['logw'][:], X['t2'][:], -0.6065306597126334, ALU.mult)
            Pa = bank(); fw.mm(Pa[0:64, :], xaT[:], a2[:])
            fw.tt('dve', X['t1'][:], Pa[0:64, :], bc['a0'][:], ALU.add)
            fw.act(X['a'][:], X['t1'][:], AF.Sigmoid)
            Pg = bank(); fw.mm(Pg[0:64, :], sg1[:], g2a[:], start=True, stop=False); fw.mm(Pg[0:64, :], sg2[:], g2b[:], start=False, stop=True)
            fw.cp('act', X['g'][:], Pg[0:64, :])
            fw.tt('pool', X['kk'][:], k, bc['kkp'][:], ALU.mult)
            fw.tt('pool', X['t1'][:], X['kk'][:], X['kk'][:], ALU.mult)
            fw.I('dve', 'tensor_reduce', out=sm[:, 0:8], in_=X['t1'][:].rearrange("p (h j) -> p h j", h=8), axis=AX.X, op=ALU.add)
            fw.act(sm[:, 8:16], sm[:, 0:8], AF.Sqrt)
            fw.ts('dve', sm[:, 8:16], sm[:, 8:16], 1e-12, ALU.max)
            fw.I('dve', 'reciprocal', out=sm[:, 16:24], in_=sm[:, 8:16])
            for h in range(8):
                fw.ts('dve', X['kk'][:, h * 64:(h + 1) * 64], X['kk'][:, h * 64:(h + 1) * 64], sm[:, 16 + h:17 + h], ALU.mult, part=True)
            fw.ts('pool', X['t1'][:], X['a'][:], -1.0, ALU.add)
            fw.tt('pool', X['t1'][:], X['t1'][:], bc['kap'][:], ALU.mult)
            fw.ts('pool', X['t1'][:], X['t1'][:], 1.0, ALU.add)
            fw.tt('dve', X['km'][:], k, X['t1'][:], ALU.mult)
            fw.tt('pool', X['b'][:], X['kk'][:], X['a'][:], ALU.mult)
            Pc = bank(); fw.mm(Pc[0:64, :], tri, X['logw'][:])
            fw.act(X['W'][:], Pc[0:64, :], AF.Exp)
            fw.act(X['Winv'][:], Pc[0:64, :], AF.Exp, scale=-1.0)
            fw.tt('dve', X['t1'][:], Pc[0:64, :], X['logw'][:], ALU.subtract)
            fw.act(X['Wprev'][:], X['t1'][:], AF.Exp)
            Pt = bank(); fw.mm(Pt[0:64, :], ones, X['logw'][:])
            fw.act(X['WCb'][:], Pt[0:64, :], AF.Exp)
            Pw = bank()
            for h in range(8):
                fw.mm(Pw[0:64, h:h + 1], X['logw'][:, h * 64:(h + 1) * 64], ones[:, 0:1])
            fw.act(wcc[d][:], Pw[0:64, 0:8], AF.Exp)
            fw.I('dve', 'scalar_tensor_tensor', out=at[:], in0=X['kk'][:], scalar=-1.0, in1=X['Wprev'][:], op0=ALU.mult, op1=ALU.mult)
            fw.tt('pool', X['bt'][:], X['b'][:], X['Winv'][:], ALU.mult)
            fw.tt('pool', X['kt'][:], X['km'][:], X['Winv'][:], ALU.mult)
            fw.tt('pool', rt[:], r, X['W'][:], ALU.mult)
            fw.tt('pool', Bh[d][:], X['bt'][:], X['WCb'][:], ALU.mult)
            fw.tt('pool', Kh[d][:], X['kt'][:], X['WCb'][:], ALU.mult)
            for (src, dst, e_) in ((at, aT[d], 'act'), (X['bt'], bT, 'dve'), (X['kt'], kT, 'act'), (rt, rT[d], 'dve')):
                Pq = bank()
                for h in range(8):
                    fw.mm(Pq[0:64, h * 64:(h + 1) * 64], src[:, h * 64:(h + 1) * 64], I64)
                fw.cp(e_, dst[:], Pq[0:64, :])
            for (l_, r_, dst, msk) in ((bT, aT[d], Aab, MUs), (aT[d], bT, AabT, MLs), (kT, aT[d], Aak[d], MUs), (bT, rT[d], Abr[d], MUi), (kT, rT[d], Akr[d], MUi)):
                Pq = bank()
                for h in range(8):
                    fw.mm(Pq[0:64, h * 64:(h + 1) * 64], l_[:, h * 64:(h + 1) * 64], r_[:, h * 64:(h + 1) * 64])
                fw.tt('dve', dst[:], Pq[0:64, :], msk, ALU.mult)
            T_ = Tm[d]
            fw.tt('pool', T_[:], Aab[:], EYE, ALU.add)
            Pc_, PTc_ = Aab, AabT
            for lvl in range(5):
                Pn, PTn = Pm[lvl % 2], PTm[lvl % 2]
                Pq = bank()
                for h in range(8):
                    hs = slice(h * 64, (h + 1) * 64)
                    fw.mm(Pq[0:64, hs], Pc_[:, hs], PTc_[:, hs])
                fw.cp('act', PTn[:], Pq[0:64, :])
                if lvl < 4:
                    Pq2 = bank()
                    for h in range(8):
                        hs = slice(h * 64, (h + 1) * 64)
                        fw.mm(Pq2[0:64, hs], PTc_[:, hs], Pc_[:, hs])
                    fw.cp('dve', Pn[:], Pq2[0:64, :])
                Pq3 = bank()
                for h in range(8):
                    hs = slice(h * 64, (h + 1) * 64)
                    fw.mm(Pq3[0:64, hs], PTn[:, hs], T_[:, hs])
                fw.tt('dve', T_[:], T_[:], Pq3[0:64, :], ALU.add)
                Pc_, PTc_ = Pn, PTn
            for h in range(8):
                hs = slice(h * 64, (h + 1) * 64)
                fw.mm(pX[0:64, hs], aT[d][:, hs], ST[:, hs], start=True, stop=False)
                fw.mm(pX[0:64, hs], Aak[d][:, hs], V[:, hs], start=False, stop=True)
            fw.cp('act', XT[:], pX[0:64, :])
            for h in range(8):
                hs = slice(h * 64, (h + 1) * 64)
                fw.mm(pS[0:64, hs], T_[:, hs], XT[:, hs])
            fw.cp('dve', SAT[:], pS[0:64, :])
            for h in range(8):
                hs = slice(h * 64, (h + 1) * 64)
                fw.mm(pY[0:64, hs], rT[d][:, hs], ST[:, hs], start=True, stop=False)
                fw.mm(pY[0:64, hs], Abr[d][:, hs], SAT[:, hs], start=False, stop=False)
                fw.mm(pY[0:64, hs], Akr[d][:, hs], V[:, hs], start=False, stop=True)
            fw.cp('act', X['Y'][:], pY[0:64, :])
            for h in range(8):
                hs = slice(h * 64, (h + 1) * 64)
                fw.mm(pN[0:64, hs], Bh[d][:, hs], SAT[:, hs], start=True, stop=False)
                fw.mm(pN[0:64, hs], Kh[d][:, hs], V[:, hs], start=False, stop=True)
            for h in range(8):
                hs = slice(h * 64, (h + 1) * 64)
                fw.I('dve', 'scalar_tensor_tensor', part=True, out=ST[:, hs], in0=ST[:, hs], scalar=wcc[d][:, h:h + 1], in1=pN[0:64, hs], op0=ALU.mult, op1=ALU.add)
            Y = X['Y']; yn = X['yn']; t1 = X['t1']; t2 = X['t2']
            Y3 = Y[:].rearrange("p (h j) -> p h j", h=8)
            fw.I('dve', 'tensor_reduce', out=sm[:, 24:32], in_=Y3, axis=AX.X, op=ALU.add)
            fw.tt('pool', t1[:], Y[:], Y[:], ALU.mult)
            fw.I('dve', 'tensor_reduce', out=sm[:, 32:40], in_=t1[:].rearrange("p (h j) -> p h j", h=8), axis=AX.X, op=ALU.add)
            fw.ts('dve', sm[:, 24:32], sm[:, 24:32], 1.0 / 64, ALU.mult)
            fw.tt('dve', sm[:, 40:48], sm[:, 24:32], sm[:, 24:32], ALU.mult)
            fw.I('dve', 'scalar_tensor_tensor', out=sm[:, 32:40], in0=sm[:, 32:40], scalar=1.0 / 64, in1=sm[:, 40:48], op0=ALU.mult, op1=ALU.subtract)
            fw.act(sm[:, 40:48], sm[:, 32:40], AF.Sqrt, bias=64e-5)
            fw.I('dve', 'reciprocal', out=sm[:, 48:56], in_=sm[:, 40:48])
            for h in range(8):
                hs = slice(h * 64, (h + 1) * 64)
                fw.ts('dve', yn[:, hs], Y[:, hs], sm[:, 24 + h:25 + h], ALU.subtract, sm[:, 48 + h:49 + h], ALU.mult, part=True)
            fw.tt('pool', yn[:], yn[:], bc['lnw'][:], ALU.mult)
            fw.tt('pool', yn[:], yn[:], bc['lnb'][:], ALU.add)
            fw.tt('pool', t1[:], r, X['km'][:], ALU.mult)
            fw.tt('pool', t1[:], t1[:], bc['rkp'][:], ALU.mult)
            fw.I('dve', 'tensor_reduce', out=sm[:, 56:64], in_=t1[:].rearrange("p (h j) -> p h j", h=8), axis=AX.X, op=ALU.add)
            for h in range(8):
                hs = slice(h * 64, (h + 1) * 64)
                fw.I('dve', 'scalar_tensor_tensor', part=True, out=t2[:, hs], in0=V[:, hs], scalar=sm[:, 56 + h:57 + h], in1=yn[:, hs], op0=ALU.mult, op1=ALU.add)
            fw.tt('pool', t2[:], t2[:], X['g'][:], ALU.mult)
            fw.dma('sp', yrw_d[t0:t0 + 64, hg * 512:(hg + 1) * 512], t2[:])
    fw.barrier()


DBG_C = 9
DBG_C0 = 9
NQ = 3360


def stage_C0(fw, C, u_d, Pn, KcT, Vaug, es_outer):
    with ExitStack() as es:
        sb = lambda n, s, dt=F32: fw.sb('C0_' + n, s, dt, es)
        kcT = sb('kcT', [64, 4, 4096], BF16); vcT = sb('vcT', [64, 4, 4096], BF16)
        Nt = [sb('N%d' % i, [128, 512]) for i in range(2)]
        Nb = [sb('Nb%d' % i, [128, 512], BF16) for i in range(2)]
        w1 = {kv: sb('w1' + kv, [64, 32, 256], BF16) for kv in 'kv'}
        w2 = {kv: sb('w2' + kv, [128, 2, 64], BF16) for kv in 'kv'}
        posT = {kv: sb('pos' + kv, [64, 32], BF16) for kv in 'kv'}
        b1 = {kv: sb('b1' + kv, [128, 2]) for kv in 'kv'}
        b2k = sb('b2k', [64, 1]); b2v = sb('b2v', [128, 64]); kn0 = sb('kn0', [64, 1])
        bias = {kv: sb('bias' + kv, [128, 2]) for kv in 'kv'}
        hid = [sb('hid%d' % i, [128, 256], BF16) for i in range(2)]
        x = sb('x', [128, 256]); x2 = sb('x2', [128, 256]); x3 = sb('x3', [128, 256]); sg = sb('sg', [128, 256])
        kc_f = sb('kc_f', [64, 256]); sq = sb('sq', [64, 256]); rs = sb('rs', [64, 256])
        vtmp = sb('vtmp', [128, 64])
        pT = [fw.ps('C0_pT%d' % i, [128, 4, 128], F32, es) for i in range(2)]
        pH = [fw.ps('C0_pH%d' % i, [128, 512], F32, es) for i in range(2)]
        pK = fw.ps('C0_pK', [128, 512], F32, es); pB = fw.ps('C0_pB', [128, 512], F32, es)
        for kv in 'kv':
            fw.dma('pool', w1[kv][:], Pn['w1' + kv][:, :].rearrange("(l d) h -> d l h", d=64))
            fw.dma('pool', w2[kv][:], Pn['w2' + kv][:, :].rearrange("(c p) d -> p c d", p=128))
            fw.dma('pool', posT[kv][:], Pn['posT' + kv][:, :])
            fw.dma('sp', b1[kv][:], Pn['b1' + kv][:, :])
        fw.dma('sp', b2k[:], Pn['b2k'][:, :]); fw.dma('sp', b2v[:], Pn['b2v'][:, :].partition_broadcast(128))
        fw.dma('sp', kn0[:], Pn['kn0'][:, :])
        fw.dma('sp', Vaug[:, 0, :, 65:129], Pn['ovl'][0:128, :, :], part=True)
        fw.dma('sp', Vaug[:, 1, :, 65:129], Pn['ovl'][128:256, :, :], part=True)
        fw.I('dve', 'memset', ap=Vaug[:, :, :, 64:65], constant=1.0, part=True)
        for tt in range(32 if DBG_C0 >= 1 else 0):
            t0 = tt * 128
            N = Nt[tt % 2]; NB = Nb[tt % 2]
            fw.dma('sp', N[:], u_d[t0:t0 + 128, NQ + 1024:NQ + 1536])
            fw.cp('dve', NB[:], N[:])
            for hf, dstT in ((0, kcT), (1, vcT)):
                P = pT[hf]
                for j in range(4):
                    fw.mm(P[0:64, j, :], NB[:, (hf * 4 + j) * 64:(hf * 4 + j + 1) * 64], C['ident_b'])
                fw.cp('act' if hf else 'dve', dstT[:, :, t0:t0 + 128], P[0:64, 0:4, :], part=True)
        for kv in ('kv' if DBG_C0 >= 2 else ''):
            for hh in range(2):
                for l in range(32):
                    fw.mm(pB[:, hh:hh + 1], w1[kv][:, l, hh * 128:(hh + 1) * 128], posT[kv][:, l:l + 1], start=(l == 0), stop=(l == 31))
            fw.tt('dve', bias[kv][:], pB[:, 0:2], b1[kv][:], ALU.add)
        hi = 0
        for kv in ('kv' if DBG_C0 >= 3 else ''):
            src = kcT if kv == 'k' else vcT
            for g in range(4):
                for hh in range(2):
                    P = pH[hi % 2]; hi += 1
                    for l in range(32):
                        fw.mm(P[:, 0:255], w1[kv][:, l, hh * 128:(hh + 1) * 128], (src[:, g, :].rearrange("p (n s) -> p n s", s=16)[:, 0:255, l] if l < 16 else src[:, g, :].rearrange("p (n s) -> p n s", s=16)[:, 1:256, l - 16]), start=(l == 0), stop=(l == 31))
                    fw.ts('dve', x[:, 0:255], P[:, 0:255], bias[kv][:, hh:hh + 1], ALU.add)
                    fw.tt('pool', x2[:, 0:255], x[:, 0:255], x[:, 0:255], ALU.mult)
                    fw.tt('pool', x3[:, 0:255], x2[:, 0:255], x[:, 0:255], ALU.mult)
                    fw.I('dve', 'scalar_tensor_tensor', out=x2[:, 0:255], in0=x3[:, 0:255], scalar=0.044715, in1=x[:, 0:255], op0=ALU.mult, op1=ALU.add)
                    fw.act(sg[:, 0:255], x2[:, 0:255], AF.Sigmoid, scale=1.5957691216057308)
                    fw.tt('dve', hid[hh][:, 0:255], x[:, 0:255], sg[:, 0:255], ALU.mult)
                if DBG_C0 < 4: continue
                if kv == 'k':
                    for hh in range(2):
                        fw.mm(pK[0:64, 0:255], w2['k'][:, hh, :], hid[hh][:, 0:255], start=(hh == 0), stop=(hh == 1))
                    fw.ts('dve', kc_f[:, 0:255], pK[0:64, 0:255], b2k[:, 0:1], ALU.add)
                    fw.tt('pool', sq[:, 0:255], kc_f[:, 0:255], kc_f[:, 0:255], ALU.mult)
                    fw.mm(pK[0:64, 256:511], C['ones'][0:64, 0:64], sq[:, 0:255])
                    fw.act(rs[:, 0:255], pK[0:64, 256:511], AF.Sqrt, bias=RMS_EPS, scale=1.0 / 64)
                    fw.I('dve', 'reciprocal', out=rs[:, 0:255], in_=rs[:, 0:255])
                    fw.tt('pool', kc_f[:, 0:255], kc_f[:, 0:255], rs[:, 0:255], ALU.mult)
                    fw.ts('dve', KcT[:, g, 0:255], kc_f[:, 0:255], kn0[:, 0:1], ALU.mult, part=True)
                else:
                    for c in range(2):
                        n = 128 if c == 0 else 127
                        for hh in range(2):
                            fw.mm(pK[0:n, 0:64], hid[hh][:, c * 128:c * 128 + n], w2['v'][:, hh, :], start=(hh == 0), stop=(hh == 1))
                        fw.tt('dve', Vaug[0:n, c, g, 0:64], pK[0:n, 0:64], b2v[0:n, :], ALU.add, part=True)
    fw.barrier()


def stage_C(fw, C, u_d, Pn, ynsa_d):
    with ExitStack() as es:
        sb = lambda n, s, dt=F32: fw.sb('C_' + n, s, dt, es)
        KcT = sb('KcT', [64, 4, 256], BF16); Vaug = sb('Vaug', [128, 2, 4, 129], BF16)
        fw.I('dve', 'memset', ap=KcT[:], constant=0.0)
        fw.I('dve', 'memset', ap=Vaug[:], constant=0.0)
        stage_C0(fw, C, u_d, Pn, KcT, Vaug, es)
        if DBG_C < 1:
            return
        ksT = sb('ksT', [64, 4, 4096], BF16); kwT = sb('kwT', [64, 4, 4096], BF16)
        Vs = sb('Vs', [128, 32, 4, 65], BF16); Vw = sb('Vw', [128, 32, 4, 65], BF16)
        Nt = [sb('N0', [128, 2608])] * 2
        Eb = sb('Eb', [128, 32, 4, 128], BF16)
        Ew = sb('Ew', [128, 5, 4, 128], BF16)
        sq = sb('sq', [128, 1024]); qn = sb('qn', [128, 1024], BF16); kn = sb('kn', [128, 512], BF16)
        qnb = sb('qnb', [128, 1024]); knb = sb('knb', [128, 512])
        st = sb('st', [128, 96]); gat = sb('gat', [128, 48])
        qT = [sb('qT%d' % i, [64, 4, 128], BF16) for i in range(2)]
        E = [sb('E%d' % i, [128, 4, 128], BF16) for i in range(3)]
        cm = [sb('cm%d' % i, [128, 2, 128], BF16) for i in range(2)]
        keep = [sb('keep%d' % i, [128, 64]) for i in range(2)]; addm = [sb('addm%d' % i, [128, 64]) for i in range(2)]
        selx = sb('selx', [64, 32, 128], BF16)
        cauT = sb('cauT', [128, 128], BF16); acauT = sb('acauT', [128, 128], BF16)
        imp = sb('imp', [128, 64]); mr = sb('mr', [128, 64]); selm = sb('selm', [128, 64], BF16); selT = sb('selT', [64, 128], BF16)
        t8 = sb('t8', [128, 24]); rsd = sb('rsd', [128, 16]); coef = sb('coef', [128, 16])
        yt = [sb('yt0', [128, 1024])] * 2
        qnw = sb('qnw', [128, 1024]); knw = sb('knw', [128, 512])
        pS = [fw.ps('C_pS%d' % i, [128, 512], F32, es) for i in range(2)]
        pM = fw.ps('C_pM', [128, 512], F32, es)
        pT = fw.ps('C_pT', [128, 4, 128], F32, es)
        pOc = [fw.ps('C_pOc%d' % i, [128, 2, 129], F32, es) for i in range(2)]
        pOs = fw.ps('C_pOs', [128, 4, 65], F32, es); pOw = fw.ps('C_pOw', [128, 4, 65], F32, es)
        fw.dma('sp', qnw[:], Pn['qnw'][:, :].partition_broadcast(128))
        fw.dma('sp', knw[:], Pn['knw'][:, :].partition_broadcast(128))
        fw.dma('sp', selx[:], Pn['selx'][:, :, :]); fw.dma('sp', cauT[:], Pn['cauT'][:, :]); fw.dma('sp', acauT[:], Pn['acauT'][:, :])
        fw.I('dve', 'memset', ap=Vs[:, :, :, 64:65], constant=1.0, part=True)
        fw.I('dve', 'memset', ap=Vw[:, :, :, 64:65], constant=1.0, part=True)
        ei = 0; si = 0
        for i in range(32):
            t0 = i * 128
            N = Nt[i % 2]
            fw.dma('sp', N[:], u_d[t0:t0 + 128, NQ:NQ + 2608])
            fw.dma('sp', cm[i % 2][:], Pn['cmask'][i, :, :, :])
            fw.dma('sp', keep[i % 2][:], Pn['keep'][i, :, :]); fw.dma('sp', addm[i % 2][:], Pn['addm'][i, :, :])
            fw.tt('pool', sq[:], N[:, 0:1024], N[:, 0:1024], ALU.mult)
            fw.I('dve', 'tensor_reduce', out=st[:, 0:16], in_=sq[:].rearrange("p (h j) -> p h j", h=16), axis=AX.X, op=ALU.add)
            fw.act(st[:, 16:32], st[:, 0:16], AF.Sqrt, bias=RMS_EPS, scale=1.0 / 64)
            fw.I('dve', 'reciprocal', out=st[:, 32:48], in_=st[:, 16:32])
            fw.ts('dve', st[:, 32:48], st[:, 32:48], 0.125, ALU.mult)
            for h in range(16):
                fw.ts('dve', qnb[:, h * 64:(h + 1) * 64], N[:, h * 64:(h + 1) * 64], st[:, 32 + h:33 + h], ALU.mult, part=True)
            fw.tt('dve', qn[:], qnb[:], qnw[:], ALU.mult)
            for (bi, off, dstT, Vd) in ((0, 1536, ksT, Vs), (1, 2048, kwT, Vw)):
                fw.tt('pool', sq[:, 0:256], N[:, off:off + 256], N[:, off:off + 256], ALU.mult)
                fw.I('dve', 'tensor_reduce', out=st[:, 48:52], in_=sq[:, 0:256].rearrange("p (h j) -> p h j", h=4), axis=AX.X, op=ALU.add)
                fw.act(st[:, 52:56], st[:, 48:52], AF.Sqrt, bias=RMS_EPS, scale=1.0 / 64)
                fw.I('dve', 'reciprocal', out=st[:, 56:60], in_=st[:, 52:56])
                for g in range(4):
                    fw.ts('dve', knb[:, bi * 256 + g * 64:bi * 256 + (g + 1) * 64], N[:, off + g * 64:off + (g + 1) * 64], st[:, 56 + g:57 + g], ALU.mult, part=True)
                fw.tt('dve', kn[:, bi * 256:(bi + 1) * 256], knb[:, bi * 256:(bi + 1) * 256], knw[:, bi * 256:(bi + 1) * 256], ALU.mult, part=True)
                for g in range(4):
                    fw.mm(pT[0:64, g, :], kn[:, bi * 256 + g * 64:bi * 256 + (g + 1) * 64], C['ident_b'])
                fw.cp('act', dstT[:, :, t0:t0 + 128], pT[0:64, 0:4, :], part=True)
                fw.cp('dve', Vd[:, i, :, 0:64], N[:, off + 256:off + 512].rearrange("p (g d) -> p g d", g=4), part=True)
            fw.act(gat[:], N[:, 2560:2608], AF.Sigmoid)
            y = yt[i % 2]
            for g in range(4 if DBG_C >= 2 else 0):
                Q = qT[si % 2]; si += 1
                for r_ in range(4):
                    h = g * 4 + r_
                    fw.mm(pT[0:64, r_, :], qn[:, h * 64:(h + 1) * 64], C['ident_b'])
                fw.cp('act', Q[:], pT[0:64, 0:4, :])
                Q2 = Q[:].rearrange("p r q -> p (r q)")
                nchunk = 1 if (8 * i + 6) < 128 else 2
                Ets = []
                for c in range(nchunk):
                    n = 128 if c == 0 else 127
                    P = pS[ei % 2]; Et = E[ei % 3]; ei += 1
                    Ets.append(Et)
                    fw.mm(P[0:n, :], KcT[:, g, c * 128:c * 128 + n], Q2)
                    fw.act(Et[0:n].rearrange("p r q -> p (r q)"), P[0:n, :], AF.Exp)
                    for r_ in range(4):
                        fw.tt('dve', Et[0:n, r_, :], Et[0:n, r_, :], cm[i % 2][0:n, c, :], ALU.mult, part=True)
                for r_ in range(4):
                    for c in range(nchunk):
                        n = 128 if c == 0 else 127
                        fw.mm(pOc[r_ // 2][:, r_ % 2, :], Ets[c][0:n, r_, :], Vaug[0:n, c, g, :], start=(c == 0), stop=(c == nchunk - 1))
                for r_ in range(4):
                    fw.ts('dve', rsd[:, r_:r_ + 1], pOc[r_ // 2][:, r_ % 2, 64:65], 1e-30, ALU.max, part=True)
                fw.I('dve', 'reciprocal', out=rsd[:, 4:8], in_=rsd[:, 0:4])
                fw.ts('dve', imp[:], pOc[0][:, 0, 65:129], rsd[:, 4:5], ALU.mult)
                for r_ in range(1, 4):
                    fw.I('dve', 'scalar_tensor_tensor', out=imp[:], in0=pOc[r_ // 2][:, r_ % 2, 65:129], scalar=rsd[:, 4 + r_:5 + r_], in1=imp[:], op0=ALU.mult, op1=ALU.add)
                if DBG_C < 3: continue
                fw.tt('dve', imp[:], imp[:], keep[i % 2][:], ALU.mult)
                fw.tt('dve', imp[:], imp[:], addm[i % 2][:], ALU.add)
                fw.I('dve', 'max', out=t8[:, 0:8], in_=imp[:])
                fw.I('dve', 'match_replace', out=mr[:], in_to_replace=t8[:, 0:8], in_values=imp[:], imm_value=-2e30)
                fw.I('dve', 'max', out=t8[:, 8:16], in_=mr[:])
                fw.ts('dve', t8[:, 16:17], t8[:, 15:16], -1e29, ALU.max)
                fw.ts('dve', selm[:], imp[:], t8[:, 16:17], ALU.is_ge)
                fw.mm(pT[0:64, 0, :], selm[:], C['ident_b'])
                fw.cp('act', selT[:], pT[0:64, 0, :])
                if DBG_C < 4: continue
                for kt in range(i + 1):
                    P = pS[ei % 2]; ei += 1
                    fw.mm(P[:], ksT[:, g, kt * 128:(kt + 1) * 128], Q2)
                    fw.mm(pM[:, 0:128], selx[:, kt, :], selT[:])
                    fw.act(Eb[:, kt].rearrange("p r q -> p (r q)"), P[:], AF.Exp, part=True)
                    for r_ in range(4):
                        fw.tt('dve', Eb[:, kt, r_, :], Eb[:, kt, r_, :], pM[:, 0:128], ALU.mult, part=True)
                        if kt == i:
                            fw.tt('dve', Eb[:, kt, r_, :], Eb[:, kt, r_, :], cauT[:], ALU.mult, part=True)
                for r_ in range(4):
                    for kt in range(i + 1):
                        fw.mm(pOs[:, r_, :], Eb[:, kt, r_, :], Vs[:, kt, g, :], start=(kt == 0), stop=(kt == i))
                if DBG_C < 5: continue
                k0 = max(0, i - 4)
                for kt in range(k0, i + 1):
                    P = pS[ei % 2]; ei += 1
                    sl_ = kt - k0
                    fw.mm(P[:], kwT[:, g, kt * 128:(kt + 1) * 128], Q2)
                    fw.act(Ew[:, sl_].rearrange("p r q -> p (r q)"), P[:], AF.Exp, part=True)
                    msk = cauT if kt == i else (acauT if kt == i - 4 else None)
                    if msk is not None:
                        for r_ in range(4):
                            fw.tt('dve', Ew[:, sl_, r_, :], Ew[:, sl_, r_, :], msk[:], ALU.mult, part=True)
                for r_ in range(4):
                    for kt in range(k0, i + 1):
                        fw.mm(pOw[:, r_, :], Ew[:, kt - k0, r_, :], Vw[:, kt, g, :], start=(kt == k0), stop=(kt == i))
                if DBG_C < 6: continue
                for r_ in range(4):
                    fw.ts('dve', rsd[:, 8 + r_:9 + r_], pOs[:, r_, 64:65], 1e-30, ALU.max, part=True)
                    fw.ts('dve', rsd[:, 12 + r_:13 + r_], pOw[:, r_, 64:65], 1e-30, ALU.max, part=True)
                fw.I('dve', 'reciprocal', out=coef[:, 4:12], in_=rsd[:, 8:16])
                fw.cp('dve', coef[:, 0:4], rsd[:, 4:8])
                for r_ in range(4):
                    h = g * 4 + r_
                    for b_ in range(3):
                        fw.tt('dve', coef[:, b_ * 4 + r_:b_ * 4 + r_ + 1], coef[:, b_ * 4 + r_:b_ * 4 + r_ + 1], gat[:, h * 3 + b_:h * 3 + b_ + 1], ALU.mult, part=True)
                for r_ in range(4):
                    h = g * 4 + r_
                    ys = y[:, h * 64:(h + 1) * 64]
                    fw.ts('dve', ys, pOc[r_ // 2][:, r_ % 2, 0:64], coef[:, r_:r_ + 1], ALU.mult, part=True)
                    fw.I('dve', 'scalar_tensor_tensor', part=True, out=ys, in0=pOs[:, r_, 0:64], scalar=coef[:, 4 + r_:5 + r_], in1=ys, op0=ALU.mult, op1=ALU.add)
                    fw.I('dve', 'scalar_tensor_tensor', part=True, out=ys, in0=pOw[:, r_, 0:64], scalar=coef[:, 8 + r_:9 + r_], in1=ys, op0=ALU.mult, op1=ALU.add)
            fw.dma('sp', ynsa_d[t0:t0 + 128, :], y[:])
    fw.barrier()


DBG_D = 9
NE = 32


def conv_weights(fw, w1_d, w2_d, w1b_d, w2b_d):
    for e in range(NE):
        for i in range(4):
            fw.dma('pool', w1b_d[e][i * 512:(i + 1) * 512, :].rearrange("(p a) c -> p (a c)", p=128),
                   w1_d[e, i * 512:(i + 1) * 512, :].rearrange("(p a) c -> p (a c)", p=128), part=True)
            yield
        for i in range(2):
            fw.dma('pool', w2b_d[e][i * 1024:(i + 1) * 1024, :].rearrange("(p a) c -> p (a c)", p=128),
                   w2_d[e, i * 1024:(i + 1) * 1024, :].rearrange("(p a) c -> p (a c)", p=128), part=True)
            yield


def stage_D(fw, C, yrw_d, ynsa_d, sel_d, xown_d, wout_d, mg_d, rw_d, rb_d, h1_d, xn2T_d, gates_d, gatesT_d):
    with ExitStack() as es:
        wo = fw.sb('D_wo', [128, 16, 2048], BF16, es)
        Y0 = [fw.sb('D_Y0%d' % i, [128, 2048], F32, es) for i in range(2)]
        Y1 = [fw.sb('D_Y1%d' % i, [128, 2048], F32, es) for i in range(2)]
        XO = [fw.sb('D_XO%d' % i, [128, 2048], F32, es) for i in range(2)]
        Yf = fw.sb('D_Yf', [128, 2048], F32, es)
        Yb = fw.sb('D_Yb', [128, 2048], BF16, es)
        yT = fw.sb('D_yT', [128, 16, 128], BF16, es)
        H = fw.sb('D_H', [128, 2048], F32, es)
        XN = fw.sb('D_XN', [128, 2048], F32, es)
        junk = fw.sb('D_junk', [128, 2048], BF16, es)
        xTh = fw.sb('D_xTh', [128, 16, 128], BF16, es)
        xTl = fw.sb('D_xTl', [128, 16, 128], BF16, es)
        XH = fw.sb('D_XH', [128, 2048], BF16, es)
        XL = fw.sb('D_XL', [128, 2048], BF16, es)
        rwh = fw.sb('D_rwh', [128, 16, 32], BF16, es)
        rwl = fw.sb('D_rwl', [128, 16, 32], BF16, es)
        xTb = fw.sb('D_xTb', [128, 16, 128], BF16, es)
        ss = fw.sb('D_ss', [128, 4], F32, es)
        sel = fw.sb('D_sel', [128, 2], F32, es)
        mg = fw.sb('D_mg', [128, 16], F32, es)
        rwg = fw.sb('D_rwg', [128, 16, 32], F32, es)
        rb = fw.sb('D_rb', [128, 32], F32, es)
        lg = fw.sb('D_lg', [128, 32], F32, es)
        ex = fw.sb('D_ex', [128, 32], F32, es)
        mk = fw.sb('D_mk', [128, 32], F32, es)
        t8 = fw.sb('D_t8', [128, 16], F32, es)
        gt_sb = fw.sb('D_gT', [32, 128], F32, es)
        pT = [fw.ps('D_pT%d' % i, [128, 8, 128], BF16, es) for i in range(2)]
        pM = [fw.ps('D_pM%d' % i, [128, 512], F32, es) for i in range(3)]
        pR = fw.ps('D_pR', [128, 512], F32, es)
        for i in range(4):
            fw.dma('pool', wo[:, :, i * 512:(i + 1) * 512], wout_d[:, i * 512:(i + 1) * 512].rearrange("(k p) c -> p k c", p=128), part=True)
        fw.dma('sp', sel[:], sel_d[:]); fw.dma('sp', mg[:], mg_d[:])
        fw.dma('sp', rwg[:], rw_d[:, :].rearrange("(k p) e -> p k e", p=128))
        fw.dma('sp', rb[:], rb_d[:, :].partition_broadcast(128))
        for k in range(16):
            fw.ts('dve', rwg[:, k, :], rwg[:, k, :], mg[:, k:k + 1], ALU.mult, part=True)
        fw.cp('dve', rwh[:], rwg[:])
        fw.tt('dve', rwl[:], rwg[:], rwh[:], ALU.subtract)
        for tt in range(16):
            t0 = tt * 128
            y0 = Y0[tt % 2]; y1 = Y1[tt % 2]; xo = XO[tt % 2]
            fw.dma('sp', y0[:, 0:1024], yrw_d[t0:t0 + 128, :], part=True)
            fw.dma('sp', y0[:, 1024:2048], ynsa_d[t0:t0 + 128, :], part=True)
            fw.dma('sp', y1[:, 0:1024], yrw_d[2048 + t0:2048 + t0 + 128, :], part=True)
            fw.dma('sp', y1[:, 1024:2048], ynsa_d[2048 + t0:2048 + t0 + 128, :], part=True)
            fw.dma('sp', xo[:], xown_d[t0:t0 + 128, :])
            fw.ts('pool', Yf[:], y0[:], sel[:, 0:1], ALU.mult)
            fw.I('dve', 'scalar_tensor_tensor', out=Yb[:], in0=y1[:], scalar=sel[:, 1:2], in1=Yf[:], op0=ALU.mult, op1=ALU.add)
            for hh in range(2):
                P = pT[hh]
                for j in range(8):
                    k = hh * 8 + j
                    fw.tr(P[:, j, :], Yb[:, k * 128:(k + 1) * 128], C['ident_b'])
                fw.cp('act' if hh else 'dve', yT[:, hh * 8:(hh + 1) * 8, :], P[:], part=True)
            for cb in range(4):
                P = pM[cb % 3]
                for k in range(16):
                    fw.mm(P[:], yT[:, k, :], wo[:, k, cb * 512:(cb + 1) * 512], start=(k == 0), stop=(k == 15))
                fw.tt('dve', H[:, cb * 512:(cb + 1) * 512], P[:], xo[:, cb * 512:(cb + 1) * 512], ALU.add, part=True)
            fw.dma('sp', h1_d[t0:t0 + 128, :], H[:])
            if DBG_D < 2: continue
            rms_rstd(fw, H[:], ss, junk[:])
            fw.ts('pool', XN[:], H[:], ss[:, 2:3], ALU.mult)
            fw.cp('dve', XH[:], XN[:])
            fw.tt('dve', XL[:], XN[:], XH[:], ALU.subtract)
            for (src, dstT, scaled) in ((XH, xTh, True), (XL, xTl, False)):
                for hh in range(2):
                    P = pT[hh]
                    for j in range(8):
                        k = hh * 8 + j
                        fw.tr(P[:, j, :], src[:, k * 128:(k + 1) * 128], C['ident_b'])
                    fw.cp('act', dstT[:, hh * 8:(hh + 1) * 8, :], P[:], part=True)
                    if scaled:
                        for j in range(8):
                            k = hh * 8 + j
                            fw.ts('dve', xTb[:, k, :], P[:, j, :], mg[:, k:k + 1], ALU.mult, part=True)
            fw.dma('sp', xn2T_d[:, :, t0:t0 + 128], xTb[:])
            if DBG_D < 3: continue
            n = 0
            for (a_, w_) in ((xTh, rwh), (xTl, rwh), (xTh, rwl)):
                for k in range(16):
                    fw.mm(pR[:, 0:32], a_[:, k, :], w_[:, k, :], start=(n == 0), stop=(n == 47)); n += 1
            fw.tt('dve', lg[:], pR[:, 0:32], rb[:], ALU.add)
            fw.I('dve', 'max', out=t8[:, 0:8], in_=lg[:])
            fw.ts('dve', mk[:], lg[:], t8[:, 3:4], ALU.is_ge)
            fw.ts('dve', t8[:, 8:9], t8[:, 0:1], -1.0, ALU.mult)
            fw.act(ex[:], lg[:], AF.Exp, bias=t8[:, 8:9])
            fw.tt('dve', ex[:], ex[:], mk[:], ALU.mult)
            fw.I('dve', 'tensor_reduce', out=t8[:, 9:10], in_=ex[:], axis=AX.X, op=ALU.add)
            fw.I('dve', 'reciprocal', out=t8[:, 10:11], in_=t8[:, 9:10])
            fw.ts('dve', lg[:], ex[:], t8[:, 10:11], ALU.mult)
            fw.dma('sp', gates_d[t0:t0 + 128, :], lg[:])
    fw.barrier()


def stage_E(fw, C, w1b_d, w2b_d, b1_d, b2_d, h1_d, xn2T_d, gates_d, gatesT_d, h2_d):
    TP = 512; NT = TP // 128
    with ExitStack() as es:
        xT = fw.sb('E_xT', [128, 16, TP], BF16, es)
        acc = fw.sb('E_acc', [128, NT, 2048], F32, es)
        actT = fw.sb('E_actT', [128, 16, TP], BF16, es)
        W1 = [fw.sb('E_W1%d' % i, [128, 16, 512], BF16, es) for i in range(2)]
        W2 = [fw.sb('E_W2%d' % i, [128, 16, 512], BF16, es) for i in range(2)]
        b1 = fw.sb('E_b1', [128, 32, 32], F32, es)
        b2bc = [fw.sb('E_b2bc0', [128, 2048], F32, es)] * 2
        gat = fw.sb('E_gat', [128, NT, 32], F32, es)
        h1t = [fw.sb('E_h10', [128, 2048], F32, es)] * 2
        tg = [fw.sb('E_tg%d' % i, [128, 512], F32, es) for i in range(2)]
        tsg = [fw.sb('E_ts%d' % i, [128, 512], F32, es) for i in range(2)]
        tl = [fw.sb('E_tl%d' % i, [128, 512], F32, es) for i in range(2)]
        tl2 = [fw.sb('E_tm%d' % i, [128, 512], F32, es) for i in range(2)]
        pG = [fw.ps('E_pG%d' % i, [128, 512], F32, es) for i in range(2)]
        pL = [fw.ps('E_pL%d' % i, [128, 512], F32, es) for i in range(2)]
        pY = [fw.ps('E_pY%d' % i, [128, 512], F32, es) for i in range(4)]
        fw.dma('sp', b1[:], b1_d[:])
        w1i = 0; w2i = 0; si = 0; yi = 0
        for tp in range(2048 // TP):
            T0 = tp * TP
            fw.dma('sp', xT[:], xn2T_d[:, :, T0:T0 + TP])
            fw.dma('sp', gat[:], gates_d[T0:T0 + TP, :].rearrange("(n p) e -> p n e", p=128))
            fw.I('pool', 'memset', ap=acc[:], constant=0.0)
            for e in range(NE):
                for hb in range(8):
                    W = W1[w1i % 2]; w1i += 1
                    q = 'sp' if w1i % 2 else 'act'
                    fw.dma(q, W[:, :, 0:256], w1b_d[e][:, hb * 256:(hb + 1) * 256].rearrange("(k p) c -> p k c", p=128), part=True)
                    fw.dma(q, W[:, :, 256:512], w1b_d[e][:, 2048 + hb * 256:2048 + (hb + 1) * 256].rearrange("(k p) c -> p k c", p=128), part=True)
                    for sub in range(2):
                        G = pG[si % 2]; L = pL[si % 2]
                        g_ = tg[si % 2]; s_ = tsg[si % 2]; l_ = tl[si % 2]; m_ = tl2[si % 2]; si += 1
                        for k in range(16):
                            fw.mm(G[:], W[:, k, sub * 128:(sub + 1) * 128], xT[:, k, :], start=(k == 0), stop=(k == 15))
                        for k in range(16):
                            fw.mm(L[:], W[:, k, 256 + sub * 128:256 + (sub + 1) * 128], xT[:, k, :], start=(k == 0), stop=(k == 15))
                        cg = hb * 2 + sub
                        fw.ts('dve', g_[:], G[:], b1[:, e, cg:cg + 1], ALU.add, 7.0, ALU.min)
                        fw.act(s_[:], g_[:], AF.Sigmoid, scale=1.702)
                        fw.ts('dve', l_[:], L[:], b1[:, e, 16 + cg:16 + cg + 1], ALU.add, 7.0, ALU.min)
                        fw.ts('pool', m_[:], l_[:], -7.0, ALU.max, 1.0, ALU.add)
                        fw.tt('pool', s_[:], s_[:], g_[:], ALU.mult)
                        fw.tt('dve', actT[:, cg, :], s_[:], m_[:], ALU.mult, part=True)
                bb = b2bc[e % 2]
                fw.dma('sp', bb[:], b2_d[e:e + 1, :].partition_broadcast(128))
                for tl_ in range(NT):
                    fw.I('dve', 'scalar_tensor_tensor', part=True, out=acc[:, tl_, :], in0=bb[:], scalar=gat[:, tl_, e:e + 1], in1=acc[:, tl_, :], op0=ALU.mult, op1=ALU.add)
                for fb in range(4):
                    W = W2[w2i % 2]; w2i += 1
                    q = 'sp' if w2i % 2 else 'act'
                    fw.dma(q, W[:], w2b_d[e][:, fb * 512:(fb + 1) * 512].rearrange("(k p) c -> p k c", p=128))
                    for tl_ in range(NT):
                        P = pY[yi % 4]; yi += 1
                        for k in range(16):
                            fw.mm(P[:], actT[:, k, tl_ * 128:(tl_ + 1) * 128], W[:, k, :], start=(k == 0), stop=(k == 15))
                        fw.I('dve', 'scalar_tensor_tensor', part=True, out=acc[:, tl_, fb * 512:(fb + 1) * 512], in0=P[:],
                             scalar=gat[:, tl_, e:e + 1], in1=acc[:, tl_, fb * 512:(fb + 1) * 512], op0=ALU.mult, op1=ALU.add)
            for tl_ in range(NT):
                h = h1t[tl_ % 2]
                fw.dma('sp', h[:], h1_d[T0 + tl_ * 128:T0 + (tl_ + 1) * 128, :])
                fw.tt('pool', h[:], h[:], acc[:, tl_, :], ALU.add)
                fw.dma('sp', h2_d[T0 + tl_ * 128:T0 + (tl_ + 1) * 128, :], h[:])
    fw.barrier()


def stage_F(fw, C, h2_d, pown_d, pg_d, plw_d, pgw_d, out_d):
    with ExitStack() as es:
        pgw = fw.sb('F_pgw', [128, 16, 2048], BF16, es)
        plw = fw.sb('F_plw', [128, 2, 2048], BF16, es)
        Hh = [fw.sb('F_H%d' % i, [128, 2048], F32, es) for i in range(2)]
        Pp = [fw.sb('F_P%d' % i, [128, 256], F32, es) for i in range(2)]
        Pb = fw.sb('F_Pb', [128, 256], BF16, es)
        XB = fw.sb('F_XB', [128, 2048], BF16, es)
        junk = fw.sb('F_junk', [128, 2048], BF16, es)
        xT = fw.sb('F_xT', [128, 16, 128], BF16, es)
        ppT = fw.sb('F_ppT', [128, 2, 128], BF16, es)
        gsb = [fw.sb('F_g%d' % i, [128, 512], F32, es) for i in range(2)]
        O = [fw.sb('F_O%d' % i, [128, 2048], F32, es) for i in range(2)]
        ss = fw.sb('F_ss', [128, 4], F32, es)
        pg = fw.sb('F_pg', [128, 16], F32, es)
        pT = [fw.ps('F_pT%d' % i, [128, 8, 128], BF16, es) for i in range(2)]
        pA = [fw.ps('F_pA%d' % i, [128, 512], F32, es) for i in range(2)]
        pB = [fw.ps('F_pB%d' % i, [128, 512], F32, es) for i in range(2)]
        pQ = fw.ps('F_pQ', [128, 2, 128], BF16, es)
        for i in range(4):
            fw.dma('pool', pgw[:, :, i * 512:(i + 1) * 512], pgw_d[:, i * 512:(i + 1) * 512].rearrange("(k p) c -> p k c", p=128), part=True)
        fw.dma('pool', plw[:], plw_d[:, :].rearrange("(k p) c -> p k c", p=128))
        fw.dma('sp', pg[:], pg_d[:])
        ci = 0
        for tt in range(16):
            t0 = tt * 128
            h = Hh[tt % 2]; pp = Pp[tt % 2]; o = O[tt % 2]
            fw.dma('sp', h[:], h2_d[t0:t0 + 128, :])
            fw.dma('sp', pp[:], pown_d[t0:t0 + 128, :])
            rms_rstd(fw, h[:], ss, junk[:])
            fw.ts('dve', XB[:], h[:], ss[:, 2:3], ALU.mult)
            fw.cp('dve', Pb[:], pp[:])
            for hh in range(2):
                P = pT[hh]
                for j in range(8):
                    k = hh * 8 + j
                    fw.tr(P[:, j, :], XB[:, k * 128:(k + 1) * 128], C['ident_b'])
                for j in range(8):
                    k = hh * 8 + j
                    if j % 2: fw.act(xT[:, k, :], P[:, j, :], AF.Copy, scale=pg[:, k:k + 1], part=True)
                    else: fw.ts('dve', xT[:, k, :], P[:, j, :], pg[:, k:k + 1], ALU.mult, part=True)
            for j in range(2):
                fw.tr(pQ[:, j, :], Pb[:, j * 128:(j + 1) * 128], C['ident_b'])
            fw.cp('act', ppT[:], pQ[:])
            for cb in range(4):
                A_ = pA[ci % 2]; B_ = pB[ci % 2]; g_ = gsb[ci % 2]; ci += 1
                cs = slice(cb * 512, (cb + 1) * 512)
                for k in range(16):
                    fw.mm(A_[:], xT[:, k, :], pgw[:, k, cs], start=(k == 0), stop=(k == 15))
                for k in range(2):
                    fw.mm(B_[:], ppT[:, k, :], plw[:, k, cs], start=(k == 0), stop=(k == 1))
                fw.act(g_[:], A_[:], AF.Sigmoid)
                fw.tt('dve', g_[:], g_[:], B_[:], ALU.mult)
                fw.tt('pool', o[:, cs], g_[:], h[:, cs], ALU.add, part=True)
            fw.dma('sp', out_d[t0:t0 + 128, :], o[:])
    fw.barrier()


DBG_SP = 9
CAP = 512
NST = CAP // 128


def stage_D2(fw, C, yrw_d, ynsa_d, sel_d, xown_d, wout_d, mg_d, mgrow_d, rw_d, rb_d, h1_d, xn2_d, mk_d, pos_d, gates_d, lts_d):
    with ExitStack() as es:
        wo = fw.sb('D_wo', [128, 16, 2048], BF16, es)
        Y0 = [fw.sb('D_Y0%d' % i, [128, 2048], F32, es) for i in range(2)]
        Y1 = [fw.sb('D_Y1%d' % i, [128, 2048], F32, es) for i in range(2)]
        XO = [fw.sb('D_XO%d' % i, [128, 2048], F32, es) for i in range(2)]
        Yf = fw.sb('D_Yf', [128, 2048], F32, es)
        Yb = fw.sb('D_Yb', [128, 2048], BF16, es)
        yT = fw.sb('D_yT', [128, 16, 128], BF16, es)
        H = fw.sb('D_H', [128, 2048], F32, es)
        XN = fw.sb('D_XN', [128, 2048], F32, es)
        XG = fw.sb('D_XG', [128, 2048], BF16, es)
        gbc = fw.sb('D_gbc', [128, 2048], F32, es)
        junk = fw.sb('D_junk', [128, 2048], BF16, es)
        xTh = fw.sb('D_xTh', [128, 16, 128], BF16, es); xTl = fw.sb('D_xTl', [128, 16, 128], BF16, es)
        XH = fw.sb('D_XH', [128, 2048], BF16, es); XL = fw.sb('D_XL', [128, 2048], BF16, es)
        rwh = fw.sb('D_rwh', [128, 16, 32], BF16, es); rwl = fw.sb('D_rwl', [128, 16, 32], BF16, es)
        ss = fw.sb('D_ss', [128, 4], F32, es); sel = fw.sb('D_sel', [128, 2], F32, es)
        mg = fw.sb('D_mg', [128, 16], F32, es); rwg = fw.sb('D_rwg', [128, 16, 32], F32, es)
        rb = fw.sb('D_rb', [128, 32], F32, es); lg = fw.sb('D_lg', [128, 32], F32, es)
        ex = fw.sb('D_ex', [128, 32], F32, es); mk = fw.sb('D_mk', [128, 32], F32, es)
        mkb = fw.sb('D_mkb', [128, 32], BF16, es); msum = fw.sb('D_msum', [128, 32], F32, es); msb = fw.sb('D_msb', [128, 32], BF16, es)
        pos = fw.sb('D_pos', [128, 32], F32, es)
        t8 = fw.sb('D_t8', [128, 16], F32, es)
        lts = fw.sb('D_lts', [128, 128], BF16, es); onb = fw.sb('D_onb', [128, 128], BF16, es)
        pT = [fw.ps('D_pT%d' % i, [128, 8, 128], BF16, es) for i in range(2)]
        pM = [fw.ps('D_pM%d' % i, [128, 512], F32, es) for i in range(3)]
        pR = fw.ps('D_pR', [128, 512], F32, es); pQ = fw.ps('D_pQ', [128, 512], F32, es)
        for i in range(4):
            fw.dma('pool', wo[:, :, i * 512:(i + 1) * 512], wout_d[:, i * 512:(i + 1) * 512].rearrange("(k p) c -> p k c", p=128), part=True)
        fw.dma('sp', sel[:], sel_d[:]); fw.dma('sp', mg[:], mg_d[:])
        fw.dma('sp', gbc[:], mgrow_d[:, :].partition_broadcast(128))
        fw.dma('sp', rwg[:], rw_d[:, :].rearrange("(k p) e -> p k e", p=128))
        fw.dma('sp', rb[:], rb_d[:, :].partition_broadcast(128))
        fw.dma('sp', lts[:], lts_d[:, :])
        fw.cp('dve', onb[:], C['ones'])
        fw.I('pool', 'memset', ap=msum[:], constant=0.0)
        for k in range(16):
            fw.ts('dve', rwg[:, k, :], rwg[:, k, :], mg[:, k:k + 1], ALU.mult, part=True)
        fw.cp('dve', rwh[:], rwg[:])
        fw.tt('dve', rwl[:], rwg[:], rwh[:], ALU.subtract)
        for tt in range(16):
            t0 = tt * 128
            y0 = Y0[tt % 2]; y1 = Y1[tt % 2]; xo = XO[tt % 2]
            fw.dma('sp', y0[:, 0:1024], yrw_d[t0:t0 + 128, :], part=True)
            fw.dma('sp', y0[:, 1024:2048], ynsa_d[t0:t0 + 128, :], part=True)
            fw.dma('sp', y1[:, 0:1024], yrw_d[2048 + t0:2048 + t0 + 128, :], part=True)
            fw.dma('sp', y1[:, 1024:2048], ynsa_d[2048 + t0:2048 + t0 + 128, :], part=True)
            fw.dma('sp', xo[:], xown_d[t0:t0 + 128, :])
            fw.ts('pool', Yf[:], y0[:], sel[:, 0:1], ALU.mult)
            fw.I('dve', 'scalar_tensor_tensor', out=Yb[:], in0=y1[:], scalar=sel[:, 1:2], in1=Yf[:], op0=ALU.mult, op1=ALU.add)
            for hh in range(2):
                P = pT[hh]
                for j in range(8):
                    k = hh * 8 + j
                    fw.tr(P[:, j, :], Yb[:, k * 128:(k + 1) * 128], C['ident_b'])
                fw.cp('act' if hh else 'dve', yT[:, hh * 8:(hh + 1) * 8, :], P[:], part=True)
            for cb in range(4):
                P = pM[cb % 3]
                for k in range(16):
                    fw.mm(P[:], yT[:, k, :], wo[:, k, cb * 512:(cb + 1) * 512], start=(k == 0), stop=(k == 15))
                fw.tt('dve', H[:, cb * 512:(cb + 1) * 512], P[:], xo[:, cb * 512:(cb + 1) * 512], ALU.add, part=True)
            fw.dma('sp', h1_d[t0:t0 + 128, :], H[:])
            rms_rstd(fw, H[:], ss, junk[:])
            fw.ts('pool', XN[:], H[:], ss[:, 2:3], ALU.mult)
            fw.tt('dve', XG[:], XN[:], gbc[:], ALU.mult)
            fw.dma('sp', xn2_d[t0:t0 + 128, :], XG[:])
            fw.cp('dve', XH[:], XN[:])
            fw.tt('dve', XL[:], XN[:], XH[:], ALU.subtract)
            for (src, dstT) in ((XH, xTh), (XL, xTl)):
                for hh in range(2):
                    P = pT[hh]
                    for j in range(8):
                        k = hh * 8 + j
                        fw.tr(P[:, j, :], src[:, k * 128:(k + 1) * 128], C['ident_b'])
                    fw.cp('act', dstT[:, hh * 8:(hh + 1) * 8, :], P[:], part=True)
            n = 0
            for (a_, w_) in ((xTh, rwh), (xTl, rwh), (xTh, rwl)):
                for k in range(16):
                    fw.mm(pR[:, 0:32], a_[:, k, :], w_[:, k, :], start=(n == 0), stop=(n == 47)); n += 1
            fw.tt('dve', lg[:], pR[:, 0:32], rb[:], ALU.add)
            fw.I('dve', 'max', out=t8[:, 0:8], in_=lg[:])
            fw.ts('dve', mk[:], lg[:], t8[:, 3:4], ALU.is_ge)
            fw.ts('dve', t8[:, 8:9], t8[:, 0:1], -1.0, ALU.mult)
            fw.act(ex[:], lg[:], AF.Exp, bias=t8[:, 8:9])
            fw.tt('dve', ex[:], ex[:], mk[:], ALU.mult)
            fw.I('dve', 'tensor_reduce', out=t8[:, 9:10], in_=ex[:], axis=AX.X, op=ALU.add)
            fw.I('dve', 'reciprocal', out=t8[:, 10:11], in_=t8[:, 9:10])
            fw.ts('dve', lg[:], ex[:], t8[:, 10:11], ALU.mult)
            fw.dma('sp', mk_d[t0:t0 + 128, :], mk[:])
            if DBG_SP < 1: continue
            fw.cp('dve', mkb[:], mk[:]); fw.cp('dve', msb[:], msum[:])
            fw.mm(pQ[:, 0:32], lts[:], mkb[:], start=True, stop=False)
            fw.mm(pQ[:, 0:32], onb[:], msb[:], start=False, stop=True)
            fw.cp('dve', pos[:], pQ[:, 0:32])
            fw.tt('pool', msum[:], msum[:], mk[:], ALU.add)
            fw.dma('sp', pos_d[t0:t0 + 128, :], pos[:])
            fw.dma('sp', gates_d[t0:t0 + 128, :], lg[:])
    fw.barrier()


def stage_E1(fw, C, w1b_d, w2b_d, b1_d, b2_d, xn2_d, mk_d, pos_d, iota_d, yc_d):
    with ExitStack() as es:
        xn = fw.sb('E_xn', [128, 16, 2048], BF16, es)
        Sel = fw.sb('E_Sel', [128, 16, CAP], BF16, es)
        XcT = fw.sb('E_XcT', [128, 16, CAP], BF16, es)
        actT = fw.sb('E_actT', [128, 16, CAP], BF16, es)
        W1 = [fw.sb('E_W1%d' % i, [128, 16, 256], BF16, es) for i in range(2)]
        W2 = [fw.sb('E_W2%d' % i, [128, 16, 256], BF16, es) for i in range(2)]
        b1 = fw.sb('E_b1', [128, 32, 32], F32, es)
        b2bc = fw.sb('E_b2bc', [128, 2048], F32, es)
        pos = fw.sb('E_pos', [128, 16, 32], F32, es); mk = fw.sb('E_mk', [128, 16, 32], F32, es)
        iota = fw.sb('E_iota', [128, CAP], F32, es)
        tg = [fw.sb('E_tg%d' % i, [128, CAP], F32, es) for i in range(2)]
        tsg = [fw.sb('E_ts%d' % i, [128, CAP], F32, es) for i in range(2)]
        tl = [fw.sb('E_tl%d' % i, [128, CAP], F32, es) for i in range(2)]
        tl2 = [fw.sb('E_tm%d' % i, [128, CAP], F32, es) for i in range(2)]
        Yst = [fw.sb('E_Y%d' % i, [128, 256], BF16, es) for i in range(2)]
        pC = [fw.ps('E_pC%d' % i, [128, 512], F32, es) for i in range(2)]
        pG = [fw.ps('E_pG%d' % i, [128, 512], F32, es) for i in range(2)]
        pL = [fw.ps('E_pL%d' % i, [128, 512], F32, es) for i in range(2)]
        pY = [fw.ps('E_pY%d' % i, [128, 512], F32, es) for i in range(2)]
        fw.dma('sp', b1[:], b1_d[:])
        fw.dma('sp', xn[:], xn2_d[:, :].rearrange("(n p) f -> p n f", p=128))
        fw.dma('sp', pos[:], pos_d[:, :].rearrange("(n p) e -> p n e", p=128))
        fw.dma('sp', mk[:], mk_d[:, :].rearrange("(n p) e -> p n e", p=128))
        fw.dma('sp', iota[:], iota_d[:, :])
        w1i = 0; w2i = 0; si = 0; ci = 0; yi = 0
        for e in range(NE):
            for tl_ in range(16):
                fw.ts('dve', Sel[:, tl_, :], iota[:], pos[:, tl_, e:e + 1], ALU.is_equal, mk[:, tl_, e:e + 1], ALU.mult, part=True)
            for k in range(16):
                P = pC[ci % 2]; ci += 1
                for tl_ in range(16):
                    fw.mm(P[:, 0:CAP], xn[:, tl_, k * 128:(k + 1) * 128], Sel[:, tl_, :], start=(tl_ == 0), stop=(tl_ == 15))
                fw.cp('act', XcT[:, k, :], P[:, 0:CAP], part=True)
            for hb in range(16):
                W = W1[w1i % 2]; w1i += 1
                q = 'sp' if w1i % 2 else 'act'
                fw.dma(q, W[:, :, 0:128], w1b_d[e][:, hb * 128:(hb + 1) * 128].rearrange("(k p) c -> p k c", p=128), part=True)
                fw.dma(q, W[:, :, 128:256], w1b_d[e][:, 2048 + hb * 128:2048 + (hb + 1) * 128].rearrange("(k p) c -> p k c", p=128), part=True)
                G = pG[si % 2]; L = pL[si % 2]
                g_ = tg[si % 2]; s_ = tsg[si % 2]; l_ = tl[si % 2]; m_ = tl2[si % 2]; si += 1
                for k in range(16):
                    fw.mm(G[:, 0:CAP], W[:, k, 0:128], XcT[:, k, :], start=(k == 0), stop=(k == 15))
                for k in range(16):
                    fw.mm(L[:, 0:CAP], W[:, k, 128:256], XcT[:, k, :], start=(k == 0), stop=(k == 15))
                fw.ts('dve', g_[:], G[:, 0:CAP], b1[:, e, hb:hb + 1], ALU.add, 7.0, ALU.min)
                fw.act(s_[:], g_[:], AF.Sigmoid, scale=1.702)
                fw.ts('dve', l_[:], L[:, 0:CAP], b1[:, e, 16 + hb:16 + hb + 1], ALU.add, 7.0, ALU.min)
                fw.ts('pool', m_[:], l_[:], -7.0, ALU.max, 1.0, ALU.add)
                fw.tt('pool', s_[:], s_[:], g_[:], ALU.mult)
                fw.tt('dve', actT[:, hb, :], s_[:], m_[:], ALU.mult, part=True)
            fw.dma('sp', b2bc[:], b2_d[e:e + 1, :].partition_broadcast(128))
            for fb in range(8):
                W = W2[w2i % 2]; w2i += 1
                q = 'sp' if w2i % 2 else 'act'
                fw.dma(q, W[:], w2b_d[e][:, fb * 256:(fb + 1) * 256].rearrange("(k p) c -> p k c", p=128))
                for st in range(NST):
                    P = pY[yi % 2]; Y = Yst[yi % 2]; yi += 1
                    for k in range(16):
                        fw.mm(P[:, 0:256], actT[:, k, st * 128:(st + 1) * 128], W[:, k, :], start=(k == 0), stop=(k == 15))
                    fw.tt('dve', Y[:], P[:, 0:256], b2bc[:, fb * 256:(fb + 1) * 256], ALU.add)
                    fw.dma('pool', yc_d[e * CAP + st * 128:e * CAP + (st + 1) * 128, fb * 256:(fb + 1) * 256], Y[:])
    fw.barrier()


def stage_E2(fw, C, yc_d, pos_d, gates_d, iotap_d, h1_d, h2_d):
    with ExitStack() as es:
        pb = [fw.sb('G_pb0', [128, 32, 128], F32, es)] * 2
        gb = [fw.sb('G_gb0', [128, 32, 128], F32, es)] * 2
        Dp = fw.sb('G_Dp', [128, 32, 128], F32, es); Dg = fw.sb('G_Dg', [128, 32, 128], F32, es)
        pt = [fw.sb('G_pt%d' % i, [128, 32], F32, es) for i in range(2)]; gt = [fw.sb('G_gt%d' % i, [128, 32], F32, es) for i in range(2)]
        pB = [fw.ps('G_pB%d' % i, [128, 512], F32, es) for i in range(4)]
        tmp = fw.sb('G_tmp', [128, 32, 128], F32, es)
        S = [fw.sb('G_S%d' % i, [128, 32, NST, 128], BF16, es) for i in range(2)]
        Yc = [fw.sb('G_Yc%d' % i, [128, NST, 512], BF16, es) for i in range(3)]
        h = [fw.sb('G_h%d' % i, [128, 2048], F32, es) for i in range(2)]
        iop = fw.sb('G_iop', [128, NST], F32, es)
        pA = [fw.ps('G_pA%d' % i, [128, 512], F32, es) for i in range(2)]
        fw.dma('sp', iop[:], iotap_d[:, :])
        yi = 0; ai = 0
        for tt in range(16):
            t0 = tt * 128
            P_ = pb[tt % 2]; G_ = gb[tt % 2]; S_ = S[tt % 2]; H_ = h[tt % 2]
            fw.dma('sp', pt[tt % 2][:], pos_d[t0:t0 + 128, :]); fw.dma('sp', gt[tt % 2][:], gates_d[t0:t0 + 128, :])
            bi = 0
            for (src, Dx, dst) in ((pt[tt % 2], Dp, P_), (gt[tt % 2], Dg, G_)):
                for e in range(32):
                    fw.ts('dve' if e % 2 else 'pool', Dx[:, e, :], C['ident_f'], src[:, e:e + 1], ALU.mult, part=True)
                for c in range(8):
                    B_ = pB[bi % 4]; bi += 1
                    fw.mm(B_[:], C['ones'], Dx[:, c * 4:(c + 1) * 4, :].rearrange("p e t -> p (e t)"))
                    fw.cp('act', dst[:, c * 4:(c + 1) * 4, :].rearrange("p e t -> p (e t)"), B_[:], part=True)
            fw.dma('sp', H_[:], h1_d[t0:t0 + 128, :])
            for st in range(NST):
                fw.ts('dve', tmp[:], P_[:], iop[:, st:st + 1], ALU.is_equal)
                fw.tt('dve', S_[:, :, st, :], tmp[:], G_[:], ALU.mult, part=True)
            for fb in range(4):
                A_ = pA[ai % 2]; ai += 1
                for e in range(NE):
                    Y = Yc[yi % 3]; yi += 1
                    fw.dma('sp' if yi % 2 else 'act', Y[:], yc_d[e * CAP:(e + 1) * CAP, fb * 512:(fb + 1) * 512].rearrange("(s p) f -> p s f", p=128))
                    for st in range(NST):
                        fw.mm(A_[:], S_[:, e, st, :], Y[:, st, :], start=(e == 0 and st == 0), stop=(e == NE - 1 and st == NST - 1))
                fw.tt('dve', H_[:, fb * 512:(fb + 1) * 512], H_[:, fb * 512:(fb + 1) * 512], A_[:], ALU.add, part=True)
            fw.dma('sp', h2_d[t0:t0 + 128, :], H_[:])
    fw.barrier()


BFNP = ml_dtypes.bfloat16


def _consts():
    c = {}
    c['ident_b'] = np.eye(128).astype(BFNP)
    c['ident_f'] = np.eye(128, dtype=np.float32)
    c['ones'] = np.ones((128, 128), np.float32)
    return c


def _rw_consts():
    r = np.arange(64)[:, None]; s = np.arange(64)[None, :]
    rep = lambda m: np.tile(m.astype(np.float32), (1, 8))
    return {'c_triu': (r <= s).astype(np.float32), 'c_mus': rep(r < s), 'c_mls': rep(r > s), 'c_mui': rep(r <= s), 'c_eye8': rep(r == s)}


def _rw_params(I, hg):
    sl = slice(hg * 512, (hg + 1) * 512)
    mu = I['rw_mu'][0]
    p = {}
    p['mu'] = np.concatenate([mu[0:1024][sl], mu[1024:2048][sl], mu[2048:3072][sl], mu[3072:3360]])[None, :]
    p['w0'] = I['rw_w0'][0][sl][None]; p['a0'] = I['rw_a0'][0][sl][None]; p['kkp'] = I['rw_k_k'][0][sl][None]
    p['kap'] = I['rw_k_a'][0][sl][None]; p['rkp'] = I['rw_r_k'][0].reshape(-1)[sl][None]
    p['lnw'] = I['rw_lnx_w'][0][sl][None]; p['lnb'] = I['rw_lnx_b'][0][sl][None]
    p['w2'] = I['rw_w2'][0][:, sl]; p['a2'] = I['rw_a2'][0][:, sl]; p['g2'] = I['rw_g2'][0][:, sl]
    return {k: np.ascontiguousarray(v, dtype=np.float32) for k, v in p.items()}


def _nsa_consts():
    c = {}
    i = np.arange(32)[:, None, None, None]; nl = np.arange(128)[None, :, None, None]; cc = np.arange(2)[None, None, :, None]; q = np.arange(128)[None, None, None, :]
    c['cmask'] = ((16 * (128 * cc + nl) + 31) <= (128 * i + q)).astype(BFNP)
    i = np.arange(32)[:, None, None]; q = np.arange(128)[None, :, None]; j = np.arange(64)[None, None, :]
    qblk = (128 * i + q) // 64
    forced = (j == 0) | (j == qblk) | (j == qblk - 1); fut = j > qblk
    c['keep'] = (~(forced | fut)).astype(np.float32)
    c['addm'] = np.where(fut, -1e30, np.where(forced, 1e30, 0.0)).astype(np.float32)
    j = np.arange(64)[:, None, None]; kt = np.arange(32)[None, :, None]; k = np.arange(128)[None, None, :]
    c['selx'] = (j == 2 * kt + k // 64).astype(BFNP)
    kl = np.arange(128)[:, None]; ql = np.arange(128)[None, :]
    c['cauT'] = (kl <= ql).astype(BFNP); c['acauT'] = (kl > ql).astype(BFNP)
    n = np.arange(256)[:, None]; j = np.arange(64)[None, :]
    ov = ((16 * n < 64 * j + 64) & (16 * n + 32 > 64 * j) & (n < 255)).astype(BFNP)
    c['ovl'] = np.ascontiguousarray(np.tile(ov[:, None, :], (1, 4, 1)))
    return c


def _nsa_params(I):
    p = {}
    p['qnw'] = np.tile(I['nsa_q_norm'][0], 16)[None, :]
    kn = I['nsa_k_norm'][0]
    p['knw'] = np.concatenate([np.tile(kn[1], 4), np.tile(kn[2], 4)])[None, :]
    p['kn0'] = kn[0][:, None]
    p['b2k'] = I['cmp_k_b2'][0][:, None]; p['b2v'] = I['cmp_v_b2'][0][None, :]
    for kv, nm in (('k', 'cmp_k'), ('v', 'cmp_v')):
        p['w1' + kv] = I[nm + '_w1'][0]; p['w2' + kv] = I[nm + '_w2'][0]
        p['b1' + kv] = I[nm + '_b1'][0].reshape(2, 128).T
    p['posTk'] = I['cmp_pos_k'][0].T; p['posTv'] = I['cmp_pos_v'][0].T
    return {k: np.ascontiguousarray(v, dtype=np.float32) for k, v in p.items()}


def _shared_inputs(I):
    pa = lambda v: np.ascontiguousarray(v.reshape(16, 128).T, dtype=np.float32)
    m = {}
    for k, v in _consts().items(): m['c_' + k] = v
    rc = _rw_consts()
    for hg in range(2):
        for k, v in _rw_params(I, hg).items(): m['rw%d_%s' % (hg, k)] = v
        for k, v in rc.items(): m['rw%d_%s' % (hg, k)] = v
    for k, v in _nsa_consts().items(): m['n_' + k] = v
    for k, v in _nsa_params(I).items(): m['n_' + k] = v
    m['w_in'] = np.ascontiguousarray(I['w_in'][0]); m['mix_g'] = pa(I['mix_norm_g'][0])
    m['w_out'] = np.ascontiguousarray(I['w_out'][0]); m['moe_g'] = pa(I['moe_norm_g'][0])
    m['router_w'] = np.ascontiguousarray(I['router_w'][0]); m['router_b'] = np.ascontiguousarray(I['router_b'].reshape(1, 32))
    m['moe_w1'] = np.ascontiguousarray(I['moe_w1'][0]); m['moe_w2'] = np.ascontiguousarray(I['moe_w2'][0])
    m['moe_b1'] = np.ascontiguousarray(I['moe_b1'][0].reshape(32, 32, 128).transpose(2, 0, 1)); m['moe_b2'] = np.ascontiguousarray(I['moe_b2'][0])
    m['moe_g_row'] = np.ascontiguousarray(I['moe_norm_g'][0][None, :], dtype=np.float32)
    m['c_lts'] = (np.arange(128)[:, None] < np.arange(128)[None, :]).astype(BFNP)
    m['c_iota'] = np.tile(np.arange(CAP, dtype=np.float32)[None, :], (128, 1))
    m['c_iotap'] = np.ascontiguousarray(np.arange(128, dtype=np.float32)[:, None] + 128.0 * np.arange(NST, dtype=np.float32)[None, :])
    m['ple_g'] = pa(I['ple_norm_g'][0]); m['ple_w'] = np.ascontiguousarray(I['ple_w'][0]); m['ple_gate_w'] = np.ascontiguousarray(I['ple_gate_w'][0])
    return m


def build_program(shared):
    nc = bass.Bass("TRN2", target_bir_lowering=False)
    with ExitStack() as es:
        fw = FW(nc, es)
        def EI(n, shape=None, dt=None):
            v = shared.get(n)
            if shape is None: shape = list(v.shape)
            if dt is None: dt = BF16 if (v is not None and v.dtype == BFNP) else F32
            return fw.dram(n, shape, dt, kind="ExternalInput")
        x_d = EI("x_full", [4096, 2048], F32); xown_d = EI("x_own", [2048, 2048], F32)
        pown_d = EI("p_own", [2048, 256], F32); sel_d = EI("sel", [128, 2], F32)
        D_ = {k: EI(k) for k in shared}
        out_d = fw.dram("out", [2048, 2048], F32, kind="ExternalOutput")
        u_d = fw.dram("u_d", [4096, IN_W], F32)
        yrw_d = fw.dram("yrw_d", [4096, 1024], F32); ynsa_d = fw.dram("ynsa_d", [4096, 1024], F32)
        h1_d = fw.dram("h1_d", [2048, 2048], F32); h2_d = fw.dram("h2_d", [2048, 2048], F32)
        gates_d = fw.dram("gates_d", [2048, 32], F32)
        xn2_d = fw.dram("xn2_d", [2048, 2048], BF16); mk_d = fw.dram("mk_d", [2048, 32], F32); pos_d = fw.dram("pos_d", [2048, 32], F32)
        yc_d = fw.dram("yc_d", [32 * CAP, 2048], BF16)
        w1b_d = [fw.dram("w1b_%d" % e, [2048, 4096], BF16) for e in range(32)]
        w2b_d = [fw.dram("w2b_%d" % e, [2048, 2048], BF16) for e in range(32)]
        C = {}
        for k, dt in (('ident_b', BF16), ('ident_f', F32), ('ones', F32)):
            t = fw.sb('k_' + k, [128, 128], dt); fw.dma('sp', t[:], D_['c_' + k][:, :]); C[k] = t[:]
        stage_A(fw, C, x_d, D_['w_in'], D_['mix_g'], u_d)
        conv = conv_weights(fw, D_['moe_w1'], D_['moe_w2'], w1b_d, w2b_d)
        for hg in range(2):
            P_ = {k[4:]: v for k, v in D_.items() if k.startswith('rw%d_' % hg)}
            stage_B(fw, C, u_d, P_, yrw_d, hg, conv if hg == 0 else None)
        for _ in conv: pass
        Pn = {k[2:]: v for k, v in D_.items() if k.startswith('n_')}
        stage_C(fw, C, u_d, Pn, ynsa_d)
        stage_D2(fw, C, yrw_d, ynsa_d, sel_d, xown_d, D_['w_out'], D_['moe_g'], D_['moe_g_row'], D_['router_w'], D_['router_b'], h1_d, xn2_d, mk_d, pos_d, gates_d, D_['c_lts'])
        stage_E1(fw, C, w1b_d, w2b_d, D_['moe_b1'], D_['moe_b2'], xn2_d, mk_d, pos_d, D_['c_iota'], yc_d)
        stage_E2(fw, C, yc_d, pos_d, gates_d, D_['c_iotap'], h1_d, h2_d)
        stage_F(fw, C, h2_d, pown_d, D_['ple_g'], D_['ple_w'], D_['ple_gate_w'], out_d)
        fw.emit()
    return nc


def kernel(**inputs):
    I = {k: np.asarray(v) for k, v in inputs.items()}
    shared = _shared_inputs(I)
    nc = build_program(shared)
    x = I['x']; p = I['p']
    in_maps = []
    for c in range(8):
        b, s = c // 2, c % 2
        m = dict(shared)
        m['x_full'] = np.ascontiguousarray(x[b], dtype=np.float32)
        m['x_own'] = np.ascontiguousarray(x[b, s * 2048:(s + 1) * 2048], dtype=np.float32)
        m['p_own'] = np.ascontiguousarray(p[0, b, s * 2048:(s + 1) * 2048], dtype=np.float32)
        sel = np.zeros((128, 2), np.float32); sel[:, s] = 1.0
        m['sel'] = sel
        in_maps.append(m)
    res = run_bass_kernel_spmd(nc, in_maps, core_ids=list(range(8)))
    out = np.empty((4, 4096, 2048), np.float32)
    for c in range(8):
        b, s = c // 2, c % 2
        out[b, s * 2048:(s + 1) * 2048] = np.asarray(res.results[c]['out'], dtype=np.float32)
    return out
```

```python
from contextlib import ExitStack
import numpy as np
import ml_dtypes
import concourse.bass as bass
import concourse.mybir as mybir
from concourse.bass_utils import run_bass_kernel_spmd

F32 = mybir.dt.float32
BF16 = mybir.dt.bfloat16
ALU = mybir.AluOpType
AF = mybir.ActivationFunctionType
AX = mybir.AxisListType

COMPUTE = ('pe', 'act', 'dve', 'pool')
QUEUES = ('sp', 'act', 'pool')
NDSEM = 20
WRITE_KEYS = ('out', 'ap', 'accum_out')


class T:
    def __init__(self, h, name):
        self.h = h; self.name = name
        self.lw = []; self.rd = []; self.prd = []

    def __getitem__(self, k):
        return A(self.h[k], self)


class A:
    def __init__(self, ap, t):
        self.ap = ap; self.t = t

    def __getitem__(self, k): return A(self.ap[k], self.t)
    def unsqueeze(self, i): return A(self.ap.unsqueeze(i), self.t)
    def to_broadcast(self, s): return A(self.ap.to_broadcast(list(s)), self.t)
    def rearrange(self, pat, **kw): return A(self.ap.rearrange(pat, **kw), self.t)
    def partition_broadcast(self, n): return A(self.ap.partition_broadcast(n), self.t)
    def bc(self, s): return A(self.ap.to_broadcast(list(s)), self.t)


class FW:
    def __init__(self, nc, es):
        self.nc = nc; self.es = es
        self.eng = {'pe': nc.tensor, 'act': nc.scalar, 'dve': nc.vector, 'pool': nc.gpsimd, 'sp': nc.sync}
        self.prog = {e: [] for e in self.eng}
        self.sem = {}; self.cnt = {}
        for e in COMPUTE:
            self.sem[e] = es.enter_context(nc.semaphore("s_" + e)); self.cnt[e] = 0
        self.dsem = {}; self.dcnt = {}; self.dnext = {}
        for q in QUEUES:
            self.dsem[q] = [es.enter_context(nc.semaphore("d_%s_%d" % (q, i))) for i in range(NDSEM)]
            self.dcnt[q] = [0] * NDSEM; self.dnext[q] = 0
        self.waited = {e: {} for e in self.eng}
        self.tiles = []; self.n_inst = 0

    def sb(self, name, shape, dtype=F32, es=None):
        h = (es or self.es).enter_context(self.nc.sbuf_tensor(name, list(shape), dtype))
        t = T(h, name); self.tiles.append(t); return t

    def ps(self, name, shape, dtype=F32, es=None):
        h = (es or self.es).enter_context(self.nc.psum_tensor(name, list(shape), dtype))
        t = T(h, name); self.tiles.append(t); return t

    def dram(self, name, shape, dtype=F32, kind="Internal"):
        h = self.nc.dram_tensor(name, list(shape), dtype, kind=kind).ap()
        t = T(h, name); self.tiles.append(t); return t

    def _collect(self, e, reads, writes, dma=False):
        own = None if dma else self.sem.get(e)
        waits = {}
        def add(tok, raw):
            s, v = tok
            if s is own and not raw: return
            k = id(s)
            if self.waited[e].get(k, 0) >= v: return
            if k not in waits or waits[k][1] < v: waits[k] = (s, v)
        for t in reads:
            for tok in t.lw: add(tok, True)
        for t in writes:
            for tok in t.rd: add(tok, False)
            for tok in t.prd: add(tok, False)
            for tok in t.lw: add(tok, False)
        return list(waits.values())

    @staticmethod
    def _compact(toks):
        best = {}
        for s, v in toks:
            k = id(s)
            if k not in best or best[k][1] < v: best[k] = (s, v)
        return list(best.values())

    def _update(self, tok, reads, writes, part):
        for t in reads:
            t.rd.append(tok)
            if len(t.rd) > 16: t.rd = self._compact(t.rd)
        for t in writes:
            if part and not t.rd:
                t.lw.append(tok)
                if len(t.lw) > 16: t.lw = self._compact(t.lw)
            else:
                t.prd = t.rd; t.rd = []; t.lw = [tok]

    def op(self, e, fn, reads=(), writes=(), part=False):
        waits = self._collect(e, reads, writes)
        for s, v in waits: self.waited[e][id(s)] = v
        sem = self.sem[e]; self.cnt[e] += 1
        tok = (sem, self.cnt[e])
        self.prog[e].append((waits, fn, sem, 1))
        self._update(tok, reads, writes, part)
        self.n_inst += 1
        return tok

    def I(self, e, meth, part=False, **kw):
        reads = []; writes = []; args = {}
        for k, v in kw.items():
            if isinstance(v, A):
                args[k] = v.ap
                if k in WRITE_KEYS:
                    writes.append(v.t)
                    if k == 'accum_out': reads.append(v.t)
                else:
                    reads.append(v.t)
            else:
                args[k] = v
        fn = lambda eng, m=meth, a=args: getattr(eng, m)(**a)
        return self.op(e, fn, reads, writes, part)

    def dma(self, q, out, in_, part=False, **kw):
        reads = [in_.t]; writes = [out.t]
        waits = self._collect(q, reads, writes, dma=True)
        i = self.dnext[q]; self.dnext[q] = (i + 1) % NDSEM
        s = self.dsem[q][i]
        if self.dcnt[q][i] > self.waited[q].get(id(s), 0):
            waits.append((s, self.dcnt[q][i]))
        for ss, v in waits:
            self.waited[q][id(ss)] = max(self.waited[q].get(id(ss), 0), v)
        self.dcnt[q][i] += 16
        tok = (s, self.dcnt[q][i])
        fn = lambda eng, o=out.ap, a=in_.ap, kw=kw: eng.dma_start(out=o, in_=a, **kw)
        self.prog[q].append((waits, fn, s, 16))
        self._update(tok, reads, writes, part)
        self.n_inst += 1
        return tok

    def barrier(self):
        targets = []
        for e in COMPUTE:
            if self.cnt[e] > 0: targets.append((self.sem[e], self.cnt[e]))
        for q in QUEUES:
            for i in range(NDSEM):
                if self.dcnt[q][i] > 0: targets.append((self.dsem[q][i], self.dcnt[q][i]))
        for e in self.eng:
            w = []
            for s, v in targets:
                if s is self.sem.get(e): continue
                if self.waited[e].get(id(s), 0) < v:
                    w.append((s, v)); self.waited[e][id(s)] = v
            if w: self.prog[e].append((w, None, None, 0))
        for t in self.tiles:
            t.lw = []; t.rd = []; t.prd = []

    def emit(self):
        with self.nc.Block() as block:
            def run(name):
                def body(eng):
                    for waits, fn, sem, inc in self.prog[name]:
                        for s, v in waits: eng.wait_ge(s, v)
                        if fn is not None: fn(eng).then_inc(sem, inc)
                return body
            block.tensor(run('pe')); block.scalar(run('act')); block.vector(run('dve'))
            block.gpsimd(run('pool')); block.sync(run('sp'))

    def mm(self, out, lhsT, rhs, start=True, stop=True):
        return self.I('pe', 'matmul', part=True, out=out, lhsT=lhsT, rhs=rhs, start=start, stop=stop)

    def tr(self, out, in_, identity):
        return self.I('pe', 'transpose', part=True, out=out, in_=in_, identity=identity)

    def tt(self, e, out, in0, in1, op, part=False):
        return self.I(e, 'tensor_tensor', part=part, out=out, in0=in0, in1=in1, op=op)

    def ts(self, e, out, in0, s1, op0, s2=None, op1=None, part=False):
        if op1 is None:
            return self.I(e, 'tensor_scalar', part=part, out=out, in0=in0, scalar1=s1, scalar2=None, op0=op0)
        return self.I(e, 'tensor_scalar', part=part, out=out, in0=in0, scalar1=s1, scalar2=s2, op0=op0, op1=op1)

    def cp(self, e, out, in_, part=False):
        if e == 'act':
            return self.I('act', 'activation', part=part, out=out, in_=in_, func=AF.Copy)
        return self.I(e, 'tensor_copy', part=part, out=out, in_=in_)

    def act(self, out, in_, func, part=False, **kw):
        return self.I('act', 'activation', part=part, out=out, in_=in_, func=func, **kw)


D = 2048; T_SEQ = 4096; IN_W = 5968
RMS_EPS = 1e-6


def rms_rstd(fw, xt, ss, junk, eps=RMS_EPS, d=D):
    fw.I('pool', 'memset', ap=ss[:, 0:1], constant=0.0)
    fw.act(junk, xt, AF.Square, accum_out=ss[:, 0:1])
    fw.act(ss[:, 1:2], ss[:, 0:1], AF.Sqrt, bias=eps, scale=1.0 / d)
    fw.I('dve', 'reciprocal', out=ss[:, 2:3], in_=ss[:, 1:2])


def stage_A(fw, C, x_d, w_in_d, g_d, u_d):
    with ExitStack() as es:
        xnT = fw.sb('A_xnT', [128, 16, 2048], BF16, es)
        wt = [fw.sb('A_wt%d' % i, [128, 16, 512], BF16, es) for i in range(2)]
        xt = [fw.sb('A_xt%d' % i, [128, 2048], F32, es) for i in range(2)]
        xb = [fw.sb('A_xb%d' % i, [128, 2048], BF16, es) for i in range(2)]
        junk = fw.sb('A_junk', [128, 2048], BF16, es)
        ss = [fw.sb('A_ss%d' % i, [128, 4], F32, es) for i in range(2)]
        ost = [fw.sb('A_ost%d' % i, [128, 512], F32, es) for i in range(4)]
        g_sb = fw.sb('A_g', [128, 16], F32, es)
        pT = [fw.ps('A_pT%d' % i, [128, 8, 128], BF16, es) for i in range(2)]
        pM = [fw.ps('A_pM%d' % i, [128, 512], F32, es) for i in range(4)]
        fw.dma('sp', g_sb[:], g_d[:])
        nblk = (IN_W + 511) // 512
        wi = 0; oi = 0
        for half in range(2):
            for tt in range(16):
                t0 = half * 2048 + tt * 128
                X = xt[tt % 2]; XB = xb[tt % 2]; S = ss[tt % 2]
                fw.dma('sp', X[:], x_d[t0:t0 + 128, :])
                rms_rstd(fw, X[:], S, junk[:])
                fw.ts('dve', XB[:], X[:], S[:, 2:3], ALU.mult)
                for hh in range(2):
                    P = pT[hh]
                    for j in range(8):
                        k = hh * 8 + j
                        fw.tr(P[:, j, :], XB[:, k * 128:(k + 1) * 128], C['ident_b'])
                    for j in range(8):
                        k = hh * 8 + j
                        fw.ts('dve' if j % 2 else 'pool_', xnT[:, k, tt * 128:(tt + 1) * 128], P[:, j, :], g_sb[:, k:k + 1], ALU.mult, part=True) if False else \
                            fw.act(xnT[:, k, tt * 128:(tt + 1) * 128], P[:, j, :], AF.Copy, scale=g_sb[:, k:k + 1], part=True) if j % 2 else \
                            fw.ts('dve', xnT[:, k, tt * 128:(tt + 1) * 128], P[:, j, :], g_sb[:, k:k + 1], ALU.mult, part=True)
            for cb in range(nblk):
                c0 = cb * 512; cw = min(512, IN_W - c0)
                W = wt[wi % 2]; wi += 1
                fw.dma('pool', W[:, :, 0:cw], w_in_d[:, c0:c0 + cw].rearrange("(k p) c -> p k c", p=128))
                for tt in range(16):
                    t0 = half * 2048 + tt * 128
                    P = pM[oi % 4]; O = ost[oi % 4]
                    for k in range(16):
                        fw.mm(P[:, 0:cw], xnT[:, k, tt * 128:(tt + 1) * 128], W[:, k, 0:cw], start=(k == 0), stop=(k == 15))
                    fw.cp('act' if oi % 2 else 'dve', O[:, 0:cw], P[:, 0:cw])
                    fw.dma('sp', u_d[t0:t0 + 128, c0:c0 + cw], O[:, 0:cw])
                    oi += 1
    fw.barrier()


RWC = 3360


def stage_B(fw, C, u_d, P_, yrw_d, hg, conv=None):
    NCH = 64; HW = 512
    c_r = hg * 512; c_k = 1024 + hg * 512; c_v = 2048 + hg * 512
    with ExitStack() as es:
        sb = lambda n, s, dt=F32: fw.sb('B%d_%s' % (hg, n), s, dt, es)
        U = sb('U', [64, 1824]); Us = sb('Us', [64, 1824]); mu = sb('mu', [64, 1824])
        bc = {n: sb(n, [64, HW]) for n in ('w0', 'a0', 'kkp', 'kap', 'rkp', 'lnw', 'lnb')}
        w2 = sb('w2', [64, HW]); a2 = sb('a2', [64, HW]); g2a = sb('g2a', [128, HW]); g2b = sb('g2b', [32, HW])
        names = ['logw', 'a', 'g', 'kk', 'km', 'b', 'W', 'Winv', 'Wprev', 'WCb', 'bt', 'kt', 't1', 't2', 'Y', 'yn']
        X = {n: sb(n, [64, HW]) for n in names}
        Vt = [sb('V%d' % i, [64, HW]) for i in range(2)]
        Bh = [sb('Bh%d' % i, [64, HW]) for i in range(2)]
        Kh = [sb('Kh%d' % i, [64, HW]) for i in range(2)]
        at = sb('at', [64, HW]); rt = sb('rt', [64, HW])
        aT = [sb('aT%d' % i, [64, HW]) for i in range(2)]; rT = [sb('rT%d' % i, [64, HW]) for i in range(2)]
        bT = sb('bT', [64, HW]); kT = sb('kT', [64, HW])
        Aab = sb('Aab', [64, HW]); AabT = sb('AabT', [64, HW])
        Aak = [sb('Aak%d' % i, [64, HW]) for i in range(2)]
        Abr = [sb('Abr%d' % i, [64, HW]) for i in range(2)]
        Akr = [sb('Akr%d' % i, [64, HW]) for i in range(2)]
        Tm = [sb('Tm%d' % i, [64, HW]) for i in range(2)]
        Pm = [sb('Pm%d' % i, [64, HW]) for i in range(2)]; PTm = [sb('PTm%d' % i, [64, HW]) for i in range(2)]
        XT = sb('XT', [64, HW]); SAT = sb('SAT', [64, HW]); ST = sb('ST', [64, HW])
        txw = sb('txw', [64, 64]); xaT = sb('xaT', [64, 64]); sg1 = sb('sg1', [128, 64]); sg2 = sb('sg2', [32, 64])
        sm = sb('sm', [64, 64]); wcc = [sb('wcc%d' % i, [64, 8]) for i in range(2)]
        pp = [fw.ps('B%d_p%d' % (hg, i), [128, 512], F32, es) for i in range(4)]
        pX = fw.ps('B%d_pX' % hg, [128, 512], F32, es); pS = fw.ps('B%d_pS' % hg, [128, 512], F32, es)
        pY = fw.ps('B%d_pY' % hg, [128, 512], F32, es); pN = fw.ps('B%d_pN' % hg, [128, 512], F32, es)
        pi = [0]
        def bank():
            pi[0] += 1; return pp[pi[0] % 4]
        I64 = C['ident_f'][0:64, 0:64]; ones = C['ones'][0:64, 0:64]
        triu_t = sb('triu', [64, 64]); fw.dma('sp', triu_t[:], P_['c_triu'][:, :]); tri = triu_t[:]
        mk_t = {}
        for n_ in ('mus', 'mls', 'mui', 'eye8'):
            mk_t[n_] = sb('m_' + n_, [64, HW]); fw.dma('sp', mk_t[n_][:], P_['c_' + n_][:, :])
        MUs = mk_t['mus'][:]; MLs = mk_t['mls'][:]; MUi = mk_t['mui'][:]; EYE = mk_t['eye8'][:]
        fw.dma('sp', mu[:], P_['mu'][:, :].partition_broadcast(64))
        for n in bc: fw.dma('sp', bc[n][:], P_[n][:, :].partition_broadcast(64))
        fw.dma('sp', w2[:], P_['w2'][:, :]); fw.dma('sp', a2[:], P_['a2'][:, :])
        fw.dma('sp', g2a[:], P_['g2'][0:128, :]); fw.dma('sp', g2b[:], P_['g2'][128:160, :])
        fw.I('pool', 'memset', ap=ST[:], constant=0.0)
        cols = ((c_r, 0), (c_k, 512), (c_v, 1024))
        for c in range(NCH):
            t0 = c * 64; d = c % 2
            if conv is not None:
                for _ in range(3): next(conv, None)
            for (cs, o) in cols:
                fw.dma('sp', U[:, o:o + 512], u_d[t0:t0 + 64, cs:cs + 512], part=True)
            fw.dma('sp', U[:, 1536:1824], u_d[t0:t0 + 64, 3072:3360], part=True)
            if c == 0:
                fw.I('pool', 'memset', ap=Us[:], constant=0.0)
                for (cs, o) in cols:
                    fw.dma('sp', Us[1:64, o:o + 512], u_d[0:63, cs:cs + 512], part=True)
                fw.dma('sp', Us[1:64, 1536:1824], u_d[0:63, 3072:3360], part=True)
            else:
                for (cs, o) in cols:
                    fw.dma('sp', Us[:, o:o + 512], u_d[t0 - 1:t0 + 63, cs:cs + 512], part=True)
                fw.dma('sp', Us[:, 1536:1824], u_d[t0 - 1:t0 + 63, 3072:3360], part=True)
            fw.tt('pool', Us[:], Us[:], U[:], ALU.subtract)
            fw.tt('pool', Us[:], Us[:], mu[:], ALU.mult)
            fw.tt('dve', U[:], U[:], Us[:], ALU.add)
            r = U[:, 0:512]; k = U[:, 512:1024]
            V = Vt[d]
            fw.cp('pool', V[:], U[:, 1024:1536])
            P1 = bank()
            fw.mm(P1[0:64, 0:64], U[:, 1536:1600], I64)
            fw.mm(P1[0:64, 64:128], U[:, 1600:1664], I64)
            fw.mm(P1[0:128, 128:192], U[:, 1664:1792], I64)
            fw.mm(P1[0:32, 192:256], U[:, 1792:1824], I64)
            fw.act(txw[:], P1[0:64, 0:64], AF.Tanh)
            fw.cp('dve', xaT[:], P1[0:64, 64:128])
            fw.act(sg1[:], P1[0:128, 128:192], AF.Sigmoid)
            fw.act(sg2[:], P1[0:32, 192:256], AF.Sigmoid)
            Pz = bank(); fw.mm(Pz[0:64, :], txw[:], w2[:])
            fw.tt('dve', X['t1'][:], Pz[0:64, :], bc['w0'][:], ALU.add)
            fw.act(X['t2'][:], X['t1'][:], AF.Sigmoid)
            fw.ts('pool', X['logw'][:], X['t2'][:], -0.6065306597126334, ALU.mult)
            Pa = bank(); fw.mm(Pa[0:64, :], xaT[:], a2[:])
            fw.tt('dve', X['t1'][:], Pa[0:64, :], bc['a0'][:], ALU.add)
            fw.act(X['a'][:], X['t1'][:], AF.Sigmoid)
            Pg = bank(); fw.mm(Pg[0:64, :], sg1[:], g2a[:], start=True, stop=False); fw.mm(Pg[0:64, :], sg2[:], g2b[:], start=False, stop=True)
            fw.cp('act', X['g'][:], Pg[0:64, :])
            fw.tt('pool', X['kk'][:], k, bc['kkp'][:], ALU.mult)
            fw.tt('pool', X['t1'][:], X['kk'][:], X['kk'][:], ALU.mult)
            fw.I('dve', 'tensor_reduce', out=sm[:, 0:8], in_=X['t1'][:].rearrange("p (h j) -> p h j", h=8), axis=AX.X, op=ALU.add)
            fw.act(sm[:, 8:16], sm[:, 0:8], AF.Sqrt)
            fw.ts('dve', sm[:, 8:16], sm[:, 8:16], 1e-12, ALU.max)
            fw.I('dve', 'reciprocal', out=sm[:, 16:24], in_=sm[:, 8:16])
            for h in range(8):
                fw.ts('dve', X['kk'][:, h * 64:(h + 1) * 64], X['kk'][:, h * 64:(h + 1) * 64], sm[:, 16 + h:17 + h], ALU.mult, part=True)
            fw.ts('pool', X['t1'][:], X['a'][:], -1.0, ALU.add)
            fw.tt('pool', X['t1'][:], X['t1'][:], bc['kap'][:], ALU.mult)
            fw.ts('pool', X['t1'][:], X['t1'][:], 1.0, ALU.add)
            fw.tt('dve', X['km'][:], k, X['t1'][:], ALU.mult)
            fw.tt('pool', X['b'][:], X['kk'][:], X['a'][:], ALU.mult)
            Pc = bank(); fw.mm(Pc[0:64, :], tri, X['logw'][:])
            fw.act(X['W'][:], Pc[0:64, :], AF.Exp)
            fw.act(X['Winv'][:], Pc[0:64, :], AF.Exp, scale=-1.0)
            fw.tt('dve', X['t1'][:], Pc[0:64, :], X['logw'][:], ALU.subtract)
            fw.act(X['Wprev'][:], X['t1'][:], AF.Exp)
            Pt = bank(); fw.mm(Pt[0:64, :], ones, X['logw'][:])
            fw.act(X['WCb'][:], Pt[0:64, :], AF.Exp)
            Pw = bank()
            for h in range(8):
                fw.mm(Pw[0:64, h:h + 1], X['logw'][:, h * 64:(h + 1) * 64], ones[:, 0:1])
            fw.act(wcc[d][:], Pw[0:64, 0:8], AF.Exp)
            fw.I('dve', 'scalar_tensor_tensor', out=at[:], in0=X['kk'][:], scalar=-1.0, in1=X['Wprev'][:], op0=ALU.mult, op1=ALU.mult)
            fw.tt('pool', X['bt'][:], X['b'][:], X['Winv'][:], ALU.mult)
            fw.tt('pool', X['kt'][:], X['km'][:], X['Winv'][:], ALU.mult)
            fw.tt('pool', rt[:], r, X['W'][:], ALU.mult)
            fw.tt('pool', Bh[d][:], X['bt'][:], X['WCb'][:], ALU.mult)
            fw.tt('pool', Kh[d][:], X['kt'][:], X['WCb'][:], ALU.mult)
            for (src, dst, e_) in ((at, aT[d], 'act'), (X['bt'], bT, 'dve'), (X['kt'], kT, 'act'), (rt, rT[d], 'dve')):
                Pq = bank()
                for h in range(8):
                    fw.mm(Pq[0:64, h * 64:(h + 1) * 64], src[:, h * 64:(h + 1) * 64], I64)
                fw.cp(e_, dst[:], Pq[0:64, :])
            for (l_, r_, dst, msk) in ((bT, aT[d], Aab, MUs), (aT[d], bT, AabT, MLs), (kT, aT[d], Aak[d], MUs), (bT, rT[d], Abr[d], MUi), (kT, rT[d], Akr[d], MUi)):
                Pq = bank()
                for h in range(8):
                    fw.mm(Pq[0:64, h * 64:(h + 1) * 64], l_[:, h * 64:(h + 1) * 64], r_[:, h * 64:(h + 1) * 64])
                fw.tt('dve', dst[:], Pq[0:64, :], msk, ALU.mult)
            T_ = Tm[d]
            fw.tt('pool', T_[:], Aab[:], EYE, ALU.add)
            Pc_, PTc_ = Aab, AabT
            for lvl in range(5):
                Pn, PTn = Pm[lvl % 2], PTm[lvl % 2]
                Pq = bank()
                for h in range(8):
                    hs = slice(h * 64, (h + 1) * 64)
                    fw.mm(Pq[0:64, hs], Pc_[:, hs], PTc_[:, hs])
                fw.cp('act', PTn[:], Pq[0:64, :])
                if lvl < 4:
                    Pq2 = bank()
                    for h in range(8):
                        hs = slice(h * 64, (h + 1) * 64)
                        fw.mm(Pq2[0:64, hs], PTc_[:, hs], Pc_[:, hs])
                    fw.cp('dve', Pn[:], Pq2[0:64, :])
                Pq3 = bank()
                for h in range(8):
                    hs = slice(h * 64, (h + 1) * 64)
                    fw.mm(Pq3[0:64, hs], PTn[:, hs], T_[:, hs])
                fw.tt('dve', T_[:], T_[:], Pq3[0:64, :], ALU.add)
                Pc_, PTc_ = Pn, PTn
            for h in range(8):
                hs = slice(h * 64, (h + 1) * 64)
                fw.mm(pX[0:64, hs], aT[d][:, hs], ST[:, hs], start=True, stop=False)
                fw.mm(pX[0:64, hs], Aak[d][:, hs], V[:, hs], start=False, stop=True)
            fw.cp('act', XT[:], pX[0:64, :])
            for h in range(8):
                hs = slice(h * 64, (h + 1) * 64)
                fw.mm(pS[0:64, hs], T_[:, hs], XT[:, hs])
            fw.cp('dve', SAT[:], pS[0:64, :])
            for h in range(8):
                hs = slice(h * 64, (h + 1) * 64)
                fw.mm(pY[0:64, hs], rT[d][:, hs], ST[:, hs], start=True, stop=False)
                fw.mm(pY[0:64, hs], Abr[d][:, hs], SAT[:, hs], start=False, stop=False)
                fw.mm(pY[0:64, hs], Akr[d][:, hs], V[:, hs], start=False, stop=True)
            fw.cp('act', X['Y'][:], pY[0:64, :])
            for h in range(8):
                hs = slice(h * 64, (h + 1) * 64)
                fw.mm(pN[0:64, hs], Bh[d][:, hs], SAT[:, hs], start=True, stop=False)
                fw.mm(pN[0:64, hs], Kh[d][:, hs], V[:, hs], start=False, stop=True)
            for h in range(8):
                hs = slice(h * 64, (h + 1) * 64)
                fw.I('dve', 'scalar_tensor_tensor', part=True, out=ST[:, hs], in0=ST[:, hs], scalar=wcc[d][:, h:h + 1], in1=pN[0:64, hs], op0=ALU.mult, op1=ALU.add)
            Y = X['Y']; yn = X['yn']; t1 = X['t1']; t2 = X['t2']
            Y3 = Y[:].rearrange("p (h j) -> p h j", h=8)
            fw.I('dve', 'tensor_reduce', out=sm[:, 24:32], in_=Y3, axis=AX.X, op=ALU.add)
            fw.tt('pool', t1[:], Y[:], Y[:], ALU.mult)
            fw.I('dve', 'tensor_reduce', out=sm[:, 32:40], in_=t1[:].rearrange("p (h j) -> p h j", h=8), axis=AX.X, op=ALU.add)
            fw.ts('dve', sm[:, 24:32], sm[:, 24:32], 1.0 / 64, ALU.mult)
            fw.tt('dve', sm[:, 40:48], sm[:, 24:32], sm[:, 24:32], ALU.mult)
            fw.I('dve', 'scalar_tensor_tensor', out=sm[:, 32:40], in0=sm[:, 32:40], scalar=1.0 / 64, in1=sm[:, 40:48], op0=ALU.mult, op1=ALU.subtract)
            fw.act(sm[:, 40:48], sm[:, 32:40], AF.Sqrt, bias=64e-5)
            fw.I('dve', 'reciprocal', out=sm[:, 48:56], in_=sm[:, 40:48])
            for h in range(8):
                hs = slice(h * 64, (h + 1) * 64)
                fw.ts('dve', yn[:, hs], Y[:, hs], sm[:, 24 + h:25 + h], ALU.subtract, sm[:, 48 + h:49 + h], ALU.mult, part=True)
            fw.tt('pool', yn[:], yn[:], bc['lnw'][:], ALU.mult)
            fw.tt('pool', yn[:], yn[:], bc['lnb'][:], ALU.add)
            fw.tt('pool', t1[:], r, X['km'][:], ALU.mult)
            fw.tt('pool', t1[:], t1[:], bc['rkp'][:], ALU.mult)
            fw.I('dve', 'tensor_reduce', out=sm[:, 56:64], in_=t1[:].rearrange("p (h j) -> p h j", h=8), axis=AX.X, op=ALU.add)
            for h in range(8):
                hs = slice(h * 64, (h + 1) * 64)
                fw.I('dve', 'scalar_tensor_tensor', part=True, out=t2[:, hs], in0=V[:, hs], scalar=sm[:, 56 + h:57 + h], in1=yn[:, hs], op0=ALU.mult, op1=ALU.add)
            fw.tt('pool', t2[:], t2[:], X['g'][:], ALU.mult)
            fw.dma('sp', yrw_d[t0:t0 + 64, hg * 512:(hg + 1) * 512], t2[:])
    fw.barrier()


DBG_C = 9
DBG_C0 = 9
NQ = 3360


def stage_C0(fw, C, u_d, Pn, KcT, Vaug, es_outer):
    with ExitStack() as es:
        sb = lambda n, s, dt=F32: fw.sb('C0_' + n, s, dt, es)
        kcT = sb('kcT', [64, 4, 4096], BF16); vcT = sb('vcT', [64, 4, 4096], BF16)
        Nt = [sb('N%d' % i, [128, 512]) for i in range(2)]
        Nb = [sb('Nb%d' % i, [128, 512], BF16) for i in range(2)]
        w1 = {kv: sb('w1' + kv, [64, 32, 256], BF16) for kv in 'kv'}
        w2 = {kv: sb('w2' + kv, [128, 2, 64], BF16) for kv in 'kv'}
        posT = {kv: sb('pos' + kv, [64, 32], BF16) for kv in 'kv'}
        b1 = {kv: sb('b1' + kv, [128, 2]) for kv in 'kv'}
        b2k = sb('b2k', [64, 1]); b2v = sb('b2v', [128, 64]); kn0 = sb('kn0', [64, 1])
        bias = {kv: sb('bias' + kv, [128, 2]) for kv in 'kv'}
        hid = [sb('hid%d' % i, [128, 256], BF16) for i in range(2)]
        x = sb('x', [128, 256]); x2 = sb('x2', [128, 256]); x3 = sb('x3', [128, 256]); sg = sb('sg', [128, 256])
        kc_f = sb('kc_f', [64, 256]); sq = sb('sq', [64, 256]); rs = sb('rs', [64, 256])
        vtmp = sb('vtmp', [128, 64])
        pT = [fw.ps('C0_pT%d' % i, [128, 4, 128], F32, es) for i in range(2)]
        pH = [fw.ps('C0_pH%d' % i, [128, 512], F32, es) for i in range(2)]
        pK = fw.ps('C0_pK', [128, 512], F32, es); pB = fw.ps('C0_pB', [128, 512], F32, es)
        for kv in 'kv':
            fw.dma('pool', w1[kv][:], Pn['w1' + kv][:, :].rearrange("(l d) h -> d l h", d=64))
            fw.dma('pool', w2[kv][:], Pn['w2' + kv][:, :].rearrange("(c p) d -> p c d", p=128))
            fw.dma('pool', posT[kv][:], Pn['posT' + kv][:, :])
            fw.dma('sp', b1[kv][:], Pn['b1' + kv][:, :])
        fw.dma('sp', b2k[:], Pn['b2k'][:, :]); fw.dma('sp', b2v[:], Pn['b2v'][:, :].partition_broadcast(128))
        fw.dma('sp', kn0[:], Pn['kn0'][:, :])
        fw.dma('sp', Vaug[:, 0, :, 65:129], Pn['ovl'][0:128, :, :], part=True)
        fw.dma('sp', Vaug[:, 1, :, 65:129], Pn['ovl'][128:256, :, :], part=True)
        fw.I('dve', 'memset', ap=Vaug[:, :, :, 64:65], constant=1.0, part=True)
        for tt in range(32 if DBG_C0 >= 1 else 0):
            t0 = tt * 128
            N = Nt[tt % 2]; NB = Nb[tt % 2]
            fw.dma('sp', N[:], u_d[t0:t0 + 128, NQ + 1024:NQ + 1536])
            fw.cp('dve', NB[:], N[:])
            for hf, dstT in ((0, kcT), (1, vcT)):
                P = pT[hf]
                for j in range(4):
                    fw.mm(P[0:64, j, :], NB[:, (hf * 4 + j) * 64:(hf * 4 + j + 1) * 64], C['ident_b'])
                fw.cp('act' if hf else 'dve', dstT[:, :, t0:t0 + 128], P[0:64, 0:4, :], part=True)
        for kv in ('kv' if DBG_C0 >= 2 else ''):
            for hh in range(2):
                for l in range(32):
                    fw.mm(pB[:, hh:hh + 1], w1[kv][:, l, hh * 128:(hh + 1) * 128], posT[kv][:, l:l + 1], start=(l == 0), stop=(l == 31))
            fw.tt('dve', bias[kv][:], pB[:, 0:2], b1[kv][:], ALU.add)
        hi = 0
        for kv in ('kv' if DBG_C0 >= 3 else ''):
            src = kcT if kv == 'k' else vcT
            for g in range(4):
                for hh in range(2):
                    P = pH[hi % 2]; hi += 1
                    for l in range(32):
                        fw.mm(P[:, 0:255], w1[kv][:, l, hh * 128:(hh + 1) * 128], (src[:, g, :].rearrange("p (n s) -> p n s", s=16)[:, 0:255, l] if l < 16 else src[:, g, :].rearrange("p (n s) -> p n s", s=16)[:, 1:256, l - 16]), start=(l == 0), stop=(l == 31))
                    fw.ts('dve', x[:, 0:255], P[:, 0:255], bias[kv][:, hh:hh + 1], ALU.add)
                    fw.tt('pool', x2[:, 0:255], x[:, 0:255], x[:, 0:255], ALU.mult)
                    fw.tt('pool', x3[:, 0:255], x2[:, 0:255], x[:, 0:255], ALU.mult)
                    fw.I('dve', 'scalar_tensor_tensor', out=x2[:, 0:255], in0=x3[:, 0:255], scalar=0.044715, in1=x[:, 0:255], op0=ALU.mult, op1=ALU.add)
                    fw.act(sg[:, 0:255], x2[:, 0:255], AF.Sigmoid, scale=1.5957691216057308)
                    fw.tt('dve', hid[hh][:, 0:255], x[:, 0:255], sg[:, 0:255], ALU.mult)
                if DBG_C0 < 4: continue
                if kv == 'k':
                    for hh in range(2):
                        fw.mm(pK[0:64, 0:255], w2['k'][:, hh, :], hid[hh][:, 0:255], start=(hh == 0), stop=(hh == 1))
                    fw.ts('dve', kc_f[:, 0:255], pK[0:64, 0:255], b2k[:, 0:1], ALU.add)
                    fw.tt('pool', sq[:, 0:255], kc_f[:, 0:255], kc_f[:, 0:255], ALU.mult)
                    fw.mm(pK[0:64, 256:511], C['ones'][0:64, 0:64], sq[:, 0:255])
                    fw.act(rs[:, 0:255], pK[0:64, 256:511], AF.Sqrt, bias=RMS_EPS, scale=1.0 / 64)
                    fw.I('dve', 'reciprocal', out=rs[:, 0:255], in_=rs[:, 0:255])
                    fw.tt('pool', kc_f[:, 0:255], kc_f[:, 0:255], rs[:, 0:255], ALU.mult)
                    fw.ts('dve', KcT[:, g, 0:255], kc_f[:, 0:255], kn0[:, 0:1], ALU.mult, part=True)
                else:
                    for c in range(2):
                        n = 128 if c == 0 else 127
                        for hh in range(2):
                            fw.mm(pK[0:n, 0:64], hid[hh][:, c * 128:c * 128 + n], w2['v'][:, hh, :], start=(hh == 0), stop=(hh == 1))
                        fw.tt('dve', Vaug[0:n, c, g, 0:64], pK[0:n, 0:64], b2v[0:n, :], ALU.add, part=True)
    fw.barrier()


def stage_C(fw, C, u_d, Pn, ynsa_d, conv=None):
    with ExitStack() as es:
        sb = lambda n, s, dt=F32: fw.sb('C_' + n, s, dt, es)
        KcT = sb('KcT', [64, 4, 256], BF16); Vaug = sb('Vaug', [128, 2, 4, 129], BF16)
        fw.I('dve', 'memset', ap=KcT[:], constant=0.0)
        fw.I('dve', 'memset', ap=Vaug[:], constant=0.0)
        stage_C0(fw, C, u_d, Pn, KcT, Vaug, es)
        if DBG_C < 1:
            return
        ksT = sb('ksT', [64, 4, 4096], BF16); kwT = sb('kwT', [64, 4, 4096], BF16)
        Vs = sb('Vs', [128, 32, 4, 65], BF16); Vw = sb('Vw', [128, 32, 4, 65], BF16)
        Nt = [sb('N0', [128, 2608])] * 2
        Eb = sb('Eb', [128, 32, 4, 128], BF16)
        Ew = sb('Ew', [128, 5, 4, 128], BF16)
        sq = sb('sq', [128, 1024]); qn = sb('qn', [128, 1024], BF16); kn = sb('kn', [128, 512], BF16)
        qnb = sb('qnb', [128, 1024]); knb = sb('knb', [128, 512])
        st = sb('st', [128, 96]); gat = sb('gat', [128, 48])
        qT = [sb('qT%d' % i, [64, 4, 128], BF16) for i in range(2)]
        E = [sb('E%d' % i, [128, 4, 128], BF16) for i in range(3)]
        cm = [sb('cm%d' % i, [128, 2, 128], BF16) for i in range(2)]
        keep = [sb('keep%d' % i, [128, 64]) for i in range(2)]; addm = [sb('addm%d' % i, [128, 64]) for i in range(2)]
        selx = sb('selx', [64, 32, 128], BF16)
        cauT = sb('cauT', [128, 128], BF16); acauT = sb('acauT', [128, 128], BF16)
        imp = sb('imp', [128, 64]); mr = sb('mr', [128, 64]); selm = sb('selm', [128, 64], BF16); selT = sb('selT', [64, 128], BF16)
        t8 = sb('t8', [128, 24]); rsd = sb('rsd', [128, 16]); coef = sb('coef', [128, 16])
        yt = [sb('yt0', [128, 1024])] * 2
        qnw = sb('qnw', [128, 1024]); knw = sb('knw', [128, 512])
        pS = [fw.ps('C_pS%d' % i, [128, 512], F32, es) for i in range(2)]
        pM = fw.ps('C_pM', [128, 512], F32, es)
        pT = fw.ps('C_pT', [128, 4, 128], F32, es)
        pOc = [fw.ps('C_pOc%d' % i, [128, 2, 129], F32, es) for i in range(2)]
        pOs = fw.ps('C_pOs', [128, 4, 65], F32, es); pOw = fw.ps('C_pOw', [128, 4, 65], F32, es)
        fw.dma('sp', qnw[:], Pn['qnw'][:, :].partition_broadcast(128))
        fw.dma('sp', knw[:], Pn['knw'][:, :].partition_broadcast(128))
        fw.dma('sp', selx[:], Pn['selx'][:, :, :]); fw.dma('sp', cauT[:], Pn['cauT'][:, :]); fw.dma('sp', acauT[:], Pn['acauT'][:, :])
        fw.I('dve', 'memset', ap=Vs[:, :, :, 64:65], constant=1.0, part=True)
        fw.I('dve', 'memset', ap=Vw[:, :, :, 64:65], constant=1.0, part=True)
        ei = 0; si = 0
        for i in range(32):
            t0 = i * 128
            N = Nt[i % 2]
            fw.dma('sp', N[:], u_d[t0:t0 + 128, NQ:NQ + 2608])
            fw.dma('sp', cm[i % 2][:], Pn['cmask'][i, :, :, :])
            fw.dma('sp', keep[i % 2][:], Pn['keep'][i, :, :]); fw.dma('sp', addm[i % 2][:], Pn['addm'][i, :, :])
            fw.tt('pool', sq[:], N[:, 0:1024], N[:, 0:1024], ALU.mult)
            fw.I('dve', 'tensor_reduce', out=st[:, 0:16], in_=sq[:].rearrange("p (h j) -> p h j", h=16), axis=AX.X, op=ALU.add)
            fw.act(st[:, 16:32], st[:, 0:16], AF.Sqrt, bias=RMS_EPS, scale=1.0 / 64)
            fw.I('dve', 'reciprocal', out=st[:, 32:48], in_=st[:, 16:32])
            fw.ts('dve', st[:, 32:48], st[:, 32:48], 0.125, ALU.mult)
            for h in range(16):
                fw.ts('dve', qnb[:, h * 64:(h + 1) * 64], N[:, h * 64:(h + 1) * 64], st[:, 32 + h:33 + h], ALU.mult, part=True)
            fw.tt('dve', qn[:], qnb[:], qnw[:], ALU.mult)
            for (bi, off, dstT, Vd) in ((0, 1536, ksT, Vs), (1, 2048, kwT, Vw)):
                fw.tt('pool', sq[:, 0:256], N[:, off:off + 256], N[:, off:off + 256], ALU.mult)
                fw.I('dve', 'tensor_reduce', out=st[:, 48:52], in_=sq[:, 0:256].rearrange("p (h j) -> p h j", h=4), axis=AX.X, op=ALU.add)
                fw.act(st[:, 52:56], st[:, 48:52], AF.Sqrt, bias=RMS_EPS, scale=1.0 / 64)
                fw.I('dve', 'reciprocal', out=st[:, 56:60], in_=st[:, 52:56])
                for g in range(4):
                    fw.ts('dve', knb[:, bi * 256 + g * 64:bi * 256 + (g + 1) * 64], N[:, off + g * 64:off + (g + 1) * 64], st[:, 56 + g:57 + g], ALU.mult, part=True)
                fw.tt('dve', kn[:, bi * 256:(bi + 1) * 256], knb[:, bi * 256:(bi + 1) * 256], knw[:, bi * 256:(bi + 1) * 256], ALU.mult, part=True)
                for g in range(4):
                    fw.mm(pT[0:64, g, :], kn[:, bi * 256 + g * 64:bi * 256 + (g + 1) * 64], C['ident_b'])
                fw.cp('act', dstT[:, :, t0:t0 + 128], pT[0:64, 0:4, :], part=True)
                fw.cp('dve', Vd[:, i, :, 0:64], N[:, off + 256:off + 512].rearrange("p (g d) -> p g d", g=4), part=True)
            fw.act(gat[:], N[:, 2560:2608], AF.Sigmoid)
            y = yt[i % 2]
            for g in range(4 if DBG_C >= 2 else 0):
                if conv is not None:
                    next(conv, None); next(conv, None)
                Q = qT[si % 2]; si += 1
                for r_ in range(4):
                    h = g * 4 + r_
                    fw.mm(pT[0:64, r_, :], qn[:, h * 64:(h + 1) * 64], C['ident_b'])
                fw.cp('act', Q[:], pT[0:64, 0:4, :])
                Q2 = Q[:].rearrange("p r q -> p (r q)")
                nchunk = 1 if (8 * i + 6) < 128 else 2
                Ets = []
                for c in range(nchunk):
                    n = 128 if c == 0 else 127
                    P = pS[ei % 2]; Et = E[ei % 3]; ei += 1
                    Ets.append(Et)
                    fw.mm(P[0:n, :], KcT[:, g, c * 128:c * 128 + n], Q2)
                    fw.act(Et[0:n].rearrange("p r q -> p (r q)"), P[0:n, :], AF.Exp)
                    for r_ in range(4):
                        fw.tt('dve', Et[0:n, r_, :], Et[0:n, r_, :], cm[i % 2][0:n, c, :], ALU.mult, part=True)
                for r_ in range(4):
                    for c in range(nchunk):
                        n = 128 if c == 0 else 127
                        fw.mm(pOc[r_ // 2][:, r_ % 2, :], Ets[c][0:n, r_, :], Vaug[0:n, c, g, :], start=(c == 0), stop=(c == nchunk - 1))
                for r_ in range(4):
                    fw.ts('dve', rsd[:, r_:r_ + 1], pOc[r_ // 2][:, r_ % 2, 64:65], 1e-30, ALU.max, part=True)
                fw.I('dve', 'reciprocal', out=rsd[:, 4:8], in_=rsd[:, 0:4])
                fw.ts('dve', imp[:], pOc[0][:, 0, 65:129], rsd[:, 4:5], ALU.mult)
                for r_ in range(1, 4):
                    fw.I('dve', 'scalar_tensor_tensor', out=imp[:], in0=pOc[r_ // 2][:, r_ % 2, 65:129], scalar=rsd[:, 4 + r_:5 + r_], in1=imp[:], op0=ALU.mult, op1=ALU.add)
                if DBG_C < 3: continue
                fw.tt('dve', imp[:], imp[:], keep[i % 2][:], ALU.mult)
                fw.tt('dve', imp[:], imp[:], addm[i % 2][:], ALU.add)
                fw.I('dve', 'max', out=t8[:, 0:8], in_=imp[:])
                fw.I('dve', 'match_replace', out=mr[:], in_to_replace=t8[:, 0:8], in_values=imp[:], imm_value=-2e30)
                fw.I('dve', 'max', out=t8[:, 8:16], in_=mr[:])
                fw.ts('dve', t8[:, 16:17], t8[:, 15:16], -1e29, ALU.max)
                fw.ts('dve', selm[:], imp[:], t8[:, 16:17], ALU.is_ge)
                fw.mm(pT[0:64, 0, :], selm[:], C['ident_b'])
                fw.cp('act', selT[:], pT[0:64, 0, :])
                if DBG_C < 4: continue
                for kt in range(i + 1):
                    P = pS[ei % 2]; ei += 1
                    fw.mm(P[:], ksT[:, g, kt * 128:(kt + 1) * 128], Q2)
                    fw.mm(pM[:, 0:128], selx[:, kt, :], selT[:])
                    fw.act(Eb[:, kt].rearrange("p r q -> p (r q)"), P[:], AF.Exp, part=True)
                    for r_ in range(4):
                        fw.tt('dve', Eb[:, kt, r_, :], Eb[:, kt, r_, :], pM[:, 0:128], ALU.mult, part=True)
                        if kt == i:
                            fw.tt('dve', Eb[:, kt, r_, :], Eb[:, kt, r_, :], cauT[:], ALU.mult, part=True)
                for r_ in range(4):
                    for kt in range(i + 1):
                        fw.mm(pOs[:, r_, :], Eb[:, kt, r_, :], Vs[:, kt, g, :], start=(kt == 0), stop=(kt == i))
                if DBG_C < 5: continue
                k0 = max(0, i - 4)
                for kt in range(k0, i + 1):
                    P = pS[ei % 2]; ei += 1
                    sl_ = kt - k0
                    fw.mm(P[:], kwT[:, g, kt * 128:(kt + 1) * 128], Q2)
                    fw.act(Ew[:, sl_].rearrange("p r q -> p (r q)"), P[:], AF.Exp, part=True)
                    msk = cauT if kt == i else (acauT if kt == i - 4 else None)
                    if msk is not None:
                        for r_ in range(4):
                            fw.tt('dve', Ew[:, sl_, r_, :], Ew[:, sl_, r_, :], msk[:], ALU.mult, part=True)
                for r_ in range(4):
                    for kt in range(k0, i + 1):
                        fw.mm(pOw[:, r_, :], Ew[:, kt - k0, r_, :], Vw[:, kt, g, :], start=(kt == k0), stop=(kt == i))
                if DBG_C < 6: continue
                for r_ in range(4):
                    fw.ts('dve', rsd[:, 8 + r_:9 + r_], pOs[:, r_, 64:65], 1e-30, ALU.max, part=True)
                    fw.ts('dve', rsd[:, 12 + r_:13 + r_], pOw[:, r_, 64:65], 1e-30, ALU.max, part=True)
                fw.I('dve', 'reciprocal', out=coef[:, 4:12], in_=rsd[:, 8:16])
                fw.cp('dve', coef[:, 0:4], rsd[:, 4:8])
                for r_ in range(4):
                    h = g * 4 + r_
                    for b_ in range(3):
                        fw.tt('dve', coef[:, b_ * 4 + r_:b_ * 4 + r_ + 1], coef[:, b_ * 4 + r_:b_ * 4 + r_ + 1], gat[:, h * 3 + b_:h * 3 + b_ + 1], ALU.mult, part=True)
                for r_ in range(4):
                    h = g * 4 + r_
                    ys = y[:, h * 64:(h + 1) * 64]
                    fw.ts('dve', ys, pOc[r_ // 2][:, r_ % 2, 0:64], coef[:, r_:r_ + 1], ALU.mult, part=True)
                    fw.I('dve', 'scalar_tensor_tensor', part=True, out=ys, in0=pOs[:, r_, 0:64], scalar=coef[:, 4 + r_:5 + r_], in1=ys, op0=ALU.mult, op1=ALU.add)
                    fw.I('dve', 'scalar_tensor_tensor', part=True, out=ys, in0=pOw[:, r_, 0:64], scalar=coef[:, 8 + r_:9 + r_], in1=ys, op0=ALU.mult, op1=ALU.add)
            fw.dma('sp', ynsa_d[t0:t0 + 128, :], y[:])
    fw.barrier()


DBG_D = 9
NE = 32


def conv_weights(fw, w1_d, w2_d, w1b_d, w2b_d):
    for e in range(NE):
        for i in range(4):
            fw.dma('pool', w1b_d[e][i * 512:(i + 1) * 512, :].rearrange("(p a) c -> p (a c)", p=128),
                   w1_d[e, i * 512:(i + 1) * 512, :].rearrange("(p a) c -> p (a c)", p=128), part=True)
            yield
        for i in range(2):
            fw.dma('pool', w2b_d[e][i * 1024:(i + 1) * 1024, :].rearrange("(p a) c -> p (a c)", p=128),
                   w2_d[e, i * 1024:(i + 1) * 1024, :].rearrange("(p a) c -> p (a c)", p=128), part=True)
            yield


def stage_D(fw, C, yrw_d, ynsa_d, sel_d, xown_d, wout_d, mg_d, rw_d, rb_d, h1_d, xn2T_d, gates_d, gatesT_d):
    with ExitStack() as es:
        wo = fw.sb('D_wo', [128, 16, 2048], BF16, es)
        Y0 = [fw.sb('D_Y0%d' % i, [128, 2048], F32, es) for i in range(2)]
        Y1 = [fw.sb('D_Y1%d' % i, [128, 2048], F32, es) for i in range(2)]
        XO = [fw.sb('D_XO%d' % i, [128, 2048], F32, es) for i in range(2)]
        Yf = fw.sb('D_Yf', [128, 2048], F32, es)
        Yb = fw.sb('D_Yb', [128, 2048], BF16, es)
        yT = fw.sb('D_yT', [128, 16, 128], BF16, es)
        H = fw.sb('D_H', [128, 2048], F32, es)
        XN = fw.sb('D_XN', [128, 2048], F32, es)
        junk = fw.sb('D_junk', [128, 2048], BF16, es)
        xTh = fw.sb('D_xTh', [128, 16, 128], BF16, es)
        xTl = fw.sb('D_xTl', [128, 16, 128], BF16, es)
        XH = fw.sb('D_XH', [128, 2048], BF16, es)
        XL = fw.sb('D_XL', [128, 2048], BF16, es)
        rwh = fw.sb('D_rwh', [128, 16, 32], BF16, es)
        rwl = fw.sb('D_rwl', [128, 16, 32], BF16, es)
        xTb = fw.sb('D_xTb', [128, 16, 128], BF16, es)
        ss = fw.sb('D_ss', [128, 4], F32, es)
        sel = fw.sb('D_sel', [128, 2], F32, es)
        mg = fw.sb('D_mg', [128, 16], F32, es)
        rwg = fw.sb('D_rwg', [128, 16, 32], F32, es)
        rb = fw.sb('D_rb', [128, 32], F32, es)
        lg = fw.sb('D_lg', [128, 32], F32, es)
        ex = fw.sb('D_ex', [128, 32], F32, es)
        mk = fw.sb('D_mk', [128, 32], F32, es)
        t8 = fw.sb('D_t8', [128, 16], F32, es)
        gt_sb = fw.sb('D_gT', [32, 128], F32, es)
        pT = [fw.ps('D_pT%d' % i, [128, 8, 128], BF16, es) for i in range(2)]
        pM = [fw.ps('D_pM%d' % i, [128, 512], F32, es) for i in range(3)]
        pR = fw.ps('D_pR', [128, 512], F32, es)
        for i in range(4):
            fw.dma('pool', wo[:, :, i * 512:(i + 1) * 512], wout_d[:, i * 512:(i + 1) * 512].rearrange("(k p) c -> p k c", p=128), part=True)
        fw.dma('sp', sel[:], sel_d[:]); fw.dma('sp', mg[:], mg_d[:])
        fw.dma('sp', rwg[:], rw_d[:, :].rearrange("(k p) e -> p k e", p=128))
        fw.dma('sp', rb[:], rb_d[:, :].partition_broadcast(128))
        for k in range(16):
            fw.ts('dve', rwg[:, k, :], rwg[:, k, :], mg[:, k:k + 1], ALU.mult, part=True)
        fw.cp('dve', rwh[:], rwg[:])
        fw.tt('dve', rwl[:], rwg[:], rwh[:], ALU.subtract)
        for tt in range(16):
            t0 = tt * 128
            y0 = Y0[tt % 2]; y1 = Y1[tt % 2]; xo = XO[tt % 2]
            fw.dma('sp', y0[:, 0:1024], yrw_d[t0:t0 + 128, :], part=True)
            fw.dma('sp', y0[:, 1024:2048], ynsa_d[t0:t0 + 128, :], part=True)
            fw.dma('sp', y1[:, 0:1024], yrw_d[2048 + t0:2048 + t0 + 128, :], part=True)
            fw.dma('sp', y1[:, 1024:2048], ynsa_d[2048 + t0:2048 + t0 + 128, :], part=True)
            fw.dma('sp', xo[:], xown_d[t0:t0 + 128, :])
            fw.ts('pool', Yf[:], y0[:], sel[:, 0:1], ALU.mult)
            fw.I('dve', 'scalar_tensor_tensor', out=Yb[:], in0=y1[:], scalar=sel[:, 1:2], in1=Yf[:], op0=ALU.mult, op1=ALU.add)
            for hh in range(2):
                P = pT[hh]
                for j in range(8):
                    k = hh * 8 + j
                    fw.tr(P[:, j, :], Yb[:, k * 128:(k + 1) * 128], C['ident_b'])
                fw.cp('act' if hh else 'dve', yT[:, hh * 8:(hh + 1) * 8, :], P[:], part=True)
            for cb in range(4):
                P = pM[cb % 3]
                for k in range(16):
                    fw.mm(P[:], yT[:, k, :], wo[:, k, cb * 512:(cb + 1) * 512], start=(k == 0), stop=(k == 15))
                fw.tt('dve', H[:, cb * 512:(cb + 1) * 512], P[:], xo[:, cb * 512:(cb + 1) * 512], ALU.add, part=True)
            fw.dma('sp', h1_d[t0:t0 + 128, :], H[:])
            if DBG_D < 2: continue
            rms_rstd(fw, H[:], ss, junk[:])
            fw.ts('pool', XN[:], H[:], ss[:, 2:3], ALU.mult)
            fw.cp('dve', XH[:], XN[:])
            fw.tt('dve', XL[:], XN[:], XH[:], ALU.subtract)
            for (src, dstT, scaled) in ((XH, xTh, True), (XL, xTl, False)):
                for hh in range(2):
                    P = pT[hh]
                    for j in range(8):
                        k = hh * 8 + j
                        fw.tr(P[:, j, :], src[:, k * 128:(k + 1) * 128], C['ident_b'])
                    fw.cp('act', dstT[:, hh * 8:(hh + 1) * 8, :], P[:], part=True)
                    if scaled:
                        for j in range(8):
                            k = hh * 8 + j
                            fw.ts('dve', xTb[:, k, :], P[:, j, :], mg[:, k:k + 1], ALU.mult, part=True)
            fw.dma('sp', xn2T_d[:, :, t0:t0 + 128], xTb[:])
            if DBG_D < 3: continue
            n = 0
            for (a_, w_) in ((xTh, rwh), (xTl, rwh), (xTh, rwl)):
                for k in range(16):
                    fw.mm(pR[:, 0:32], a_[:, k, :], w_[:, k, :], start=(n == 0), stop=(n == 47)); n += 1
            fw.tt('dve', lg[:], pR[:, 0:32], rb[:], ALU.add)
            fw.I('dve', 'max', out=t8[:, 0:8], in_=lg[:])
            fw.ts('dve', mk[:], lg[:], t8[:, 3:4], ALU.is_ge)
            fw.ts('dve', t8[:, 8:9], t8[:, 0:1], -1.0, ALU.mult)
            fw.act(ex[:], lg[:], AF.Exp, bias=t8[:, 8:9])
            fw.tt('dve', ex[:], ex[:], mk[:], ALU.mult)
            fw.I('dve', 'tensor_reduce', out=t8[:, 9:10], in_=ex[:], axis=AX.X, op=ALU.add)
            fw.I('dve', 'reciprocal', out=t8[:, 10:11], in_=t8[:, 9:10])
            fw.ts('dve', lg[:], ex[:], t8[:, 10:11], ALU.mult)
            fw.dma('sp', gates_d[t0:t0 + 128, :], lg[:])
    fw.barrier()


def stage_E(fw, C, w1b_d, w2b_d, b1_d, b2_d, h1_d, xn2T_d, gates_d, gatesT_d, h2_d):
    TP = 512; NT = TP // 128
    with ExitStack() as es:
        xT = fw.sb('E_xT', [128, 16, TP], BF16, es)
        acc = fw.sb('E_acc', [128, NT, 2048], F32, es)
        actT = fw.sb('E_actT', [128, 16, TP], BF16, es)
        W1 = [fw.sb('E_W1%d' % i, [128, 16, 512], BF16, es) for i in range(2)]
        W2 = [fw.sb('E_W2%d' % i, [128, 16, 512], BF16, es) for i in range(2)]
        b1 = fw.sb('E_b1', [128, 32, 32], F32, es)
        b2bc = [fw.sb('E_b2bc0', [128, 2048], F32, es)] * 2
        gat = fw.sb('E_gat', [128, NT, 32], F32, es)
        h1t = [fw.sb('E_h10', [128, 2048], F32, es)] * 2
        tg = [fw.sb('E_tg%d' % i, [128, 512], F32, es) for i in range(2)]
        tsg = [fw.sb('E_ts%d' % i, [128, 512], F32, es) for i in range(2)]
        tl = [fw.sb('E_tl%d' % i, [128, 512], F32, es) for i in range(2)]
        tl2 = [fw.sb('E_tm%d' % i, [128, 512], F32, es) for i in range(2)]
        pG = [fw.ps('E_pG%d' % i, [128, 512], F32, es) for i in range(2)]
        pL = [fw.ps('E_pL%d' % i, [128, 512], F32, es) for i in range(2)]
        pY = [fw.ps('E_pY%d' % i, [128, 512], F32, es) for i in range(4)]
        fw.dma('sp', b1[:], b1_d[:])
        w1i = 0; w2i = 0; si = 0; yi = 0
        for tp in range(2048 // TP):
            T0 = tp * TP
            fw.dma('sp', xT[:], xn2T_d[:, :, T0:T0 + TP])
            fw.dma('sp', gat[:], gates_d[T0:T0 + TP, :].rearrange("(n p) e -> p n e", p=128))
            fw.I('pool', 'memset', ap=acc[:], constant=0.0)
            for e in range(NE):
                for hb in range(8):
                    W = W1[w1i % 2]; w1i += 1
                    q = 'sp' if w1i % 2 else 'act'
                    fw.dma(q, W[:, :, 0:256], w1b_d[e][:, hb * 256:(hb + 1) * 256].rearrange("(k p) c -> p k c", p=128), part=True)
                    fw.dma(q, W[:, :, 256:512], w1b_d[e][:, 2048 + hb * 256:2048 + (hb + 1) * 256].rearrange("(k p) c -> p k c", p=128), part=True)
                    for sub in range(2):
                        G = pG[si % 2]; L = pL[si % 2]
                        g_ = tg[si % 2]; s_ = tsg[si % 2]; l_ = tl[si % 2]; m_ = tl2[si % 2]; si += 1
                        for k in range(16):
                            fw.mm(G[:], W[:, k, sub * 128:(sub + 1) * 128], xT[:, k, :], start=(k == 0), stop=(k == 15))
                        for k in range(16):
                            fw.mm(L[:], W[:, k, 256 + sub * 128:256 + (sub + 1) * 128], xT[:, k, :], start=(k == 0), stop=(k == 15))
                        cg = hb * 2 + sub
                        fw.ts('dve', g_[:], G[:], b1[:, e, cg:cg + 1], ALU.add, 7.0, ALU.min)
                        fw.act(s_[:], g_[:], AF.Sigmoid, scale=1.702)
                        fw.ts('dve', l_[:], L[:], b1[:, e, 16 + cg:16 + cg + 1], ALU.add, 7.0, ALU.min)
                        fw.ts('pool', m_[:], l_[:], -7.0, ALU.max, 1.0, ALU.add)
                        fw.tt('pool', s_[:], s_[:], g_[:], ALU.mult)
                        fw.tt('dve', actT[:, cg, :], s_[:], m_[:], ALU.mult, part=True)
                bb = b2bc[e % 2]
                fw.dma('sp', bb[:], b2_d[e:e + 1, :].partition_broadcast(128))
                for tl_ in range(NT):
                    fw.I('dve', 'scalar_tensor_tensor', part=True, out=acc[:, tl_, :], in0=bb[:], scalar=gat[:, tl_, e:e + 1], in1=acc[:, tl_, :], op0=ALU.mult, op1=ALU.add)
                for fb in range(4):
                    W = W2[w2i % 2]; w2i += 1
                    q = 'sp' if w2i % 2 else 'act'
                    fw.dma(q, W[:], w2b_d[e][:, fb * 512:(fb + 1) * 512].rearrange("(k p) c -> p k c", p=128))
                    for tl_ in range(NT):
                        P = pY[yi % 4]; yi += 1
                        for k in range(16):
                            fw.mm(P[:], actT[:, k, tl_ * 128:(tl_ + 1) * 128], W[:, k, :], start=(k == 0), stop=(k == 15))
                        fw.I('dve', 'scalar_tensor_tensor', part=True, out=acc[:, tl_, fb * 512:(fb + 1) * 512], in0=P[:],
                             scalar=gat[:, tl_, e:e + 1], in1=acc[:, tl_, fb * 512:(fb + 1) * 512], op0=ALU.mult, op1=ALU.add)
            for tl_ in range(NT):
                h = h1t[tl_ % 2]
                fw.dma('sp', h[:], h1_d[T0 + tl_ * 128:T0 + (tl_ + 1) * 128, :])
                fw.tt('pool', h[:], h[:], acc[:, tl_, :], ALU.add)
                fw.dma('sp', h2_d[T0 + tl_ * 128:T0 + (tl_ + 1) * 128, :], h[:])
    fw.barrier()


def stage_F(fw, C, h2_d, pown_d, pg_d, plw_d, pgw_d, out_d):
    with ExitStack() as es:
        pgw = fw.sb('F_pgw', [128, 16, 2048], BF16, es)
        plw = fw.sb('F_plw', [128, 2, 2048], BF16, es)
        Hh = [fw.sb('F_H%d' % i, [128, 2048], F32, es) for i in range(2)]
        Pp = [fw.sb('F_P%d' % i, [128, 256], F32, es) for i in range(2)]
        Pb = fw.sb('F_Pb', [128, 256], BF16, es)
        XB = fw.sb('F_XB', [128, 2048], BF16, es)
        junk = fw.sb('F_junk', [128, 2048], BF16, es)
        xT = fw.sb('F_xT', [128, 16, 128], BF16, es)
        ppT = fw.sb('F_ppT', [128, 2, 128], BF16, es)
        gsb = [fw.sb('F_g%d' % i, [128, 512], F32, es) for i in range(2)]
        O = [fw.sb('F_O%d' % i, [128, 2048], F32, es) for i in range(2)]
        ss = fw.sb('F_ss', [128, 4], F32, es)
        pg = fw.sb('F_pg', [128, 16], F32, es)
        pT = [fw.ps('F_pT%d' % i, [128, 8, 128], BF16, es) for i in range(2)]
        pA = [fw.ps('F_pA%d' % i, [128, 512], F32, es) for i in range(2)]
        pB = [fw.ps('F_pB%d' % i, [128, 512], F32, es) for i in range(2)]
        pQ = fw.ps('F_pQ', [128, 2, 128], BF16, es)
        for i in range(4):
            fw.dma('pool', pgw[:, :, i * 512:(i + 1) * 512], pgw_d[:, i * 512:(i + 1) * 512].rearrange("(k p) c -> p k c", p=128), part=True)
        fw.dma('pool', plw[:], plw_d[:, :].rearrange("(k p) c -> p k c", p=128))
        fw.dma('sp', pg[:], pg_d[:])
        ci = 0
        for tt in range(16):
            t0 = tt * 128
            h = Hh[tt % 2]; pp = Pp[tt % 2]; o = O[tt % 2]
            fw.dma('sp', h[:], h2_d[t0:t0 + 128, :])
            fw.dma('sp', pp[:], pown_d[t0:t0 + 128, :])
            rms_rstd(fw, h[:], ss, junk[:])
            fw.ts('dve', XB[:], h[:], ss[:, 2:3], ALU.mult)
            fw.cp('dve', Pb[:], pp[:])
            for hh in range(2):
                P = pT[hh]
                for j in range(8):
                    k = hh * 8 + j
                    fw.tr(P[:, j, :], XB[:, k * 128:(k + 1) * 128], C['ident_b'])
                for j in range(8):
                    k = hh * 8 + j
                    if j % 2: fw.act(xT[:, k, :], P[:, j, :], AF.Copy, scale=pg[:, k:k + 1], part=True)
                    else: fw.ts('dve', xT[:, k, :], P[:, j, :], pg[:, k:k + 1], ALU.mult, part=True)
            for j in range(2):
                fw.tr(pQ[:, j, :], Pb[:, j * 128:(j + 1) * 128], C['ident_b'])
            fw.cp('act', ppT[:], pQ[:])
            for cb in range(4):
                A_ = pA[ci % 2]; B_ = pB[ci % 2]; g_ = gsb[ci % 2]; ci += 1
                cs = slice(cb * 512, (cb + 1) * 512)
                for k in range(16):
                    fw.mm(A_[:], xT[:, k, :], pgw[:, k, cs], start=(k == 0), stop=(k == 15))
                for k in range(2):
                    fw.mm(B_[:], ppT[:, k, :], plw[:, k, cs], start=(k == 0), stop=(k == 1))
                fw.act(g_[:], A_[:], AF.Sigmoid)
                fw.tt('dve', g_[:], g_[:], B_[:], ALU.mult)
                fw.tt('pool', o[:, cs], g_[:], h[:, cs], ALU.add, part=True)
            fw.dma('sp', out_d[t0:t0 + 128, :], o[:])
    fw.barrier()


DBG_SP = 9
CAP = 512
NST = CAP // 128


def stage_D2(fw, C, yrw_d, ynsa_d, sel_d, xown_d, wout_d, mg_d, mgrow_d, rw_d, rb_d, h1_d, xn2_d, mk_d, pos_d, gates_d, lts_d):
    with ExitStack() as es:
        wo = fw.sb('D_wo', [128, 16, 2048], BF16, es)
        Y0 = [fw.sb('D_Y0%d' % i, [128, 2048], F32, es) for i in range(2)]
        Y1 = [fw.sb('D_Y1%d' % i, [128, 2048], F32, es) for i in range(2)]
        XO = [fw.sb('D_XO%d' % i, [128, 2048], F32, es) for i in range(2)]
        Yf = fw.sb('D_Yf', [128, 2048], F32, es)
        Yb = fw.sb('D_Yb', [128, 2048], BF16, es)
        yT = fw.sb('D_yT', [128, 16, 128], BF16, es)
        H = fw.sb('D_H', [128, 2048], F32, es)
        XN = fw.sb('D_XN', [128, 2048], F32, es)
        XG = fw.sb('D_XG', [128, 2048], BF16, es)
        gbc = fw.sb('D_gbc', [128, 2048], F32, es)
        junk = fw.sb('D_junk', [128, 2048], BF16, es)
        xTh = fw.sb('D_xTh', [128, 16, 128], BF16, es); xTl = fw.sb('D_xTl', [128, 16, 128], BF16, es)
        XH = fw.sb('D_XH', [128, 2048], BF16, es); XL = fw.sb('D_XL', [128, 2048], BF16, es)
        rwh = fw.sb('D_rwh', [128, 16, 32], BF16, es); rwl = fw.sb('D_rwl', [128, 16, 32], BF16, es)
        ss = fw.sb('D_ss', [128, 4], F32, es); sel = fw.sb('D_sel', [128, 2], F32, es)
        mg = fw.sb('D_mg', [128, 16], F32, es); rwg = fw.sb('D_rwg', [128, 16, 32], F32, es)
        rb = fw.sb('D_rb', [128, 32], F32, es); lg = fw.sb('D_lg', [128, 32], F32, es)
        ex = fw.sb('D_ex', [128, 32], F32, es); mk = fw.sb('D_mk', [128, 32], F32, es)
        mkb = fw.sb('D_mkb', [128, 32], BF16, es); msum = fw.sb('D_msum', [128, 32], F32, es); msb = fw.sb('D_msb', [128, 32], BF16, es)
        pos = fw.sb('D_pos', [128, 32], F32, es)
        t8 = fw.sb('D_t8', [128, 16], F32, es)
        lts = fw.sb('D_lts', [128, 128], BF16, es); onb = fw.sb('D_onb', [128, 128], BF16, es)
        pT = [fw.ps('D_pT%d' % i, [128, 8, 128], BF16, es) for i in range(2)]
        pM = [fw.ps('D_pM%d' % i, [128, 512], F32, es) for i in range(3)]
        pR = fw.ps('D_pR', [128, 512], F32, es); pQ = fw.ps('D_pQ', [128, 512], F32, es)
        for i in range(4):
            fw.dma('pool', wo[:, :, i * 512:(i + 1) * 512], wout_d[:, i * 512:(i + 1) * 512].rearrange("(k p) c -> p k c", p=128), part=True)
        fw.dma('sp', sel[:], sel_d[:]); fw.dma('sp', mg[:], mg_d[:])
        fw.dma('sp', gbc[:], mgrow_d[:, :].partition_broadcast(128))
        fw.dma('sp', rwg[:], rw_d[:, :].rearrange("(k p) e -> p k e", p=128))
        fw.dma('sp', rb[:], rb_d[:, :].partition_broadcast(128))
        fw.dma('sp', lts[:], lts_d[:, :])
        fw.cp('dve', onb[:], C['ones'])
        fw.I('pool', 'memset', ap=msum[:], constant=0.0)
        for k in range(16):
            fw.ts('dve', rwg[:, k, :], rwg[:, k, :], mg[:, k:k + 1], ALU.mult, part=True)
        fw.cp('dve', rwh[:], rwg[:])
        fw.tt('dve', rwl[:], rwg[:], rwh[:], ALU.subtract)
        for tt in range(16):
            t0 = tt * 128
            y0 = Y0[tt % 2]; y1 = Y1[tt % 2]; xo = XO[tt % 2]
            fw.dma('sp', y0[:, 0:1024], yrw_d[t0:t0 + 128, :], part=True)
            fw.dma('sp', y0[:, 1024:2048], ynsa_d[t0:t0 + 128, :], part=True)
            fw.dma('sp', y1[:, 0:1024], yrw_d[2048 + t0:2048 + t0 + 128, :], part=True)
            fw.dma('sp', y1[:, 1024:2048], ynsa_d[2048 + t0:2048 + t0 + 128, :], part=True)
            fw.dma('sp', xo[:], xown_d[t0:t0 + 128, :])
            fw.ts('pool', Yf[:], y0[:], sel[:, 0:1], ALU.mult)
            fw.I('dve', 'scalar_tensor_tensor', out=Yb[:], in0=y1[:], scalar=sel[:, 1:2], in1=Yf[:], op0=ALU.mult, op1=ALU.add)
            for hh in range(2):
                P = pT[hh]
                for j in range(8):
                    k = hh * 8 + j
                    fw.tr(P[:, j, :], Yb[:, k * 128:(k + 1) * 128], C['ident_b'])
                fw.cp('act' if hh else 'dve', yT[:, hh * 8:(hh + 1) * 8, :], P[:], part=True)
            for cb in range(4):
                P = pM[cb % 3]
                for k in range(16):
                    fw.mm(P[:], yT[:, k, :], wo[:, k, cb * 512:(cb + 1) * 512], start=(k == 0), stop=(k == 15))
                fw.tt('dve', H[:, cb * 512:(cb + 1) * 512], P[:], xo[:, cb * 512:(cb + 1) * 512], ALU.add, part=True)
            fw.dma('sp', h1_d[t0:t0 + 128, :], H[:])
            rms_rstd(fw, H[:], ss, junk[:])
            fw.ts('pool', XN[:], H[:], ss[:, 2:3], ALU.mult)
            fw.tt('dve', XG[:], XN[:], gbc[:], ALU.mult)
            fw.dma('sp', xn2_d[t0:t0 + 128, :], XG[:])
            fw.cp('dve', XH[:], XN[:])
            fw.tt('dve', XL[:], XN[:], XH[:], ALU.subtract)
            for (src, dstT) in ((XH, xTh), (XL, xTl)):
                for hh in range(2):
                    P = pT[hh]
                    for j in range(8):
                        k = hh * 8 + j
                        fw.tr(P[:, j, :], src[:, k * 128:(k + 1) * 128], C['ident_b'])
                    fw.cp('act', dstT[:, hh * 8:(hh + 1) * 8, :], P[:], part=True)
            n = 0
            for (a_, w_) in ((xTh, rwh), (xTl, rwh), (xTh, rwl)):
                for k in range(16):
                    fw.mm(pR[:, 0:32], a_[:, k, :], w_[:, k, :], start=(n == 0), stop=(n == 47)); n += 1
            fw.tt('dve', lg[:], pR[:, 0:32], rb[:], ALU.add)
            fw.I('dve', 'max', out=t8[:, 0:8], in_=lg[:])
            fw.ts('dve', mk[:], lg[:], t8[:, 3:4], ALU.is_ge)
            fw.ts('dve', t8[:, 8:9], t8[:, 0:1], -1.0, ALU.mult)
            fw.act(ex[:], lg[:], AF.Exp, bias=t8[:, 8:9])
            fw.tt('dve', ex[:], ex[:], mk[:], ALU.mult)
            fw.I('dve', 'tensor_reduce', out=t8[:, 9:10], in_=ex[:], axis=AX.X, op=ALU.add)
            fw.I('dve', 'reciprocal', out=t8[:, 10:11], in_=t8[:, 9:10])
            fw.ts('dve', lg[:], ex[:], t8[:, 10:11], ALU.mult)
            fw.dma('sp', mk_d[t0:t0 + 128, :], mk[:])
            if DBG_SP < 1: continue
            fw.cp('dve', mkb[:], mk[:]); fw.cp('dve', msb[:], msum[:])
            fw.mm(pQ[:, 0:32], lts[:], mkb[:], start=True, stop=False)
            fw.mm(pQ[:, 0:32], onb[:], msb[:], start=False, stop=True)
            fw.cp('dve', pos[:], pQ[:, 0:32])
            fw.tt('pool', msum[:], msum[:], mk[:], ALU.add)
            fw.dma('sp', pos_d[t0:t0 + 128, :], pos[:])
            fw.dma('sp', gates_d[t0:t0 + 128, :], lg[:])
    fw.barrier()


def stage_E1(fw, C, w1b_d, w2b_d, b1_d, b2_d, xn2_d, mk_d, pos_d, iota_d, yc_d):
    with ExitStack() as es:
        xn = fw.sb('E_xn', [128, 16, 2048], BF16, es)
        Sel = fw.sb('E_Sel', [128, 16, CAP], BF16, es)
        XcT = fw.sb('E_XcT', [128, 16, CAP], BF16, es)
        actT = fw.sb('E_actT', [128, 16, CAP], BF16, es)
        W1 = [fw.sb('E_W1%d' % i, [128, 16, 256], BF16, es) for i in range(2)]
        W2 = [fw.sb('E_W2%d' % i, [128, 16, 256], BF16, es) for i in range(2)]
        b1 = fw.sb('E_b1', [128, 32, 32], F32, es)
        b2bc = fw.sb('E_b2bc', [128, 2048], F32, es)
        pos = fw.sb('E_pos', [128, 16, 32], F32, es); mk = fw.sb('E_mk', [128, 16, 32], F32, es)
        iota = fw.sb('E_iota', [128, CAP], F32, es)
        tg = [fw.sb('E_tg%d' % i, [128, CAP], F32, es) for i in range(2)]
        tsg = [fw.sb('E_ts%d' % i, [128, CAP], F32, es) for i in range(2)]
        tl = [fw.sb('E_tl%d' % i, [128, CAP], F32, es) for i in range(2)]
        tl2 = [fw.sb('E_tm%d' % i, [128, CAP], F32, es) for i in range(2)]
        Yst = [fw.sb('E_Y%d' % i, [128, 256], BF16, es) for i in range(2)]
        pC = [fw.ps('E_pC%d' % i, [128, 512], F32, es) for i in range(2)]
        pG = [fw.ps('E_pG%d' % i, [128, 512], F32, es) for i in range(2)]
        pL = [fw.ps('E_pL%d' % i, [128, 512], F32, es) for i in range(2)]
        pY = [fw.ps('E_pY%d' % i, [128, 512], F32, es) for i in range(2)]
        fw.dma('sp', b1[:], b1_d[:])
        fw.dma('sp', xn[:], xn2_d[:, :].rearrange("(n p) f -> p n f", p=128))
        fw.dma('sp', pos[:], pos_d[:, :].rearrange("(n p) e -> p n e", p=128))
        fw.dma('sp', mk[:], mk_d[:, :].rearrange("(n p) e -> p n e", p=128))
        fw.dma('sp', iota[:], iota_d[:, :])
        w1i = 0; w2i = 0; si = 0; ci = 0; yi = 0
        for e in range(NE):
            for tl_ in range(16):
                fw.ts('dve', Sel[:, tl_, :], iota[:], pos[:, tl_, e:e + 1], ALU.is_equal, mk[:, tl_, e:e + 1], ALU.mult, part=True)
            for k in range(16):
                P = pC[ci % 2]; ci += 1
                for tl_ in range(16):
                    fw.mm(P[:, 0:CAP], xn[:, tl_, k * 128:(k + 1) * 128], Sel[:, tl_, :], start=(tl_ == 0), stop=(tl_ == 15))
                fw.cp('act', XcT[:, k, :], P[:, 0:CAP], part=True)
            for hb in range(16):
                W = W1[w1i % 2]; w1i += 1
                q = 'sp' if w1i % 2 else 'act'
                fw.dma(q, W[:, :, 0:128], w1b_d[e][:, hb * 128:(hb + 1) * 128].rearrange("(k p) c -> p k c", p=128), part=True)
                fw.dma(q, W[:, :, 128:256], w1b_d[e][:, 2048 + hb * 128:2048 + (hb + 1) * 128].rearrange("(k p) c -> p k c", p=128), part=True)
                G = pG[si % 2]; L = pL[si % 2]
                g_ = tg[si % 2]; s_ = tsg[si % 2]; l_ = tl[si % 2]; m_ = tl2[si % 2]; si += 1
                for k in range(16):
                    fw.mm(G[:, 0:CAP], W[:, k, 0:128], XcT[:, k, :], start=(k == 0), stop=(k == 15))
                for k in range(16):
                    fw.mm(L[:, 0:CAP], W[:, k, 128:256], XcT[:, k, :], start=(k == 0), stop=(k == 15))
                fw.ts('dve', g_[:], G[:, 0:CAP], b1[:, e, hb:hb + 1], ALU.add, 7.0, ALU.min)
                fw.act(s_[:], g_[:], AF.Sigmoid, scale=1.702)
                fw.ts('dve', l_[:], L[:, 0:CAP], b1[:, e, 16 + hb:16 + hb + 1], ALU.add, 7.0, ALU.min)
                fw.ts('pool', m_[:], l_[:], -7.0, ALU.max, 1.0, ALU.add)
                fw.tt('pool', s_[:], s_[:], g_[:], ALU.mult)
                fw.tt('dve', actT[:, hb, :], s_[:], m_[:], ALU.mult, part=True)
            fw.dma('sp', b2bc[:], b2_d[e:e + 1, :].partition_broadcast(128))
            for fb in range(8):
                W = W2[w2i % 2]; w2i += 1
                q = 'sp' if w2i % 2 else 'act'
                fw.dma(q, W[:], w2b_d[e][:, fb * 256:(fb + 1) * 256].rearrange("(k p) c -> p k c", p=128))
                for st in range(NST):
                    P = pY[yi % 2]; Y = Yst[yi % 2]; yi += 1
                    for k in range(16):
                        fw.mm(P[:, 0:256], actT[:, k, st * 128:(st + 1) * 128], W[:, k, :], start=(k == 0), stop=(k == 15))
                    fw.tt('dve', Y[:], P[:, 0:256], b2bc[:, fb * 256:(fb + 1) * 256], ALU.add)
                    fw.dma('pool', yc_d[e * CAP + st * 128:e * CAP + (st + 1) * 128, fb * 256:(fb + 1) * 256], Y[:])
    fw.barrier()


def stage_E2(fw, C, yc_d, pos_d, gates_d, iotap_d, h1_d, h2_d):
    with ExitStack() as es:
        pb = [fw.sb('G_pb0', [128, 32, 128], F32, es)] * 2
        gb = [fw.sb('G_gb0', [128, 32, 128], F32, es)] * 2
        Dp = fw.sb('G_Dp', [128, 32, 128], F32, es); Dg = fw.sb('G_Dg', [128, 32, 128], F32, es)
        pt = [fw.sb('G_pt%d' % i, [128, 32], F32, es) for i in range(2)]; gt = [fw.sb('G_gt%d' % i, [128, 32], F32, es) for i in range(2)]
        pB = [fw.ps('G_pB%d' % i, [128, 512], F32, es) for i in range(4)]
        tmp = fw.sb('G_tmp', [128, 32, 128], F32, es)
        S = [fw.sb('G_S%d' % i, [128, 32, NST, 128], BF16, es) for i in range(2)]
        Yc = [fw.sb('G_Yc%d' % i, [128, NST, 512], BF16, es) for i in range(3)]
        h = [fw.sb('G_h%d' % i, [128, 2048], F32, es) for i in range(2)]
        iop = fw.sb('G_iop', [128, NST], F32, es)
        pA = [fw.ps('G_pA%d' % i, [128, 512], F32, es) for i in range(2)]
        fw.dma('sp', iop[:], iotap_d[:, :])
        yi = 0; ai = 0
        for tt in range(16):
            t0 = tt * 128
            P_ = pb[tt % 2]; G_ = gb[tt % 2]; S_ = S[tt % 2]; H_ = h[tt % 2]
            fw.dma('sp', pt[tt % 2][:], pos_d[t0:t0 + 128, :]); fw.dma('sp', gt[tt % 2][:], gates_d[t0:t0 + 128, :])
            bi = 0
            for (src, Dx, dst) in ((pt[tt % 2], Dp, P_), (gt[tt % 2], Dg, G_)):
                for e in range(32):
                    fw.ts('dve' if e % 2 else 'pool', Dx[:, e, :], C['ident_f'], src[:, e:e + 1], ALU.mult, part=True)
                for c in range(8):
                    B_ = pB[bi % 4]; bi += 1
                    fw.mm(B_[:], C['ones'], Dx[:, c * 4:(c + 1) * 4, :].rearrange("p e t -> p (e t)"))
                    fw.cp('act', dst[:, c * 4:(c + 1) * 4, :].rearrange("p e t -> p (e t)"), B_[:], part=True)
            fw.dma('sp', H_[:], h1_d[t0:t0 + 128, :])
            for st in range(NST):
                fw.ts('dve', tmp[:], P_[:], iop[:, st:st + 1], ALU.is_equal)
                fw.tt('dve', S_[:, :, st, :], tmp[:], G_[:], ALU.mult, part=True)
            for fb in range(4):
                A_ = pA[ai % 2]; ai += 1
                for e in range(NE):
                    Y = Yc[yi % 3]; yi += 1
                    fw.dma('sp' if yi % 2 else 'act', Y[:], yc_d[e * CAP:(e + 1) * CAP, fb * 512:(fb + 1) * 512].rearrange("(s p) f -> p s f", p=128))
                    for st in range(NST):
                        fw.mm(A_[:], S_[:, e, st, :], Y[:, st, :], start=(e == 0 and st == 0), stop=(e == NE - 1 and st == NST - 1))
                fw.tt('dve', H_[:, fb * 512:(fb + 1) * 512], H_[:, fb * 512:(fb + 1) * 512], A_[:], ALU.add, part=True)
            fw.dma('sp', h2_d[t0:t0 + 128, :], H_[:])
    fw.barrier()


BFNP = ml_dtypes.bfloat16


def _consts():
    c = {}
    c['ident_b'] = np.eye(128).astype(BFNP)
    c['ident_f'] = np.eye(128, dtype=np.float32)
    c['ones'] = np.ones((128, 128), np.float32)
    return c


def _rw_consts():
    r = np.arange(64)[:, None]; s = np.arange(64)[None, :]
    rep = lambda m: np.tile(m.astype(np.float32), (1, 8))
    return {'c_triu': (r <= s).astype(np.float32), 'c_mus': rep(r < s), 'c_mls': rep(r > s), 'c_mui': rep(r <= s), 'c_eye8': rep(r == s)}


def _rw_params(I, hg):
    sl = slice(hg * 512, (hg + 1) * 512)
    mu = I['rw_mu'][0]
    p = {}
    p['mu'] = np.concatenate([mu[0:1024][sl], mu[1024:2048][sl], mu[2048:3072][sl], mu[3072:3360]])[None, :]
    p['w0'] = I['rw_w0'][0][sl][None]; p['a0'] = I['rw_a0'][0][sl][None]; p['kkp'] = I['rw_k_k'][0][sl][None]
    p['kap'] = I['rw_k_a'][0][sl][None]; p['rkp'] = I['rw_r_k'][0].reshape(-1)[sl][None]
    p['lnw'] = I['rw_lnx_w'][0][sl][None]; p['lnb'] = I['rw_lnx_b'][0][sl][None]
    p['w2'] = I['rw_w2'][0][:, sl]; p['a2'] = I['rw_a2'][0][:, sl]; p['g2'] = I['rw_g2'][0][:, sl]
    return {k: np.ascontiguousarray(v, dtype=np.float32) for k, v in p.items()}


def _nsa_consts():
    c = {}
    i = np.arange(32)[:, None, None, None]; nl = np.arange(128)[None, :, None, None]; cc = np.arange(2)[None, None, :, None]; q = np.arange(128)[None, None, None, :]
    c['cmask'] = ((16 * (128 * cc + nl) + 31) <= (128 * i + q)).astype(BFNP)
    i = np.arange(32)[:, None, None]; q = np.arange(128)[None, :, None]; j = np.arange(64)[None, None, :]
    qblk = (128 * i + q) // 64
    forced = (j == 0) | (j == qblk) | (j == qblk - 1); fut = j > qblk
    c['keep'] = (~(forced | fut)).astype(np.float32)
    c['addm'] = np.where(fut, -1e30, np.where(forced, 1e30, 0.0)).astype(np.float32)
    j = np.arange(64)[:, None, None]; kt = np.arange(32)[None, :, None]; k = np.arange(128)[None, None, :]
    c['selx'] = (j == 2 * kt + k // 64).astype(BFNP)
    kl = np.arange(128)[:, None]; ql = np.arange(128)[None, :]
    c['cauT'] = (kl <= ql).astype(BFNP); c['acauT'] = (kl > ql).astype(BFNP)
    n = np.arange(256)[:, None]; j = np.arange(64)[None, :]
    ov = ((16 * n < 64 * j + 64) & (16 * n + 32 > 64 * j) & (n < 255)).astype(BFNP)
    c['ovl'] = np.ascontiguousarray(np.tile(ov[:, None, :], (1, 4, 1)))
    return c


def _nsa_params(I):
    p = {}
    p['qnw'] = np.tile(I['nsa_q_norm'][0], 16)[None, :]
    kn = I['nsa_k_norm'][0]
    p['knw'] = np.concatenate([np.tile(kn[1], 4), np.tile(kn[2], 4)])[None, :]
    p['kn0'] = kn[0][:, None]
    p['b2k'] = I['cmp_k_b2'][0][:, None]; p['b2v'] = I['cmp_v_b2'][0][None, :]
    for kv, nm in (('k', 'cmp_k'), ('v', 'cmp_v')):
        p['w1' + kv] = I[nm + '_w1'][0]; p['w2' + kv] = I[nm + '_w2'][0]
        p['b1' + kv] = I[nm + '_b1'][0].reshape(2, 128).T
    p['posTk'] = I['cmp_pos_k'][0].T; p['posTv'] = I['cmp_pos_v'][0].T
    return {k: np.ascontiguousarray(v, dtype=np.float32) for k, v in p.items()}


def _shared_inputs(I):
    pa = lambda v: np.ascontiguousarray(v.reshape(16, 128).T, dtype=np.float32)
    m = {}
    for k, v in _consts().items(): m['c_' + k] = v
    rc = _rw_consts()
    for hg in range(2):
        for k, v in _rw_params(I, hg).items(): m['rw%d_%s' % (hg, k)] = v
        for k, v in rc.items(): m['rw%d_%s' % (hg, k)] = v
    for k, v in _nsa_consts().items(): m['n_' + k] = v
    for k, v in _nsa_params(I).items(): m['n_' + k] = v
    m['w_in'] = np.ascontiguousarray(I['w_in'][0]); m['mix_g'] = pa(I['mix_norm_g'][0])
    m['w_out'] = np.ascontiguousarray(I['w_out'][0]); m['moe_g'] = pa(I['moe_norm_g'][0])
    m['router_w'] = np.ascontiguousarray(I['router_w'][0]); m['router_b'] = np.ascontiguousarray(I['router_b'].reshape(1, 32))
    m['moe_w1'] = np.ascontiguousarray(I['moe_w1'][0]); m['moe_w2'] = np.ascontiguousarray(I['moe_w2'][0])
    m['moe_b1'] = np.ascontiguousarray(I['moe_b1'][0].reshape(32, 32, 128).transpose(2, 0, 1)); m['moe_b2'] = np.ascontiguousarray(I['moe_b2'][0])
    m['moe_g_row'] = np.ascontiguousarray(I['moe_norm_g'][0][None, :], dtype=np.float32)
    m['c_lts'] = (np.arange(128)[:, None] < np.arange(128)[None, :]).astype(BFNP)
    m['c_iota'] = np.tile(np.arange(CAP, dtype=np.float32)[None, :], (128, 1))
    m['c_iotap'] = np.ascontiguousarray(np.arange(128, dtype=np.float32)[:, None] + 128.0 * np.arange(NST, dtype=np.float32)[None, :])
    m['ple_g'] = pa(I['ple_norm_g'][0]); m['ple_w'] = np.ascontiguousarray(I['ple_w'][0]); m['ple_gate_w'] = np.ascontiguousarray(I['ple_gate_w'][0])
    return m


def build_program(shared):
    nc = bass.Bass("TRN2", target_bir_lowering=False)
    with ExitStack() as es:
        fw = FW(nc, es)
        def EI(n, shape=None, dt=None):
            v = shared.get(n)
            if shape is None: shape = list(v.shape)
            if dt is None: dt = BF16 if (v is not None and v.dtype == BFNP) else F32
            return fw.dram(n, shape, dt, kind="ExternalInput")
        x_d = EI("x_full", [4096, 2048], F32); xown_d = EI("x_own", [2048, 2048], F32)
        pown_d = EI("p_own", [2048, 256], F32); sel_d = EI("sel", [128, 2], F32)
        D_ = {k: EI(k) for k in shared}
        out_d = fw.dram("out", [2048, 2048], F32, kind="ExternalOutput")
        u_d = fw.dram("u_d", [4096, IN_W], F32)
        yrw_d = fw.dram("yrw_d", [4096, 1024], F32); ynsa_d = fw.dram("ynsa_d", [4096, 1024], F32)
        h1_d = fw.dram("h1_d", [2048, 2048], F32); h2_d = fw.dram("h2_d", [2048, 2048], F32)
        gates_d = fw.dram("gates_d", [2048, 32], F32)
        xn2_d = fw.dram("xn2_d", [2048, 2048], BF16); mk_d = fw.dram("mk_d", [2048, 32], F32); pos_d = fw.dram("pos_d", [2048, 32], F32)
        yc_d = fw.dram("yc_d", [32 * CAP, 2048], BF16)
        w1b_d = [fw.dram("w1b_%d" % e, [2048, 4096], BF16) for e in range(32)]
        w2b_d = [fw.dram("w2b_%d" % e, [2048, 2048], BF16) for e in range(32)]
        C = {}
        for k, dt in (('ident_b', BF16), ('ident_f', F32), ('ones', F32)):
            t = fw.sb('k_' + k, [128, 128], dt); fw.dma('sp', t[:], D_['c_' + k][:, :]); C[k] = t[:]
        stage_A(fw, C, x_d, D_['w_in'], D_['mix_g'], u_d)
        conv = conv_weights(fw, D_['moe_w1'], D_['moe_w2'], w1b_d, w2b_d)
        for hg in range(2):
            P_ = {k[4:]: v for k, v in D_.items() if k.startswith('rw%d_' % hg)}
            stage_B(fw, C, u_d, P_, yrw_d, hg, None)
        Pn = {k[2:]: v for k, v in D_.items() if k.startswith('n_')}
        stage_C(fw, C, u_d, Pn, ynsa_d, conv)
        for _ in conv: pass
        fw.barrier()
        stage_D2(fw, C, yrw_d, ynsa_d, sel_d, xown_d, D_['w_out'], D_['moe_g'], D_['moe_g_row'], D_['router_w'], D_['router_b'], h1_d, xn2_d, mk_d, pos_d, gates_d, D_['c_lts'])
        stage_E1(fw, C, w1b_d, w2b_d, D_['moe_b1'], D_['moe_b2'], xn2_d, mk_d, pos_d, D_['c_iota'], yc_d)
        stage_E2(fw, C, yc_d, pos_d, gates_d, D_['c_iotap'], h1_d, h2_d)
        stage_F(fw, C, h2_d, pown_d, D_['ple_g'], D_['ple_w'], D_['ple_gate_w'], out_d)
        fw.emit()
    return nc


def kernel(**inputs):
    I = {k: np.asarray(v) for k, v in inputs.items()}
    shared = _shared_inputs(I)
    nc = build_program(shared)
    x = I['x']; p = I['p']
    in_maps = []
    for c in range(8):
        b, s = c // 2, c % 2
        m = dict(shared)
        m['x_full'] = np.ascontiguousarray(x[b], dtype=np.float32)
        m['x_own'] = np.ascontiguousarray(x[b, s * 2048:(s + 1) * 2048], dtype=np.float32)
        m['p_own'] = np.ascontiguousarray(p[0, b, s * 2048:(s + 1) * 2048], dtype=np.float32)
        sel = np.zeros((128, 2), np.float32); sel[:, s] = 1.0
        m['sel'] = sel
        in_maps.append(m)
    res = run_bass_kernel_spmd(nc, in_maps, core_ids=list(range(8)))
    out = np.empty((4, 4096, 2048), np.float32)
    for c in range(8):
        b, s = c // 2, c % 2
        out[b, s * 2048:(s + 1) * 2048] = np.asarray(res.results[c]['out'], dtype=np.float32)
    return out
```
